# Optimizing a Trainium2 kernel written in Bass

```python
import math
import jax, jax.numpy as jnp
from jax import lax
import numpy as np

D_MODEL = 1024
BATCH = 16
SEQ = 2048
DEPTH = 1

CHUNK = 64
EPS = 1e-6
A_HEADS = 8
A_HEAD_DIM = 64
A_WIDTH = A_HEADS * A_HEAD_DIM
A_LATENT = 128
IDX_HEADS = 8
IDX_DIM = 64
TOPK_MAX = 256
Q_BLOCK = 128
REL_BUCKETS = 32
REL_MAX_DIST = 128
B_HEADS = 4
B_HEAD_DIM = 128
B_WIDTH = B_HEADS * B_HEAD_DIM
CONV_WIDTH = 4
D_MIX = A_WIDTH + B_WIDTH
D_FF = 4 * D_MODEL
_IN_SPLITS = (A_WIDTH, A_LATENT, IDX_HEADS * IDX_DIM, IDX_DIM, IDX_HEADS,
              B_WIDTH, B_WIDTH, B_WIDTH, B_HEADS, B_HEADS, B_WIDTH)
IN_COLS = sum(_IN_SPLITS)

kernel_name = 'hybrid_dsa_gdn_sandwich_block'


def rms_norm(x, g):
    xf = x.astype(jnp.float32)
    y = xf * lax.rsqrt(jnp.mean(xf * xf, axis=-1, keepdims=True) + EPS)
    return (y * g.astype(jnp.float32)).astype(x.dtype)


def l2_norm(x):
    xf = x.astype(jnp.float32)
    return xf * lax.rsqrt(jnp.sum(xf * xf, axis=-1, keepdims=True) + EPS)


def t5_bucket(rel):
    nb = REL_BUCKETS // 2
    max_exact = nb // 2
    side = jnp.where(rel > 0, nb, 0)
    n = jnp.abs(rel)
    nf = jnp.maximum(n, 1).astype(jnp.float32)
    large = max_exact + (jnp.log(nf / max_exact) / math.log(REL_MAX_DIST / max_exact)
                         * (nb - max_exact)).astype(jnp.int32)
    large = jnp.minimum(large, nb - 1)
    return side + jnp.where(n < max_exact, n, large)


def dsa_mixer(q_a, c_kv, q_idx, k_idx, w_idx, c_norm, w_uk, w_uv, rel_bias):
    B, T, _ = q_a.shape
    topk = min(TOPK_MAX, T // 4)
    nb = T // Q_BLOCK
    q = q_a.reshape(B, T, A_HEADS, A_HEAD_DIM)
    c = rms_norm(c_kv, c_norm)
    q_abs = jnp.einsum('bthd,hcd->bthc', q, w_uk) * (A_HEAD_DIM ** -0.5)
    qi = q_idx.reshape(B, T, IDX_HEADS, IDX_DIM) * (IDX_DIM ** -0.5)
    wi = w_idx * (IDX_HEADS ** -0.5)
    key_pos = jnp.arange(T)

    def blocks(a):
        return jnp.moveaxis(a.reshape(B, nb, Q_BLOCK, *a.shape[2:]), 1, 0)

    def one_block(args):
        blk, qa_b, qi_b, wi_b = args
        t = blk * Q_BLOCK + jnp.arange(Q_BLOCK)
        limit = (t // CHUNK + 1) * CHUNK
        rel = jax.nn.relu(jnp.einsum('bqhd,bsd->bqhs', qi_b, k_idx))
        score = jnp.einsum('bqhs,bqh->bqs', rel, wi_b).astype(jnp.float32)
        score = jnp.where(key_pos[None, None, :] < limit[None, :, None], score, -jnp.inf)
        _, idx = lax.top_k(score, topk)
        valid = idx < limit[None, :, None]
        c_sel = jax.vmap(lambda cb, ib: cb[ib])(c, idx)
        logits = jnp.einsum('bqhc,bqkc->bqkh', qa_b, c_sel).astype(jnp.float32)
        logits = logits + rel_bias[t5_bucket(idx - t[None, :, None])].astype(jnp.float32)
        logits = jnp.where(valid[..., None], logits, -jnp.inf)
        p = jax.nn.softmax(logits, axis=2).astype(c_sel.dtype)
        return jnp.einsum('bqkh,bqkc->bqhc', p, c_sel)

    o_lat = lax.map(one_block, (jnp.arange(nb), blocks(q_abs), blocks(qi), blocks(wi)))
    o_lat = jnp.moveaxis(o_lat, 0, 1).reshape(B, T, A_HEADS, A_LATENT)
    o = jnp.einsum('bthc,hcd->bthd', o_lat, w_uv)
    return o.reshape(B, T, A_WIDTH)


def causal_depthwise_conv(x, w):
    C = x.shape[-1]
    return lax.conv_general_dilated(x, w[:, None, :].astype(x.dtype), window_strides=(1,),
                                    padding=[(CONV_WIDTH - 1, 0)],
                                    dimension_numbers=('NWC', 'WIO', 'NWC'),
                                    feature_group_count=C)


def chunked_gated_delta_rule(q, k, v, g, beta):
    B, T, H, DK = q.shape
    DV = v.shape[-1]
    N = T // CHUNK

    def chunks(a):
        return jnp.moveaxis(a.reshape(B, N, CHUNK, H, *a.shape[3:]), 3, 1)

    q = chunks(q) * (DK ** -0.5)
    k = chunks(k)
    v = chunks(v)
    g = jnp.cumsum(chunks(g), axis=-1)
    beta = chunks(beta)
    causal = jnp.tril(jnp.ones((CHUNK, CHUNK), dtype=bool))
    strict = jnp.tril(jnp.ones((CHUNK, CHUNK), dtype=bool), -1)
    decay = jnp.exp(jnp.where(causal, g[..., :, None] - g[..., None, :], -jnp.inf))
    k_beta = k * beta[..., None]
    lower = jnp.where(strict, jnp.einsum('bhncd,bhnsd->bhncs', k_beta, k) * decay, 0.0)
    eye = jnp.eye(CHUNK, dtype=q.dtype)
    t_inv = lax.linalg.triangular_solve(eye + lower, jnp.broadcast_to(eye, lower.shape),
                                        left_side=True, lower=True)
    u = t_inv @ (v * beta[..., None])
    w = t_inv @ (k_beta * jnp.exp(g)[..., None])
    intra = jnp.where(causal, jnp.einsum('bhncd,bhnsd->bhncs', q, k) * decay, 0.0)

    def step(state, xs):
        q_c, k_c, u_c, w_c, g_c, a_c = xs
        v_new = u_c - w_c @ state
        o_c = (q_c * jnp.exp(g_c)[..., None]) @ state + a_c @ v_new
        g_last = g_c[..., -1:]
        state = state * jnp.exp(g_last)[..., None] + jnp.einsum(
            'bhcd,bhce->bhde', k_c * jnp.exp(g_last - g_c)[..., None], v_new)
        return state, o_c

    xs = tuple(jnp.moveaxis(a, 2, 0) for a in (q, k, u, w, g, intra))
    state0 = jnp.zeros((B, H, DK, DV), q.dtype)
    _, o = lax.scan(step, state0, xs)
    o = jnp.moveaxis(jnp.moveaxis(o, 0, 2), 1, 3)
    return o.reshape(B, T, H, DV)


def gated_deltanet_mixer(q_b, k_b, v_b, a_b, b_b, z_b, conv_w, a_log, dt_bias, o_norm):
    B, T, _ = q_b.shape
    qkv = jax.nn.silu(causal_depthwise_conv(jnp.concatenate([q_b, k_b, v_b], axis=-1), conv_w))
    q, k, v = jnp.split(qkv, 3, axis=-1)
    q = l2_norm(q.reshape(B, T, B_HEADS, B_HEAD_DIM))
    k = l2_norm(k.reshape(B, T, B_HEADS, B_HEAD_DIM))
    v = v.reshape(B, T, B_HEADS, B_HEAD_DIM).astype(jnp.float32)
    g = -jnp.exp(a_log.astype(jnp.float32)) * jax.nn.softplus(
        a_b.astype(jnp.float32) + dt_bias.astype(jnp.float32))
    beta = jax.nn.sigmoid(b_b.astype(jnp.float32))
    o = chunked_gated_delta_rule(q, k, v, g, beta)
    z = z_b.reshape(B, T, B_HEADS, B_HEAD_DIM).astype(jnp.float32)
    o = rms_norm(o, o_norm) * jax.nn.silu(z)
    return o.reshape(B, T, B_WIDTH).astype(q_b.dtype)


def hybrid_mixer(h, w_in, c_norm, w_uk, w_uv, rel_bias, conv_w, a_log, dt_bias, o_norm, w_out):
    proj = h @ w_in
    split_points = [int(p) for p in np.cumsum(np.array(_IN_SPLITS))[:-1]]
    q_a, c_kv, q_idx, k_idx, w_idx, q_b, k_b, v_b, a_b, b_b, z_b = jnp.split(proj, split_points, axis=-1)
    o_a = dsa_mixer(q_a, c_kv, q_idx, k_idx, w_idx, c_norm, w_uk, w_uv, rel_bias)
    o_b = gated_deltanet_mixer(q_b, k_b, v_b, a_b, b_b, z_b, conv_w, a_log, dt_bias, o_norm)
    return jnp.concatenate([o_a, o_b], axis=-1) @ w_out


def setup_inputs(seed: int = 0) -> dict:
    key = jax.random.key(seed)
    ks = jax.random.split(key, 18)
    f32 = jnp.float32

    def nrm(k, shape, scale):
        return jax.random.normal(k, shape, f32) * scale

    def gain(k, shape):
        return 1.0 + 0.01 * jax.random.normal(k, shape, f32)

    dt = jnp.exp(jax.random.uniform(ks[8], (DEPTH, B_HEADS), f32, math.log(1e-3), math.log(1e-1)))
    return {
        'x': nrm(ks[0], (BATCH, SEQ, D_MODEL), 1.0),
        'w_in': nrm(ks[1], (DEPTH, D_MODEL, IN_COLS), D_MODEL ** -0.5),
        'c_norm': gain(ks[2], (DEPTH, A_LATENT)),
        'w_uk': nrm(ks[3], (DEPTH, A_HEADS, A_LATENT, A_HEAD_DIM), A_LATENT ** -0.5),
        'w_uv': nrm(ks[4], (DEPTH, A_HEADS, A_LATENT, A_HEAD_DIM), A_LATENT ** -0.5),
        'rel_bias': nrm(ks[5], (REL_BUCKETS, A_HEADS), 0.5),
        'conv_w': nrm(ks[6], (DEPTH, CONV_WIDTH, 3 * B_WIDTH), CONV_WIDTH ** -0.5),
        'a_log': jnp.log(jax.random.uniform(ks[7], (DEPTH, B_HEADS), f32, 1.0, 16.0)),
        'dt_bias': dt + jnp.log(-jnp.expm1(-dt)),
        'o_norm': gain(ks[9], (DEPTH, B_HEAD_DIM)),
        'w_out': nrm(ks[10], (DEPTH, D_MIX, D_MODEL), D_MIX ** -0.5),
        'pre_norm_mix': gain(ks[11], (DEPTH, D_MODEL)),
        'post_norm_mix': gain(ks[12], (DEPTH, D_MODEL)),
        'pre_norm_mlp': gain(ks[13], (DEPTH, D_MODEL)),
        'post_norm_mlp': gain(ks[14], (DEPTH, D_MODEL)),
        'w_mlp_in': nrm(ks[15], (DEPTH, D_MODEL, D_FF), D_MODEL ** -0.5),
        'w_mlp_out': nrm(ks[16], (DEPTH, D_FF, D_MODEL), D_FF ** -0.5),
    }


def reference(x, w_in, c_norm, w_uk, w_uv, rel_bias, conv_w, a_log, dt_bias, o_norm, w_out,
              pre_norm_mix, post_norm_mix, pre_norm_mlp, post_norm_mlp, w_mlp_in, w_mlp_out):
    for l in range(DEPTH):
        h = rms_norm(x, pre_norm_mix[l])
        mix = hybrid_mixer(h, w_in[l], c_norm[l], w_uk[l], w_uv[l], rel_bias, conv_w[l],
                           a_log[l], dt_bias[l], o_norm[l], w_out[l])
        x = x + rms_norm(mix, post_norm_mix[l])
        h = rms_norm(x, pre_norm_mlp[l])
        y = jnp.square(jax.nn.relu(h @ w_mlp_in[l])) @ w_mlp_out[l]
        x = x + rms_norm(y, post_norm_mlp[l])
    return x
```

```python
import numpy as np
from contextlib import ExitStack
import concourse.bass as bass
import concourse.mybir as mybir
from concourse.bass_utils import run_bass_kernel_spmd

F32 = mybir.dt.float32
BF16 = mybir.dt.bfloat16
AF = mybir.ActivationFunctionType
ALU = mybir.AluOpType
AX = mybir.AxisListType

D = 1024
T = 2048
NSEQ = 2
NT = T // 128
INC = 3280
DFF = 4096
EPS = 1e-6
O_QA, O_CKV, O_QI, O_KI, O_WI, O_QB, O_KB, O_VB, O_AB, O_BB, O_ZB = (
    0, 512, 640, 1152, 1216, 1224, 1736, 2248, 2760, 2764, 2768)
NBIS = 22
TOPK = 256
NEG = -30000.0


class Dep:
    __slots__ = ("w", "r", "x")

    def __init__(self):
        self.w = None
        self.r = []
        self.x = False


class Tl:
    def __init__(self, t, nslots=0):
        self.t = t
        self.d = Dep()
        self.s = [Dep() for _ in range(nslots)]

    def __getitem__(self, idx):
        return self.t[idx]


class KB:
    def __init__(self, nc, es):
        self.nc = nc
        self.es = es
        self.engs = {"pe": nc.tensor, "dve": nc.vector, "act": nc.scalar,
                     "pool": nc.gpsimd, "sp": nc.sync}
        self.sems = {}
        self.cnt = {}
        for n in ("pe", "dve", "act", "pool"):
            self.sems[n] = es.enter_context(nc.semaphore("sem_" + n))
            self.cnt[n] = 0
        self.waited = {n: {} for n in self.engs}
        self.dq = {}
        for q, n in (("sp", 20), ("act", 8), ("pool", 8)):
            lst = []
            for i in range(n):
                nm = "dma_%s_%d" % (q, i)
                self.sems[nm] = es.enter_context(nc.semaphore(nm))
                self.cnt[nm] = 0
                lst.append(nm)
            self.dq[q] = [lst, 0]
        self.uid = 0
        self.fence = []
        self.limit = None

    def fence_now(self):
        self.fence = [(k, v) for k, v in self.cnt.items() if v > 0]

    def scope(self):
        kb = self

        class _Scope(ExitStack):
            def __exit__(self, *a):
                r = ExitStack.__exit__(self, *a)
                kb.fence_now()
                return r
        return _Scope()

    def sb(self, es, name, shape, dt, nslots=0):
        self.uid += 1
        t = Tl(es.enter_context(self.nc.sbuf_tensor("%s_%d" % (name, self.uid), shape, dt)), nslots)
        t.d.r = list(self.fence)
        for d in t.s:
            d.r = list(self.fence)
        return t

    def ps(self, es, name, shape, dt):
        self.uid += 1
        t = Tl(es.enter_context(self.nc.psum_tensor("%s_%d" % (name, self.uid), shape, dt)))
        t.d.x = True
        return t

    def _deps(self, eng, reads, writes):
        need = {}

        def add(p):
            if p is None:
                return
            s, v = p
            if need.get(s, 0) < v:
                need[s] = v

        for d in reads:
            add(d.w)
            if d.x:
                for p in d.r:
                    if p[0] != eng:
                        add(p)
        for d in writes:
            if d.w is not None and not (eng == "pe" and d.w[0] == "pe"):
                add(d.w)
            for p in d.r:
                add(p)
        e = self.engs[eng]
        wd = self.waited[eng]
        for s, v in need.items():
            if wd.get(s, 0) < v:
                e.wait_ge(self.sems[s], v)
                wd[s] = v

    def _norm(self, lst):
        out = []
        for x in lst:
            out.append(x.d if isinstance(x, Tl) else x)
        return out

    def _mark(self, me, reads, writes):
        for d in reads:
            d.r = [p for p in d.r if p[0] != me[0]]
            d.r.append(me)
        for d in writes:
            d.w = me
            d.r = []

    def op(self, eng, fn, reads=(), writes=()):
        if self.limit is not None:
            if self.limit <= 0:
                return None
            self.limit -= 1
            if self.limit == 0:
                import traceback
                print("LAST OP:", eng, traceback.extract_stack()[-2].lineno)
        reads = self._norm(reads)
        writes = self._norm(writes)
        self._deps(eng, reads, writes)
        inst = fn(self.engs[eng])
        self.cnt[eng] += 1
        inst.then_inc(self.sems[eng], 1)
        self._mark((eng, self.cnt[eng]), reads, writes)
        return inst

    def dma(self, q, out, in_, reads=(), writes=(), nc_ok=False):
        reads = self._norm(reads)
        writes = self._norm(writes)
        lst, i = self.dq[q]
        sn = lst[i % len(lst)]
        self.dq[q][1] = i + 1
        e = self.engs[q]
        wd = self.waited[q]
        if wd.get(sn, 0) < self.cnt[sn]:
            e.wait_ge(self.sems[sn], self.cnt[sn])
            wd[sn] = self.cnt[sn]
        self._deps(q, reads, writes)
        if nc_ok:
            with self.nc.allow_non_contiguous_dma(reason="small strided"):
                inst = e.dma_start(out=out, in_=in_)
        else:
            inst = e.dma_start(out=out, in_=in_)
        self.cnt[sn] += 16
        inst.then_inc(self.sems[sn], 16)
        self._mark((sn, self.cnt[sn]), reads, writes)
        return inst

    def wait_all(self, eng, deps):
        deps = self._norm(deps)
        self._deps(eng, deps, [])


def bcast_row(ap_1d_dram, nparts, n):
    return bass.AP(tensor=ap_1d_dram.tensor, offset=ap_1d_dram.offset, ap=[[0, nparts], [1, n]])


def t5_bucket_np(rel):
    nb = 16
    max_exact = 8
    side = np.where(rel > 0, nb, 0)
    n = np.abs(rel)
    nf = np.maximum(n, 1).astype(np.float32)
    large = max_exact + (np.log(nf / max_exact) / np.log(np.float32(128 / max_exact))
                         * (nb - max_exact)).astype(np.int32)
    large = np.minimum(large, nb - 1)
    return side + np.where(n < max_exact, n, large)


def onehot_rev():
    j = np.arange(384)
    dd = 127 - j
    b = t5_bucket_np(dd)
    oh = np.zeros((32, 384), np.float32)
    oh[b, j] = 1.0
    return oh


def build_nc(dbg=(), stop_after=None):
    nc = bass.Bass("TRN2", target_bir_lowering=False)

    def din(name, shape, dt=F32):
        return nc.dram_tensor(name, list(shape), dt, kind="ExternalInput").ap()

    x = din("x", [NSEQ * T, D])
    w_in = din("w_in", [D, INC])
    c_norm = din("c_norm", [128])
    w_uk = din("w_uk", [8, 128, 64])
    w_uv = din("w_uv", [8, 128, 64])
    rel_bias = din("rel_bias", [32, 8])
    conv_w = din("conv_w", [4, 1536])
    a_log = din("a_log", [4])
    dt_bias = din("dt_bias", [4])
    o_norm = din("o_norm", [128])
    w_out = din("w_out", [D, D])
    g_pre_mix = din("pre_norm_mix", [D])
    g_post_mix = din("post_norm_mix", [D])
    g_pre_mlp = din("pre_norm_mlp", [D])
    g_post_mlp = din("post_norm_mlp", [D])
    w1 = din("w_mlp_in", [D, DFF])
    w2 = din("w_mlp_out", [DFF, D])
    ohr = din("ohr", [32, 384])
    out = nc.dram_tensor("out", [NSEQ * T, D], F32, kind="ExternalOutput").ap()

    def dscr(name, shape, dt):
        return nc.dram_tensor(name, list(shape), dt, kind="Internal").ap()

    winb = dscr("winb", [D, INC], BF16)
    woutb = dscr("woutb", [D, D], BF16)
    w1b = dscr("w1b", [D, DFF], BF16)
    w2b = dscr("w2b", [DFF, D], BF16)
    vscr = dscr("vscr", [8, 384], F32)

    dbg_out = {}

    def dbg_tensor(name, shape, dt=F32):
        if name in dbg:
            dbg_out[name] = nc.dram_tensor("dbg_" + name, list(shape), dt, kind="ExternalOutput").ap()
            return dbg_out[name]
        return None

    with ExitStack() as es:
        K = KB(nc, es)
        _build(nc, K, es, locals(), dbg, stop_after, dbg_tensor)
    return nc


class Stop(Exception):
    pass


def _build(nc, K, es, g, dbg, stop_after, dbg_tensor):
    x = g["x"]; out = g["out"]
    PS = [K.ps(es, "ps%d" % i, [128, 512], F32) for i in range(8)]

    def psbf(i):
        return PS[i].t[:].bitcast(BF16)

    cst = lambda name, shape, dt=F32: K.sb(es, name, shape, dt)
    dif = cst("dif", [128, 128])
    K.op("pool", lambda e: e.iota(dif[:], pattern=[[1, 128]], base=0, channel_multiplier=-1,
                                  allow_small_or_imprecise_dtypes=True), [], [dif])
    ident = cst("ident", [128, 128])
    identb = cst("identb", [128, 128], BF16)
    K.op("dve", lambda e: e.tensor_scalar(out=ident[:], in0=dif[:], scalar1=0.0, scalar2=None,
                                          op0=ALU.is_equal), [dif], [ident])
    K.op("dve", lambda e: e.tensor_copy(out=identb[:], in_=ident[:]), [ident], [identb])
    ones = cst("ones", [128, 128])
    K.op("dve", lambda e: e.memset(ones[:], 1.0), [], [ones])
    m_le = cst("m_le", [64, 64])
    m_lt = cst("m_lt", [64, 64])
    m_ge = cst("m_ge", [64, 64])
    m_gt = cst("m_gt", [64, 64])
    for m, opx in ((m_le, ALU.is_le), (m_lt, ALU.is_lt), (m_ge, ALU.is_ge), (m_gt, ALU.is_gt)):
        K.op("dve", lambda e, m=m, opx=opx: e.tensor_scalar(out=m[:], in0=dif[0:64, 0:64], scalar1=0.0,
                                                            scalar2=None, op0=opx), [dif], [m])
    cm2 = cst("cm2", [128, 64])
    cmr2 = cst("cmr2", [128, 64])
    K.op("dve", lambda e: e.tensor_scalar(out=cm2[0:64, :], in0=dif[0:64, 0:64], scalar1=0.0, scalar2=None,
                                          op0=ALU.is_ge), [dif], [cm2])
    K.op("dve", lambda e: e.tensor_scalar(out=cm2[64:128, :], in0=dif[64:128, 64:128], scalar1=0.0,
                                          scalar2=None, op0=ALU.is_ge), [dif], [cm2])
    K.op("dve", lambda e: e.tensor_scalar(out=cmr2[0:64, :], in0=dif[0:64, 0:64], scalar1=0.0, scalar2=None,
                                          op0=ALU.is_lt), [dif], [cmr2])
    K.op("dve", lambda e: e.tensor_scalar(out=cmr2[64:128, :], in0=dif[64:128, 64:128], scalar1=0.0,
                                          scalar2=None, op0=ALU.is_lt), [dif], [cmr2])
    negdiag = cst("negdiag", [128, 128])
    K.op("dve", lambda e: e.memset(negdiag[:], 0.0), [], [negdiag])
    K.op("dve", lambda e: e.memset(negdiag[0:64, 64:128], -1e30), [], [negdiag])
    kreq = cst("kreq", [128, NT])
    for qt in range(NT):
        for hf in range(2):
            lim = qt * 128 + (hf + 1) * 64
            K.op("dve", lambda e, qt=qt, hf=hf, lim=lim: e.memset(
                kreq[hf * 64:(hf + 1) * 64, qt:qt + 1], float(min(TOPK, lim))), [], [kreq])
    pow2 = cst("pow2", [128, NBIS])
    for i in range(NBIS):
        K.op("pool", lambda e, i=i: e.memset(pow2[:, i:i + 1], float(2.0 ** (-i))), [], [pow2])
    epsc = cst("epsc", [128, 1])
    K.op("dve", lambda e: e.memset(epsc[:], EPS), [], [epsc])
    onec = cst("onec", [128, 1])
    K.op("dve", lambda e: e.memset(onec[:], 1.0), [], [onec])

    def gT(vec, name):
        t = cst(name, [128, 8])
        K.dma("sp", t[:], vec.rearrange("(k p) -> p k", p=128), [], [t], nc_ok=True)
        return t
    g1T = gT(g["g_pre_mix"], "g1T")
    g3T = gT(g["g_pre_mlp"], "g3T")

    def gB(vec, n, name, parts=128):
        t = cst(name, [parts, n])
        K.dma("sp", t[:], bcast_row(vec, parts, n), [], [t], nc_ok=True)
        return t
    g2B = gB(g["g_post_mix"], D, "g2B")
    g4B = gB(g["g_post_mlp"], D, "g4B")
    cnB = gB(g["c_norm"], 128, "cnB")
    onB = gB(g["o_norm"], 128, "onB", 64)
    alB = gB(g["a_log"], 4, "alB", 64)
    dtB = gB(g["dt_bias"], 4, "dtB", 64)
    b15B = gB(g["rel_bias"][15, :], 8, "b15B")
    negA = cst("negA", [64, 4])
    K.op("act", lambda e: e.activation(out=negA[:], in_=alB[:], func=AF.Exp), [alB], [negA])
    K.op("dve", lambda e: e.tensor_scalar(out=negA[:], in0=negA[:], scalar1=-1.0, scalar2=None, op0=ALU.mult),
         [negA], [negA])
    cw = cst("cw", [128, 12, 4])
    for j in range(4):
        K.dma("sp", cw[:, :, j], g["conv_w"][j, :].rearrange("(k p) -> p k", p=128), [], [cw], nc_ok=True)

    wukT = cst("wukT", [128, 4, 128], BF16)
    wuvb = cst("wuvb", [128, 512], BF16)
    Tb = cst("Tb", [128, 8, 256], BF16)
    with K.scope() as es2:
        tmpk = K.sb(es2, "tmpk", [128, 8, 64], F32)
        tmpv = K.sb(es2, "tmpv", [128, 8, 64], F32)
        K.dma("sp", tmpk[:], g["w_uk"].rearrange("h c d -> c h d"), [], [tmpk], nc_ok=True)
        K.dma("sp", tmpv[:], g["w_uv"].rearrange("h c d -> c h d"), [], [tmpv], nc_ok=True)
        K.op("dve", lambda e: e.tensor_copy(out=wuvb[:], in_=tmpv[:].rearrange("p h d -> p (h d)")),
             [tmpv], [wuvb])
        for j in range(4):
            K.op("pe", lambda e, j=j: e.transpose(
                out=PS[0][:, j * 128:(j + 1) * 128],
                in_=tmpk[:, 2 * j:2 * j + 2, :].rearrange("p h d -> p (h d)"), identity=ident[:]),
                [tmpk, ident], [PS[0]])
        K.op("dve", lambda e: e.tensor_copy(out=wukT[:].rearrange("p j c -> p (j c)"), in_=PS[0][:, :]),
             [PS[0]], [wukT])

        rb = K.sb(es2, "rb", [32, 8], F32)
        rbT = K.sb(es2, "rbT", [8, 32], F32)
        oh = K.sb(es2, "oh", [32, 384], F32)
        K.dma("sp", rb[:], g["rel_bias"], [], [rb])
        K.dma("sp", rbT[:], g["rel_bias"].rearrange("b h -> h b"), [], [rbT], nc_ok=True)
        K.dma("sp", oh[:], g["ohr"], [], [oh])
        K.op("pe", lambda e: e.matmul(PS[1][0:8, 0:384], lhsT=rb[:], rhs=oh[:], start=True, stop=True),
             [rb, oh], [PS[1]])
        vr = K.sb(es2, "vr", [8, 384], F32)
        K.op("dve", lambda e: e.tensor_scalar(out=vr[:], in0=PS[1][0:8, 0:384], scalar1=rbT[:, 15:16],
                                              scalar2=None, op0=ALU.subtract), [PS[1], rbT], [vr])
        vs = g["vscr"]
        dvs = Dep()
        K.dma("sp", vs, vr[:], [vr], [dvs])
        Tb32 = K.sb(es2, "Tb32", [128, 8, 256], F32)
        src = bass.AP(tensor=vs.tensor, offset=vs.offset, ap=[[1, 128], [384, 8], [1, 256]])
        K.dma("sp", Tb32[:], src, [dvs], [Tb32], nc_ok=True)
        smt = K.sb(es2, "smt", [128, 128], F32)
        Jm = K.sb(es2, "Jm", [128, 128], F32)
        K.op("pool", lambda e: e.iota(smt[:], pattern=[[1, 128]], base=0, channel_multiplier=1,
                                      allow_small_or_imprecise_dtypes=True), [], [smt])
        K.op("dve", lambda e: e.tensor_scalar(out=Jm[:], in0=smt[:], scalar1=127.0, scalar2=None,
                                              op0=ALU.is_equal), [smt], [Jm])
        Tbf = Tb32[:].rearrange("p h u -> p (h u)")
        for q in range(4):
            K.op("pe", lambda e, q=q: e.matmul(PS[2 + q][:, :], lhsT=Jm[:], rhs=Tbf[:, q * 512:(q + 1) * 512],
                                               start=True, stop=True), [Jm, Tb32], [PS[2 + q]])
            K.op("dve", lambda e, q=q: e.tensor_copy(
                out=Tb[:].rearrange("p h u -> p (h u)")[:, q * 512:(q + 1) * 512], in_=PS[2 + q][:, :]),
                [PS[2 + q]], [Tb])
        d = dbg_tensor("Tb", [128, 8, 256], BF16)
        if d is not None:
            K.dma("sp", d, Tb[:], [Tb], [Dep()])

        stg_deps = {}
        engs_rr = ["dve", "pool", "act"]
        rr = [0]

        def stage(src, dst, nrows, ncols, gT_tile, key):
            dd = Dep()
            stg_deps[key] = dd
            f = [K.sb(es2, "stf", [128, 2048], F32) for _ in range(2)]
            b = [K.sb(es2, "stb", [128, 2048], BF16) for _ in range(2)]
            i = 0
            for kc in range(nrows // 128):
                for c0 in range(0, ncols, 2048):
                    cn = min(2048, ncols - c0)
                    ft, bt = f[i % 2], b[i % 2]
                    K.dma("sp", ft[:, 0:cn], src[kc * 128:(kc + 1) * 128, c0:c0 + cn], [], [ft])
                    eng = engs_rr[rr[0] % 3]
                    rr[0] += 1
                    if gT_tile is not None:
                        if eng == "act":
                            K.op("act", lambda e, ft=ft, bt=bt, cn=cn, kc=kc: e.activation(
                                out=bt[:, 0:cn], in_=ft[:, 0:cn], func=AF.Copy, scale=gT_tile[:, kc:kc + 1]),
                                [ft, gT_tile], [bt])
                        else:
                            K.op(eng, lambda e, ft=ft, bt=bt, cn=cn, kc=kc: e.tensor_scalar(
                                out=bt[:, 0:cn], in0=ft[:, 0:cn], scalar1=gT_tile[:, kc:kc + 1], scalar2=None,
                                op0=ALU.mult), [ft, gT_tile], [bt])
                    else:
                        if eng == "act":
                            K.op("act", lambda e, ft=ft, bt=bt, cn=cn: e.activation(
                                out=bt[:, 0:cn], in_=ft[:, 0:cn], func=AF.Copy), [ft], [bt])
                        else:
                            K.op(eng, lambda e, ft=ft, bt=bt, cn=cn: e.tensor_copy(out=bt[:, 0:cn], in_=ft[:, 0:cn]),
                                 [ft], [bt])
                    K.dma("sp", dst[kc * 128:(kc + 1) * 128, c0:c0 + cn], bt[:, 0:cn], [bt], [Dep()])
                    i += 1

        stage(g["w_in"], g["winb"], D, INC, g1T, "win")
        stage(g["w_out"], g["woutb"], D, D, None, "wout")
        stage(g["w1"], g["w1b"], D, DFF, g3T, "w1")
        stage(g["w2"], g["w2b"], DFF, D, None, "w2")
    def drain_sp():
        for sn in K.dq["sp"][0]:
            for q in ("sp", "act", "pool"):
                if K.waited[q].get(sn, 0) < K.cnt[sn]:
                    K.engs[q].wait_ge(K.sems[sn], K.cnt[sn])
                    K.waited[q][sn] = K.cnt[sn]
    drain_sp()
    if stop_after == "stage":
        return

    for seq in range(NSEQ):
        with K.scope() as ss:
            _seq(nc, K, ss, g, dbg, stop_after, dbg_tensor, seq, locals())
        if stop_after is not None:
            break
    for q in ("sp", "act", "pool"):
        for sn in K.dq[q][0]:
            if K.waited["sp"].get(sn, 0) < K.cnt[sn]:
                K.engs["sp"].wait_ge(K.sems[sn], K.cnt[sn])
                K.waited["sp"][sn] = K.cnt[sn]


def _seq(nc, K, ss, g, dbg, stop_after, dbg_tensor, seq, L):
    PS = L["PS"]; psbf = L["psbf"]
    ident = L["ident"]; identb = L["identb"]; ones = L["ones"]
    epsc = L["epsc"]
    x = g["x"]; out = g["out"]
    tok0 = seq * T
    dbgon = (seq == 0)

    def dump(name, ap_sb, deps, shape, dt=F32):
        if not dbgon:
            return
        d = dbg_tensor(name, shape, dt)
        if d is not None:
            K.dma("sp", d, ap_sb, deps, [Dep()], nc_ok=True)

    mixT = K.sb(ss, "mixT", [128, 8, T], BF16)

    with K.scope() as s1:
        xT = K.sb(s1, "xT", [128, 8, T], BF16)
        with K.scope() as sa:
            xin = [K.sb(sa, "xin", [128, D], F32) for _ in range(2)]
            xb = [K.sb(sa, "xb", [128, D], BF16) for _ in range(2)]
            junk = K.sb(sa, "junk", [128, D], BF16)
            st = [K.sb(sa, "st", [128, 4], F32) for _ in range(2)]
            for i in range(NT):
                xi, xbi, sti = xin[i % 2], xb[i % 2], st[i % 2]
                K.dma("sp", xi[:], x[tok0 + i * 128: tok0 + (i + 1) * 128, :], [], [xi])
                K.op("act", lambda e: e.activation(out=junk[:], in_=xi[:], func=AF.Square,
                                                   accum_out=sti[:, 0:1]), [xi], [junk, sti])
                K.op("act", lambda e: e.activation(out=sti[:, 1:2], in_=sti[:, 0:1], func=AF.Sqrt,
                                                   bias=epsc[:, 0:1], scale=1.0 / D), [sti, epsc], [sti])
                K.op("dve", lambda e: e.reciprocal(out=sti[:, 2:3], in_=sti[:, 1:2]), [sti], [sti])
                K.op("dve", lambda e: e.tensor_scalar(out=xbi[:], in0=xi[:], scalar1=sti[:, 2:3], scalar2=None,
                                                      op0=ALU.mult), [xi, sti], [xbi])
                pb = PS[i % 2]
                for kc in range(8):
                    K.op("pe", lambda e, kc=kc: e.transpose(
                        out=pb.t[:].bitcast(BF16)[:, kc * 128:(kc + 1) * 128],
                        in_=xbi[:, kc * 128:(kc + 1) * 128], identity=identb[:]), [xbi, identb], [pb])
                K.op("act" if i % 2 else "dve", lambda e: (e.tensor_copy if hasattr(e, "tensor_copy") else e.copy)(
                    out=xT[:, :, i * 128:(i + 1) * 128],
                    in_=pb.t[:].bitcast(BF16).rearrange("p (k t) -> p k t", k=8)), [pb], [xT])
        dump("xT", xT[:, :, 0:128], [xT], [128, 8, 128], BF16)
        if stop_after == "A0":
            return
        _gdn(nc, K, s1, g, dbg, stop_after, dump, seq, L, xT, mixT)
        if stop_after == "gdn":
            return
        _dsa(nc, K, s1, g, dbg, stop_after, dump, seq, L, xT, mixT)
        if stop_after == "dsa":
            return
    _mlp(nc, K, ss, g, dbg, stop_after, dump, seq, L, mixT)


def bc_mid(ap2, n):
    return ap2.unsqueeze(1).to_broadcast([ap2.shape[0], n, ap2.shape[1]])


def bc_last(ap2, n):
    return ap2.unsqueeze(2).to_broadcast([ap2.shape[0], ap2.shape[1], n])


def _gdn(nc, K, s1, g, dbg, stop_after, dump, seq, L, xT, mixT):
    PS = L["PS"]
    ident = L["ident"]; identb = L["identb"]; ones = L["ones"]
    epsc = L["epsc"]; onec = L["onec"]
    cm2 = L["cm2"]; cmr2 = L["cmr2"]
    m_lt = L["m_lt"]; m_gt = L["m_gt"]; m_ge = L["m_ge"]
    cw = L["cw"]; onB = L["onB"]; dtB = L["dtB"]; negA = L["negA"]
    winb = g["winb"]
    NCH = T // 64
    rr = [0]

    def nb():
        rr[0] += 1
        return PS[rr[0] % 8]

    def bfv(p):
        return p.t[:].bitcast(BF16)

    with K.scope() as sg:
        cvT = K.sb(sg, "cvT", [128, 12, T], BF16)
        sz = K.sb(sg, "sz", [64, NCH, 512], BF16)
        abbb = K.sb(sg, "abbb", [64, NCH, 8], F32)
        with K.scope() as sw:
            NG = INC - O_QB
            wg = K.sb(sw, "wg", [128, 8, NG], BF16)
            K.dma("sp", wg[:], winb[:, O_QB:INC].rearrange("(k p) c -> p k c", p=128), [], [wg])
            ev = 0
            for tb in range(4):
                for cc in range(12):
                    p = nb()
                    for kc in range(8):
                        K.op("pe", lambda e: e.matmul(p[:, :], lhsT=wg[:, kc, cc * 128:(cc + 1) * 128],
                                                      rhs=xT[:, kc, tb * 512:(tb + 1) * 512],
                                                      start=(kc == 0), stop=(kc == 7)), [wg, xT], [p])
                    if ev % 2 == 0:
                        K.op("act", lambda e: e.copy(out=cvT[:, cc, tb * 512:(tb + 1) * 512], in_=p[:, :]),
                             [p], [cvT])
                    else:
                        K.op("dve", lambda e: e.tensor_copy(out=cvT[:, cc, tb * 512:(tb + 1) * 512], in_=p[:, :]),
                             [p], [cvT])
                    ev += 1
            for ch in range(NCH):
                p = nb()
                pz = nb()
                for kc in range(8):
                    K.op("pe", lambda e: e.matmul(p[0:64, 0:8], lhsT=xT[:, kc, ch * 64:(ch + 1) * 64],
                                                  rhs=wg[:, kc, 1536:1544], start=(kc == 0), stop=(kc == 7)),
                         [wg, xT], [p])
                for kc in range(8):
                    K.op("pe", lambda e: e.matmul(pz[0:64, :], lhsT=xT[:, kc, ch * 64:(ch + 1) * 64],
                                                  rhs=wg[:, kc, 1544:2056], start=(kc == 0), stop=(kc == 7)),
                         [wg, xT], [pz])
                K.op("dve", lambda e: e.tensor_copy(out=abbb[:, ch, :], in_=p[0:64, 0:8]), [p], [abbb])
                K.op("act", lambda e: e.activation(out=sz[:, ch, :], in_=pz[0:64, :], func=AF.Silu), [pz], [sz])
        dump("qkv_pre", cvT[:, :, 0:256], [cvT], [128, 12, 256], BF16)
        dump("abbb", abbb[:, 0:4, :], [abbb], [64, 4, 8])

        gst = K.sb(sg, "gst", [64, NCH * 4], F32)
        beta = K.sb(sg, "beta", [64, NCH * 4], F32)
        eg = K.sb(sg, "eg", [64, NCH * 4], F32)
        egr = K.sb(sg, "egr", [64, NCH * 4], F32)
        egl = K.sb(sg, "egl", [128, NCH * 4], F32)
        gv = lambda t: t[:].rearrange("p (c h) -> p c h", h=4)
        K.op("dve", lambda e: e.tensor_tensor(out=gv(gst), in0=abbb[:, :, 0:4], in1=bc_mid(dtB[:, :], NCH),
                                              op=ALU.add), [abbb, dtB], [gst])
        K.op("act", lambda e: e.activation(out=gst[:], in_=gst[:], func=AF.Exp), [gst], [gst])
        K.op("act", lambda e: e.activation(out=gst[:], in_=gst[:], func=AF.Ln, bias=onec[0:64, 0:1], scale=1.0),
             [gst, onec], [gst])
        K.op("dve", lambda e: e.tensor_tensor(out=gv(gst), in0=gv(gst), in1=bc_mid(negA[:, :], NCH),
                                              op=ALU.mult), [gst, negA], [gst])
        K.op("act", lambda e: e.activation(out=gv(beta), in_=abbb[:, :, 4:8], func=AF.Sigmoid), [abbb], [beta])
        pG = nb()
        K.op("pe", lambda e: e.matmul(pG[0:64, 0:128], lhsT=cm2[0:64, :], rhs=gst[:], start=True, stop=True),
             [cm2, gst], [pG])
        K.op("pe", lambda e: e.matmul(pG[0:64, 128:256], lhsT=cmr2[0:64, :], rhs=gst[:], start=True, stop=True),
             [cmr2, gst], [pG])
        K.op("pe", lambda e: e.matmul(pG[:, 256:384], lhsT=ones[0:64, :], rhs=gst[:], start=True, stop=True),
             [ones, gst], [pG])
        K.op("act", lambda e: e.activation(out=eg[:], in_=pG[0:64, 0:128], func=AF.Exp), [pG], [eg])
        K.op("act", lambda e: e.activation(out=egr[:], in_=pG[0:64, 128:256], func=AF.Exp), [pG], [egr])
        K.op("act", lambda e: e.activation(out=egl[:], in_=pG[:, 256:384], func=AF.Exp), [pG], [egl])
        dump("gst", gst[:], [gst], [64, NCH * 4])
        dump("eg", eg[:], [eg], [64, NCH * 4])

        with K.scope() as sc:
            acc = [K.sb(sc, "cacc", [128, T], F32) for _ in range(2)]
            for cc in range(12):
                a = acc[cc % 2]
                K.op("dve", lambda e: e.tensor_scalar(out=a[:, :], in0=cvT[:, cc, :], scalar1=cw[:, cc, 3:4],
                                                      scalar2=None, op0=ALU.mult), [cvT, cw], [a])
                for sh in (1, 2, 3):
                    K.op("dve", lambda e: e.scalar_tensor_tensor(
                        out=a[:, sh:T], in0=cvT[:, cc, 0:T - sh], scalar=cw[:, cc, 3 - sh:4 - sh],
                        in1=a[:, sh:T], op0=ALU.mult, op1=ALU.add), [cvT, cw, a], [a])
                K.op("act", lambda e: e.activation(out=cvT[:, cc, :], in_=a[:, :], func=AF.Silu), [a], [cvT])
        dump("qkv_conv", cvT[:, :, 0:256], [cvT], [128, 12, 256], BF16)
        if stop_after == "gdn_pre":
            return

        S32 = K.sb(sg, "S32", [128, 512], F32)
        Sb = K.sb(sg, "Sb", [128, 512], BF16)
        K.op("dve", lambda e: e.memset(S32[:], 0.0), [], [S32])
        K.op("dve", lambda e: e.memset(Sb[:], 0.0), [], [Sb])
        ncm = K.sb(sg, "ncm", [64, 64], F32)
        K.op("dve", lambda e: e.tensor_scalar(out=ncm[:], in0=cm2[0:64, :], scalar1=-1.0, scalar2=None,
                                              op0=ALU.mult), [cm2], [ncm])
        idb = identb[0:64, 0:64]
        W2 = []
        for par in range(1):
            w = {}
            w["qk32"] = K.sb(sg, "qk32", [64, 8, 128], F32)
            w["sq"] = K.sb(sg, "sq", [64, 8, 128], F32)
            w["ss"] = K.sb(sg, "ss", [64, 8], F32)
            w["rn"] = K.sb(sg, "rn", [64, 8], F32)
            w["sc"] = K.sb(sg, "sc", [64, 6, 4], F32)
            for nm in ("qh", "kh", "kb", "kbg", "kd", "qg", "vb"):
                w[nm] = K.sb(sg, nm, [64, 4, 128], BF16)
            w["fT"] = K.sb(sg, "fT", [128, 16, 64], BF16)
            w["Gb"] = K.sb(sg, "Gb", [64, 4, 64], F32)
            w["Dn"] = K.sb(sg, "Dn", [64, 256], F32)
            w["Dp"] = K.sb(sg, "Dp", [64, 256], F32)
            w["E"] = K.sb(sg, "E", [64, 256], F32)
            w["Et"] = K.sb(sg, "Et", [64, 256], F32)
            w["EL"] = K.sb(sg, "EL", [64, 4, 64], F32)
            w["ELt"] = K.sb(sg, "ELt", [64, 4, 64], F32)
            w["EAt"] = K.sb(sg, "EAt", [64, 4, 64], F32)
            w["M"] = [K.sb(sg, "M", [64, 4, 64], BF16) for _ in range(2)]
            w["Mt"] = [K.sb(sg, "Mt", [64, 4, 64], BF16) for _ in range(2)]
            w["Atm"] = K.sb(sg, "Atm", [64, 4, 64], BF16)
            w["Pt32"] = K.sb(sg, "Pt32", [64, 4, 64], F32)
            w["Ptb"] = K.sb(sg, "Ptb", [64, 4, 64], BF16)
            w["negwT"] = K.sb(sg, "negwT", [128, 4, 64], BF16)
            w["vnew"] = K.sb(sg, "vnew", [64, 512], BF16)
            w["o32"] = K.sb(sg, "o32", [64, 4, 128], F32)
            w["osq"] = K.sb(sg, "osq", [64, 4, 128], F32)
            w["os"] = K.sb(sg, "os", [64, 8], F32)
            w["ob"] = K.sb(sg, "ob", [64, 512], BF16)
            W2.append(w)

        import os as _os
        if _os.environ.get('CH_LIMIT'):
            K.limit = int(_os.environ['CH_LIMIT'])
        for ch in range(int(_os.environ.get('GDN_NCH', NCH))):
            w = W2[0]
            c0, c1 = ch * 64, (ch + 1) * 64
            g0, g1 = ch * 4, ch * 4 + 4
            qk32, sq, ss_, rn, sc = w["qk32"], w["sq"], w["ss"], w["rn"], w["sc"]
            pa = nb(); pb = nb()
            for cc in range(8):
                K.op("pe", lambda e: e.transpose(out=bfv(pa)[0:64, cc * 128:(cc + 1) * 128],
                                                 in_=cvT[:, cc, c0:c1], identity=identb[:]), [cvT, identb], [pa])
            for cc in range(4):
                K.op("pe", lambda e: e.transpose(out=bfv(pb)[0:64, cc * 128:(cc + 1) * 128],
                                                 in_=cvT[:, 8 + cc, c0:c1], identity=identb[:]), [cvT, identb], [pb])
            K.op("act", lambda e: e.copy(out=qk32[:].rearrange("p a b -> p (a b)"), in_=bfv(pa)[0:64, :]),
                 [pa], [qk32])
            K.op("dve", lambda e: e.tensor_tensor(out=sq[:], in0=qk32[:], in1=qk32[:], op=ALU.mult), [qk32], [sq])
            K.op("dve", lambda e: e.tensor_reduce(out=ss_[:], in_=sq[:], axis=AX.X, op=ALU.add), [sq], [ss_])
            K.op("act", lambda e: e.activation(out=rn[:], in_=ss_[:], func=AF.Sqrt, bias=epsc[0:64, 0:1], scale=1.0),
                 [ss_, epsc], [rn])
            K.op("dve", lambda e: e.reciprocal(out=rn[:], in_=rn[:]), [rn], [rn])
            K.op("dve", lambda e: e.tensor_scalar(out=sc[:, 0, :], in0=rn[:, 0:4], scalar1=128.0 ** -0.5,
                                                  scalar2=None, op0=ALU.mult), [rn], [sc])
            K.op("dve", lambda e: e.tensor_tensor(out=sc[:, 1, :], in0=rn[:, 4:8], in1=beta[:, g0:g1],
                                                  op=ALU.mult), [rn, beta], [sc])
            K.op("dve", lambda e: e.tensor_tensor(out=sc[:, 2, :], in0=sc[:, 1, :], in1=eg[:, g0:g1],
                                                  op=ALU.mult), [sc, eg], [sc])
            K.op("dve", lambda e: e.tensor_tensor(out=sc[:, 3, :], in0=rn[:, 4:8], in1=egr[:, g0:g1],
                                                  op=ALU.mult), [rn, egr], [sc])
            K.op("dve", lambda e: e.tensor_tensor(out=sc[:, 4, :], in0=sc[:, 0, :], in1=eg[:, g0:g1],
                                                  op=ALU.mult), [sc, eg], [sc])
            q32 = qk32[:, 0:4, :]
            k32 = qk32[:, 4:8, :]
            plan = (("qh", q32, sc[:, 0, :], "dve"), ("kh", k32, rn[:, 4:8], "dve"),
                    ("kb", k32, sc[:, 1, :], "dve"), ("kbg", k32, sc[:, 2, :], "dve"),
                    ("kd", k32, sc[:, 3, :], "dve"), ("qg", q32, sc[:, 4, :], "dve"))
            for nm, src, scl, eng in plan:
                K.op(eng, lambda e: e.tensor_tensor(out=w[nm][:], in0=src, in1=bc_last(scl, 128), op=ALU.mult),
                     [qk32, sc, rn], [w[nm]])
            K.op("dve", lambda e: e.tensor_tensor(
                out=w["vb"][:], in0=bfv(pb)[0:64, 0:512].rearrange("p (h d) -> p h d", h=4),
                in1=bc_last(beta[:, g0:g1], 128), op=ALU.mult), [pb, beta], [w["vb"]])
            pc = nb()
            for ki, nm in enumerate(("kh", "kb", "qh", "qg")):
                for h in range(4):
                    K.op("pe", lambda e: e.transpose(out=bfv(pc)[:, (ki * 4 + h) * 64:(ki * 4 + h + 1) * 64],
                                                     in_=w[nm][:, h, :], identity=idb), [w[nm], identb], [pc])
            fT = w["fT"]
            K.op("act", lambda e: e.copy(out=fT[:].rearrange("p a b -> p (a b)"), in_=bfv(pc)[:, :]), [pc], [fT])
            pd = nb(); pe_ = nb()
            for h in range(4):
                K.op("pe", lambda e: e.matmul(pd[0:64, h * 64:(h + 1) * 64], lhsT=fT[:, 4 + h, :], rhs=fT[:, h, :],
                                              start=True, stop=True), [fT], [pd])
                K.op("pe", lambda e: e.matmul(pd[0:64, 256 + h * 64:256 + (h + 1) * 64], lhsT=fT[:, h, :],
                                              rhs=fT[:, 4 + h, :], start=True, stop=True), [fT], [pd])
                K.op("pe", lambda e: e.matmul(pe_[0:64, h * 64:(h + 1) * 64], lhsT=fT[:, h, :], rhs=fT[:, 8 + h, :],
                                              start=True, stop=True), [fT], [pe_])
            Gb = w["Gb"]
            K.op("dve", lambda e: e.tensor_copy(out=Gb[:], in_=bc_last(gst[:, g0:g1], 64)), [gst], [Gb])
            for h in range(4):
                K.op("pe", lambda e: e.matmul(pe_[0:64, 256 + h * 64:256 + (h + 1) * 64], lhsT=cm2[0:64, :],
                                              rhs=Gb[:, h, :], start=True, stop=False), [cm2, Gb], [pe_])
                K.op("pe", lambda e: e.matmul(pe_[0:64, 256 + h * 64:256 + (h + 1) * 64], lhsT=Gb[:, h, :],
                                              rhs=ncm[:], start=False, stop=True), [ncm, Gb], [pe_])
            Dn, Dp, E, Et = w["Dn"], w["Dp"], w["E"], w["Et"]
            K.op("dve", lambda e: e.tensor_scalar(out=Dn[:], in0=pe_[0:64, 256:512], scalar1=0.0, scalar2=None,
                                                  op0=ALU.min), [pe_], [Dn])
            K.op("dve", lambda e: e.tensor_scalar(out=Dp[:], in0=pe_[0:64, 256:512], scalar1=0.0, scalar2=None,
                                                  op0=ALU.max), [pe_], [Dp])
            K.op("act", lambda e: e.activation(out=E[:], in_=Dn[:], func=AF.Exp), [Dn], [E])
            K.op("act", lambda e: e.activation(out=Et[:], in_=Dp[:], func=AF.Exp, scale=-1.0), [Dp], [Et])
            v4 = lambda t: t[:].rearrange("p (h s) -> p h s", h=4)
            EL, ELt, EAt = w["EL"], w["ELt"], w["EAt"]
            K.op("dve", lambda e: e.tensor_tensor(out=EL[:], in0=v4(E), in1=bc_mid(m_lt[:, :], 4), op=ALU.mult),
                 [E, m_lt], [EL])
            K.op("dve", lambda e: e.tensor_tensor(out=ELt[:], in0=v4(Et), in1=bc_mid(m_gt[:, :], 4), op=ALU.mult),
                 [Et, m_gt], [ELt])
            K.op("dve", lambda e: e.tensor_tensor(out=EAt[:], in0=v4(Et), in1=bc_mid(m_ge[:, :], 4), op=ALU.mult),
                 [Et, m_ge], [EAt])
            M, Mt = w["M"][0], w["Mt"][0]
            Atm, Pt32, Ptb = w["Atm"], w["Pt32"], w["Ptb"]
            pv4 = lambda p, o: p[0:64, o:o + 256].rearrange("p (h s) -> p h s", h=4)
            K.op("dve", lambda e: e.tensor_tensor(out=M[:], in0=pv4(pd, 0), in1=EL[:], op=ALU.mult), [pd, EL], [M])
            K.op("dve", lambda e: e.tensor_tensor(out=Mt[:], in0=pv4(pd, 256), in1=ELt[:], op=ALU.mult),
                 [pd, ELt], [Mt])
            K.op("dve", lambda e: e.tensor_tensor(out=Atm[:], in0=pv4(pe_, 0), in1=EAt[:], op=ALU.mult),
                 [pe_, EAt], [Atm])
            K.op("dve", lambda e: e.tensor_tensor(out=Pt32[:], in0=bc_mid(ident[0:64, 0:64], 4), in1=Mt[:],
                                                   op=ALU.subtract), [ident, Mt], [Pt32])
            K.op("dve", lambda e: e.tensor_copy(out=Ptb[:], in_=Pt32[:]), [Pt32], [Ptb])
            for lev in range(5):
                Mn, Mtn = w["M"][(lev + 1) % 2], w["Mt"][(lev + 1) % 2]
                p1 = nb()
                for h in range(4):
                    K.op("pe", lambda e: e.matmul(p1[0:64, h * 64:(h + 1) * 64], lhsT=Mt[:, h, :], rhs=M[:, h, :],
                                                  start=True, stop=True), [M, Mt], [p1])
                if lev < 4:
                    for h in range(4):
                        K.op("pe", lambda e: e.matmul(p1[0:64, 256 + h * 64:256 + (h + 1) * 64], lhsT=M[:, h, :],
                                                      rhs=Mt[:, h, :], start=True, stop=True), [M, Mt], [p1])
                K.op("act", lambda e: e.copy(out=Mn[:], in_=pv4(p1, 0)), [p1], [Mn])
                if lev < 4:
                    K.op("dve", lambda e: e.tensor_copy(out=Mtn[:], in_=pv4(p1, 256)), [p1], [Mtn])
                p2 = nb()
                for h in range(4):
                    K.op("pe", lambda e: e.matmul(p2[0:64, h * 64:(h + 1) * 64], lhsT=Mn[:, h, :], rhs=Ptb[:, h, :],
                                                  start=True, stop=True), [Mn, Ptb], [p2])
                K.op("dve", lambda e: e.tensor_tensor(out=Pt32[:], in0=pv4(p2, 0), in1=Pt32[:], op=ALU.add),
                     [p2, Pt32], [Pt32])
                K.op("dve", lambda e: e.tensor_copy(out=Ptb[:], in_=Pt32[:]), [Pt32], [Ptb])
                M, Mt = Mn, Mtn
            if ch < 2:
                dump("Pt%d" % ch, Pt32[:], [Pt32], [64, 4, 64])
            negwT, vnew = w["negwT"], w["vnew"]
            p3 = nb()
            for h in range(4):
                K.op("pe", lambda e: e.matmul(p3[:, h * 64:(h + 1) * 64], lhsT=w["kbg"][:, h, :], rhs=Ptb[:, h, :],
                                              start=True, stop=True), [w["kbg"], Ptb], [p3])
            K.op("act", lambda e: e.activation(out=negwT[:].rearrange("p h c -> p (h c)"), in_=p3[:, 0:256],
                                               func=AF.Copy, scale=-1.0), [p3], [negwT])
            p4 = nb()
            for h in range(4):
                K.op("pe", lambda e: e.matmul(p4[0:64, h * 128:(h + 1) * 128], lhsT=Ptb[:, h, :], rhs=w["vb"][:, h, :],
                                              start=True, stop=False), [Ptb, w["vb"]], [p4])
                K.op("pe", lambda e: e.matmul(p4[0:64, h * 128:(h + 1) * 128], lhsT=negwT[:, h, :],
                                              rhs=Sb[:, h * 128:(h + 1) * 128], start=False, stop=True),
                     [negwT, Sb], [p4])
            K.op("act", lambda e: e.copy(out=vnew[:], in_=p4[0:64, :]), [p4], [vnew])
            p5 = nb()
            for h in range(4):
                K.op("pe", lambda e: e.matmul(p5[0:64, h * 128:(h + 1) * 128], lhsT=fT[:, 12 + h, :],
                                              rhs=Sb[:, h * 128:(h + 1) * 128], start=True, stop=False),
                     [fT, Sb], [p5])
                K.op("pe", lambda e: e.matmul(p5[0:64, h * 128:(h + 1) * 128], lhsT=Atm[:, h, :],
                                              rhs=vnew[:, h * 128:(h + 1) * 128], start=False, stop=True),
                     [Atm, vnew], [p5])
            p6 = nb()
            for h in range(4):
                K.op("pe", lambda e: e.matmul(p6[:, h * 128:(h + 1) * 128], lhsT=w["kd"][:, h, :],
                                              rhs=vnew[:, h * 128:(h + 1) * 128], start=True, stop=True),
                     [w["kd"], vnew], [p6])
            S4 = S32[:].rearrange("p (h d) -> p h d", h=4)
            K.op("dve", lambda e: e.tensor_tensor(out=S4, in0=S4, in1=bc_last(egl[:, g0:g1], 128), op=ALU.mult),
                 [S32, egl], [S32])
            K.op("dve", lambda e: e.tensor_tensor(out=S32[:], in0=p6[:, :], in1=S32[:], op=ALU.add), [p6, S32], [S32])
            K.op("act", lambda e: e.copy(out=Sb[:], in_=S32[:]), [S32], [Sb])
            o32, osq, os_, ob = w["o32"], w["osq"], w["os"], w["ob"]
            K.op("act", lambda e: e.copy(out=o32[:].rearrange("p h d -> p (h d)"), in_=p5[0:64, :]), [p5], [o32])
            K.op("dve", lambda e: e.tensor_tensor(out=osq[:], in0=o32[:], in1=o32[:], op=ALU.mult), [o32], [osq])
            K.op("dve", lambda e: e.tensor_reduce(out=os_[:, 0:4], in_=osq[:], axis=AX.X, op=ALU.add), [osq], [os_])
            K.op("act", lambda e: e.activation(out=os_[:, 4:8], in_=os_[:, 0:4], func=AF.Sqrt, bias=epsc[0:64, 0:1],
                                               scale=1.0 / 128), [os_, epsc], [os_])
            K.op("dve", lambda e: e.reciprocal(out=os_[:, 4:8], in_=os_[:, 4:8]), [os_], [os_])
            K.op("dve", lambda e: e.tensor_tensor(out=o32[:], in0=o32[:], in1=bc_last(os_[:, 4:8], 128), op=ALU.mult),
                 [o32, os_], [o32])
            K.op("dve", lambda e: e.tensor_tensor(out=o32[:], in0=o32[:], in1=bc_mid(onB[:, :], 4), op=ALU.mult),
                 [o32, onB], [o32])
            K.op("dve", lambda e: e.tensor_tensor(out=ob[:], in0=o32[:].rearrange("p h d -> p (h d)"),
                                                  in1=sz[:, ch, :], op=ALU.mult), [o32, sz], [ob])
            p7 = nb()
            for h in range(4):
                K.op("pe", lambda e: e.transpose(out=bfv(p7)[:, h * 64:(h + 1) * 64], in_=ob[:, h * 128:(h + 1) * 128],
                                                 identity=idb), [ob, identb], [p7])
            K.op("act", lambda e: e.copy(out=mixT[:, 4:8, c0:c1],
                                         in_=bfv(p7)[:, 0:256].rearrange("p (h t) -> p h t", h=4)), [p7], [mixT])
        dump("ob", mixT[:, 4:8, 0:256], [mixT], [128, 4, 256], BF16)


def _dsa(nc, K, s1, g, dbg, stop_after, dump, seq, L, xT, mixT):
    PS = L["PS"]
    ident = L["ident"]; identb = L["identb"]; epsc = L["epsc"]
    negdiag = L["negdiag"]; kreq = L["kreq"]; pow2 = L["pow2"]; cnB = L["cnB"]
    wukT = L["wukT"]; wuvb = L["wuvb"]; Tb = L["Tb"]; b15B = L["b15B"]
    winb = g["winb"]
    rr = [0]

    def nb(lo=0, n=6):
        rr[0] += 1
        return PS[lo + rr[0] % n]

    def bfv(p):
        return p.t[:].bitcast(BF16)

    with K.scope() as sd:
        qabsT = K.sb(sd, "qabsT", [128, 8, T], BF16)
        cT = K.sb(sd, "cT", [128, T], BF16)
        Vaug = K.sb(sd, "Vaug", [128, NT, 8, 65], BF16)
        qiT = K.sb(sd, "qiT", [128, 4, T], BF16)
        kiT2 = K.sb(sd, "kiT2", [128, T], BF16)
        wi = K.sb(sd, "wi", [128, NT, 8], F32)
        K.op("pool", lambda e: e.memset(Vaug[:, :, :, 64:65], 1.0), [], [Vaug])
        with K.scope() as sw:
            wd = K.sb(sw, "wd", [128, 8, 1224], BF16)
            K.dma("sp", wd[:], winb[:, 0:1224].rearrange("(k p) c -> p k c", p=128), [], [wd])
            wki2 = K.sb(sw, "wki2", [128, 8, 128], BF16)
            K.op("dve", lambda e: e.tensor_copy(out=wki2[:, :, 0:64], in_=wd[:, :, O_KI:O_KI + 64]), [wd], [wki2])
            K.op("pool", lambda e: e.tensor_copy(out=wki2[:, :, 64:128], in_=wd[:, :, O_KI:O_KI + 64]), [wd], [wki2])
            qaT = K.sb(sw, "qaT", [128, 4, T], BF16)
            ckv = K.sb(sw, "ckv", [128, NT, 128], F32)
            cb = K.sb(sw, "cb", [128, 128], BF16)
            cst_ = K.sb(sw, "cst", [128, 4], F32)
            cjk = K.sb(sw, "cjk", [128, 128], BF16)
            ev = 0
            for tb in range(4):
                tsl = slice(tb * 512, (tb + 1) * 512)
                jobs = [(wd, j * 128, qaT, j) for j in range(4)] + [(wd, O_QI + j * 128, qiT, j) for j in range(4)]
                for wsrc, c0, dst, j in jobs:
                    p = nb()
                    for kc in range(8):
                        K.op("pe", lambda e: e.matmul(p[:, :], lhsT=wsrc[:, kc, c0:c0 + 128], rhs=xT[:, kc, tsl],
                                                      start=(kc == 0), stop=(kc == 7)), [wsrc, xT], [p])
                    if ev % 2:
                        K.op("act", lambda e: e.copy(out=dst[:, j, tsl], in_=p[:, :]), [p], [dst])
                    else:
                        K.op("dve", lambda e: e.tensor_copy(out=dst[:, j, tsl], in_=p[:, :]), [p], [dst])
                    ev += 1
                p = nb()
                for kc in range(8):
                    K.op("pe", lambda e: e.matmul(p[:, :], lhsT=wki2[:, kc, :], rhs=xT[:, kc, tsl],
                                                  start=(kc == 0), stop=(kc == 7)), [wki2, xT], [p])
                K.op("act", lambda e: e.copy(out=kiT2[:, tsl], in_=p[:, :]), [p], [kiT2])
            for i in range(NT):
                isl = slice(i * 128, (i + 1) * 128)
                p = nb()
                for kc in range(8):
                    K.op("pe", lambda e: e.matmul(p[:, 0:128], lhsT=xT[:, kc, isl], rhs=wd[:, kc, O_CKV:O_CKV + 128],
                                                  start=(kc == 0), stop=(kc == 7)), [wd, xT], [p])
                for kc in range(8):
                    K.op("pe", lambda e: e.matmul(p[:, 128:136], lhsT=xT[:, kc, isl], rhs=wd[:, kc, O_WI:O_WI + 8],
                                                  start=(kc == 0), stop=(kc == 7)), [wd, xT], [p])
                K.op("dve", lambda e: e.tensor_copy(out=ckv[:, i, :], in_=p[:, 0:128]), [p], [ckv])
                K.op("dve", lambda e: e.tensor_copy(out=wi[:, i, :], in_=p[:, 128:136]), [p], [wi])
                K.op("act", lambda e: e.activation(out=cjk[:], in_=ckv[:, i, :], func=AF.Square,
                                                   accum_out=cst_[:, 0:1]), [ckv], [cjk, cst_])
                K.op("act", lambda e: e.activation(out=cst_[:, 1:2], in_=cst_[:, 0:1], func=AF.Sqrt,
                                                   bias=epsc[:, 0:1], scale=1.0 / 128), [cst_, epsc], [cst_])
                K.op("dve", lambda e: e.reciprocal(out=cst_[:, 1:2], in_=cst_[:, 1:2]), [cst_], [cst_])
                K.op("dve", lambda e: e.scalar_tensor_tensor(out=cb[:], in0=ckv[:, i, :], scalar=cst_[:, 1:2],
                                                             in1=cnB[:], op0=ALU.mult, op1=ALU.mult),
                     [ckv, cst_, cnB], [cb])
                p2 = nb()
                K.op("pe", lambda e: e.transpose(out=bfv(p2)[:, 0:128], in_=cb[:], identity=identb[:]),
                     [cb, identb], [p2])
                K.op("act", lambda e: e.copy(out=cT[:, isl], in_=bfv(p2)[:, 0:128]), [p2], [cT])
                p3 = nb()
                K.op("pe", lambda e: e.matmul(p3[:, :], lhsT=cT[:, isl], rhs=wuvb[:], start=True, stop=True),
                     [cT, wuvb], [p3])
                K.op("dve", lambda e: e.tensor_copy(out=Vaug[:, i, :, 0:64],
                                                    in_=p3[:, :].rearrange("p (h d) -> p h d", h=8)), [p3], [Vaug])
            for tb in range(4):
                tsl = slice(tb * 512, (tb + 1) * 512)
                for h in range(8):
                    j, hf = h // 2, h % 2
                    psl = slice(hf * 64, (hf + 1) * 64)
                    p = nb()
                    K.op("pe", lambda e: e.matmul(p[:, :], lhsT=wukT[psl, j, :], rhs=qaT[psl, j, tsl],
                                                  start=True, stop=True), [wukT, qaT], [p])
                    K.op("act", lambda e: e.activation(out=qabsT[:, h, tsl], in_=p[:, :], func=AF.Copy, scale=0.125),
                         [p], [qabsT])
        dump("cT", cT[:, 0:256], [cT], [128, 256], BF16)
        dump("qabsT", qabsT[:, :, 0:128], [qabsT], [128, 8, 128], BF16)

        with K.scope() as sq:
            score = K.sb(sq, "score", [128, T], F32)
            relu = [K.sb(sq, "relu", [128, 512], BF16) for _ in range(4)]
            diagw = K.sb(sq, "diagw", [128, 8, 128], BF16)
            cjunk = K.sb(sq, "cjunk", [128, T], BF16)
            bs = K.sb(sq, "bs", [128, 8], F32)
            wtab = K.sb(sq, "wtab", [128, NBIS], F32)
            sel = K.sb(sq, "sel", [128, T], BF16)
            negselT = K.sb(sq, "negselT", [128, NT, 128], BF16)
            Pm = [K.sb(sq, "Pm", [128, 512], BF16) for _ in range(3)]
            oa = K.sb(sq, "oa", [128, 8, 65], F32)
            rden = K.sb(sq, "rden", [128, 8], F32)
            oab = K.sb(sq, "oab", [128, 8, 64], BF16)
            WSC = (8.0 ** -0.5) * (64.0 ** -0.5)
            pmi = 0
            import os as _os
            for qt in range(int(_os.environ.get("DSA_NQT", NT))):
                qsl = slice(qt * 128, (qt + 1) * 128)
                nkb = qt + 1
                nk = nkb * 128
                for h in range(8):
                    K.op("dve", lambda e: e.tensor_scalar(
                        out=diagw[:, h, :], in0=ident[:], scalar1=wi[:, qt, h:h + 1], scalar2=WSC,
                        op0=ALU.mult, op1=ALU.mult), [ident, wi], [diagw])
                for g4 in range((nkb + 3) // 4):
                    s0 = g4 * 512
                    sn = min(512, nk - s0)
                    psc = PS[6 + g4 % 2]
                    phs = {}

                    def qk(h):
                        j, hf = h // 2, h % 2
                        psl = slice(hf * 64, (hf + 1) * 64)
                        ph = nb()
                        phs[h] = ph
                        K.op("pe", lambda e: e.matmul(ph[:, 0:sn], lhsT=qiT[psl, j, qsl], rhs=kiT2[psl, s0:s0 + sn],
                                                      start=True, stop=True), [qiT, kiT2], [ph])
                    for h in range(3):
                        qk(h)
                    for h in range(8):
                        if h + 3 < 8:
                            qk(h + 3)
                        r = relu[h % 4]
                        ph = phs[h]
                        if h % 2:
                            K.op("act", lambda e: e.activation(out=r[:, 0:sn], in_=ph[:, 0:sn], func=AF.Relu), [ph], [r])
                        else:
                            K.op("dve", lambda e: e.tensor_scalar(out=r[:, 0:sn], in0=ph[:, 0:sn], scalar1=0.0,
                                                                  scalar2=None, op0=ALU.max), [ph], [r])
                        K.op("pe", lambda e: e.matmul(psc[:, 0:sn], lhsT=diagw[:, h, :], rhs=r[:, 0:sn],
                                                      start=(h == 0), stop=(h == 7)), [diagw, r], [psc])
                    K.op("act", lambda e: e.copy(out=score[:, s0:s0 + sn], in_=psc[:, 0:sn]), [psc], [score])
                K.op("dve", lambda e: e.tensor_reduce(out=bs[:, 0:1], in_=score[:, 0:nk], axis=AX.X, op=ALU.max,
                                                      apply_absolute_value=True), [score], [bs])
                K.op("dve", lambda e: e.tensor_scalar(out=bs[:, 0:1], in0=bs[:, 0:1], scalar1=1.0001, scalar2=1e-6,
                                                      op0=ALU.mult, op1=ALU.add), [bs], [bs])
                K.op("dve", lambda e: e.tensor_tensor(out=score[:, nk - 128:nk], in0=score[:, nk - 128:nk],
                                                      in1=negdiag[:], op=ALU.add), [score, negdiag], [score])
                K.op("dve", lambda e: e.tensor_scalar(out=wtab[:], in0=pow2[:], scalar1=bs[:, 0:1], scalar2=None,
                                                      op0=ALU.mult), [pow2, bs], [wtab])
                K.op("dve", lambda e: e.memset(bs[:, 1:2], 0.0), [], [bs])
                for it in range(NBIS):
                    K.op("dve", lambda e: e.tensor_scalar(out=cjunk[:, 0:nk], in0=score[:, 0:nk], scalar1=bs[:, 1:2],
                                                          scalar2=0.0, op0=ALU.is_ge, op1=ALU.add,
                                                          accum_out=bs[:, 2:3]), [score, bs], [cjunk, bs])
                    K.op("dve", lambda e: e.tensor_scalar(out=bs[:, 3:4], in0=bs[:, 2:3], scalar1=kreq[:, qt:qt + 1],
                                                          scalar2=0.5, op0=ALU.is_ge, op1=ALU.subtract),
                         [bs, kreq], [bs])
                    K.op("dve", lambda e: e.scalar_tensor_tensor(out=bs[:, 1:2], in0=bs[:, 3:4],
                                                                 scalar=wtab[:, it:it + 1], in1=bs[:, 1:2],
                                                                 op0=ALU.mult, op1=ALU.add), [bs, wtab], [bs])
                K.op("dve", lambda e: e.scalar_tensor_tensor(out=bs[:, 4:5], in0=wtab[:, NBIS - 1:NBIS], scalar=-0.5,
                                                             in1=bs[:, 1:2], op0=ALU.mult, op1=ALU.add),
                     [bs, wtab], [bs])
                K.op("dve", lambda e: e.tensor_scalar(out=sel[:, 0:nk], in0=score[:, 0:nk], scalar1=bs[:, 4:5],
                                                      scalar2=None, op0=ALU.is_ge), [score, bs], [sel])
                if qt == 3:
                    dump("sel3", sel[:, 0:512], [sel], [128, 512], BF16)
                    dump("score3", score[:, 0:512], [score], [128, 512])
                for k0 in range(0, nkb, 8):
                    kn = min(8, nkb - k0)
                    pT = nb()
                    for kk in range(kn):
                        K.op("pe", lambda e: e.transpose(out=bfv(pT)[:, kk * 128:(kk + 1) * 128],
                                                         in_=sel[:, (k0 + kk) * 128:(k0 + kk + 1) * 128],
                                                         identity=identb[:]), [sel, identb], [pT])
                    K.op("dve", lambda e: e.tensor_scalar(
                        out=negselT[:, k0:k0 + kn, :].rearrange("p k t -> p (k t)"), in0=bfv(pT)[:, 0:kn * 128],
                        scalar1=-1.0, scalar2=-NEG, op0=ALU.add, op1=ALU.mult), [pT], [negselT])
                poA, poB = PS[6], PS[7]
                for h in range(8):
                    po = poA if h < 4 else poB
                    hh = h % 4
                    for g4 in range((nkb + 3) // 4):
                        kbs = list(range(g4 * 4, min(nkb, g4 * 4 + 4)))
                        n = len(kbs)
                        ps_ = nb()
                        for idx, kb in enumerate(kbs):
                            reg = ps_[:, idx * 128:(idx + 1) * 128]
                            near = kb >= qt - 1
                            K.op("pe", lambda e: e.matmul(reg, lhsT=cT[:, kb * 128:(kb + 1) * 128], rhs=qabsT[:, h, qsl],
                                                          start=True, stop=False), [cT, qabsT], [ps_])
                            K.op("pe", lambda e: e.matmul(reg, lhsT=identb[:], rhs=negselT[:, kb, :],
                                                          start=False, stop=(not near)), [identb, negselT], [ps_])
                            if near:
                                u0 = 0 if kb == qt else 128
                                K.op("pe", lambda e: e.matmul(reg, lhsT=identb[:], rhs=Tb[:, h, u0:u0 + 128],
                                                              start=False, stop=True), [identb, Tb], [ps_])
                        pm = Pm[pmi % 3]
                        pmi += 1
                        K.op("act", lambda e: e.activation(out=pm[:, 0:n * 128], in_=ps_[:, 0:n * 128], func=AF.Exp,
                                                           bias=b15B[:, h:h + 1], scale=1.0), [ps_, b15B], [pm])
                        for idx, kb in enumerate(kbs):
                            K.op("pe", lambda e: e.matmul(po[:, hh * 65:(hh + 1) * 65],
                                                          lhsT=pm[:, idx * 128:(idx + 1) * 128], rhs=Vaug[:, kb, h, :],
                                                          start=(kb == 0), stop=(kb == nkb - 1)), [pm, Vaug], [po])
                K.op("act", lambda e: e.copy(out=oa[:, 0:4, :].rearrange("p h d -> p (h d)"), in_=poA[:, 0:260]),
                     [poA], [oa])
                K.op("dve", lambda e: e.tensor_copy(out=oa[:, 4:8, :].rearrange("p h d -> p (h d)"), in_=poB[:, 0:260]),
                     [poB], [oa])
                K.op("dve", lambda e: e.reciprocal(out=rden[:], in_=oa[:, :, 64]), [oa], [rden])
                K.op("dve", lambda e: e.tensor_tensor(out=oab[:], in0=oa[:, :, 0:64], in1=bc_last(rden[:, :], 64),
                                                      op=ALU.mult), [oa, rden], [oab])
                pX = nb()
                for j in range(4):
                    K.op("pe", lambda e: e.transpose(out=bfv(pX)[:, j * 128:(j + 1) * 128],
                                                     in_=oab[:, 2 * j:2 * j + 2, :].rearrange("p h d -> p (h d)"),
                                                     identity=identb[:]), [oab, identb], [pX])
                K.op("act", lambda e: e.copy(out=mixT[:, 0:4, qsl],
                                             in_=bfv(pX)[:, 0:512].rearrange("p (k t) -> p k t", k=4)), [pX], [mixT])
        dump("oa", mixT[:, 0:4, 0:512], [mixT], [128, 4, 512], BF16)


def _mlp(nc, K, ss, g, dbg, stop_after, dump, seq, L, mixT):
    PS = L["PS"]
    identb = L["identb"]; epsc = L["epsc"]; g2B = L["g2B"]; g4B = L["g4B"]
    woutb, w1b, w2b = g["woutb"], g["w1b"], g["w2b"]
    x = g["x"]; out = g["out"]
    tok0 = seq * T

    def bfv(p):
        return p.t[:].bitcast(BF16)

    with K.scope() as sm:
        wo = K.sb(sm, "wo", [128, 8, D], BF16)
        K.dma("sp", wo[:], woutb.rearrange("(k p) c -> p k c", p=128), [], [wo])
        x1 = K.sb(sm, "x1", [128, 4, D], F32)
        xr = [K.sb(sm, "xr", [128, D], F32) for _ in range(2)]
        xnb = K.sb(sm, "xnb", [128, D], BF16)
        xnT = K.sb(sm, "xnT", [128, 8, 512], BF16)
        hT = K.sb(sm, "hT", [128, 32, 512], BF16)
        w1t = [K.sb(sm, "w1t", [128, 8, 512], BF16) for _ in range(2)]
        w2t = [K.sb(sm, "w2t", [128, 4, D], BF16) for _ in range(2)]
        r32 = [K.sb(sm, "r32", [128, 512], F32) for _ in range(2)]
        yt = K.sb(sm, "yt", [128, D], F32)
        junk = K.sb(sm, "mjunk", [128, 512], BF16)
        st = K.sb(sm, "mst", [128, 8], F32)

        def rms_from_psum(pA, pB, col):
            K.op("act", lambda e: e.activation(out=junk[:], in_=pA[:, :], func=AF.Square, accum_out=st[:, 0:1]),
                 [pA], [junk, st])
            K.op("act", lambda e: e.activation(out=junk[:], in_=pB[:, :], func=AF.Square, accum_out=st[:, 1:2]),
                 [pB], [junk, st])
            K.op("dve", lambda e: e.tensor_tensor(out=st[:, 2:3], in0=st[:, 0:1], in1=st[:, 1:2], op=ALU.add),
                 [st], [st])
            K.op("act", lambda e: e.activation(out=st[:, col:col + 1], in_=st[:, 2:3], func=AF.Sqrt,
                                               bias=epsc[:, 0:1], scale=1.0 / D), [st, epsc], [st])
            K.op("dve", lambda e: e.reciprocal(out=st[:, col:col + 1], in_=st[:, col:col + 1]), [st], [st])

        cnt = 0
        for tb in range(4):
            for ti in range(4):
                i = tb * 4 + ti
                xri = xr[i % 2]
                K.dma("sp", xri[:], x[tok0 + i * 128: tok0 + (i + 1) * 128, :], [], [xri])
                pA, pB = PS[2 * (ti % 2)], PS[2 * (ti % 2) + 1]
                for half, p in enumerate((pA, pB)):
                    for kc in range(8):
                        K.op("pe", lambda e: e.matmul(p[:, :], lhsT=mixT[:, kc, i * 128:(i + 1) * 128],
                                                      rhs=wo[:, kc, half * 512:(half + 1) * 512],
                                                      start=(kc == 0), stop=(kc == 7)), [mixT, wo], [p])
                rms_from_psum(pA, pB, 3)
                for half, p in enumerate((pA, pB)):
                    K.op("dve", lambda e: e.scalar_tensor_tensor(
                        out=x1[:, ti, half * 512:(half + 1) * 512], in0=p[:, :], scalar=st[:, 3:4],
                        in1=g2B[:, half * 512:(half + 1) * 512], op0=ALU.mult, op1=ALU.mult), [p, st, g2B], [x1])
                K.op("dve", lambda e: e.tensor_tensor(out=x1[:, ti, :], in0=x1[:, ti, :], in1=xri[:], op=ALU.add),
                     [x1, xri], [x1])
                K.op("act", lambda e: e.activation(out=xnb[:], in_=x1[:, ti, :], func=AF.Square,
                                                   accum_out=st[:, 4:5]), [x1], [xnb, st])
                K.op("act", lambda e: e.activation(out=st[:, 5:6], in_=st[:, 4:5], func=AF.Sqrt, bias=epsc[:, 0:1],
                                                   scale=1.0 / D), [st, epsc], [st])
                K.op("dve", lambda e: e.reciprocal(out=st[:, 5:6], in_=st[:, 5:6]), [st], [st])
                K.op("dve", lambda e: e.tensor_scalar(out=xnb[:], in0=x1[:, ti, :], scalar1=st[:, 5:6], scalar2=None,
                                                      op0=ALU.mult), [x1, st], [xnb])
                pt = PS[4 + (ti % 2)]
                for kc in range(8):
                    K.op("pe", lambda e: e.transpose(out=bfv(pt)[:, kc * 128:(kc + 1) * 128],
                                                     in_=xnb[:, kc * 128:(kc + 1) * 128], identity=identb[:]),
                         [xnb, identb], [pt])
                K.op("act", lambda e: e.copy(out=xnT[:, :, ti * 128:(ti + 1) * 128],
                                             in_=bfv(pt).rearrange("p (k t) -> p k t", k=8)), [pt], [xnT])
            for fb in range(8):
                wt = w1t[fb % 2]
                K.dma("sp", wt[:], w1b[:, fb * 512:(fb + 1) * 512].rearrange("(k p) c -> p k c", p=128), [], [wt])
                for fc in range(4):
                    p = PS[cnt % 8]
                    r = r32[cnt % 2]
                    for kc in range(8):
                        K.op("pe", lambda e: e.matmul(p[:, :], lhsT=wt[:, kc, fc * 128:(fc + 1) * 128],
                                                      rhs=xnT[:, kc, :], start=(kc == 0), stop=(kc == 7)),
                             [wt, xnT], [p])
                    K.op("act", lambda e: e.activation(out=r[:], in_=p[:, :], func=AF.Relu), [p], [r])
                    K.op("dve", lambda e: e.tensor_tensor(
                        out=hT[:, fb * 4 + fc, :], in0=r[:], in1=r[:], op=ALU.mult), [r], [hT])
                    cnt += 1
            for fg in range(8):
                wt2 = w2t[fg % 2]
                K.dma("sp", wt2[:], w2b[fg * 512:(fg + 1) * 512, :].rearrange("(c p) n -> p c n", p=128), [], [wt2])
                for c4 in range(4):
                    fc = fg * 4 + c4
                    for ti in range(4):
                        for half in range(2):
                            p = PS[ti * 2 + half]
                            K.op("pe", lambda e: e.matmul(p[:, :], lhsT=hT[:, fc, ti * 128:(ti + 1) * 128],
                                                          rhs=wt2[:, c4, half * 512:(half + 1) * 512],
                                                          start=(fc == 0), stop=(fc == 31)), [hT, wt2], [p])
            for ti in range(4):
                i = tb * 4 + ti
                pA, pB = PS[ti * 2], PS[ti * 2 + 1]
                rms_from_psum(pA, pB, 6)
                for half, p in enumerate((pA, pB)):
                    K.op("dve", lambda e: e.scalar_tensor_tensor(
                        out=yt[:, half * 512:(half + 1) * 512], in0=p[:, :], scalar=st[:, 6:7],
                        in1=g4B[:, half * 512:(half + 1) * 512], op0=ALU.mult, op1=ALU.mult), [p, st, g4B], [yt])
                K.op("dve", lambda e: e.tensor_tensor(out=yt[:], in0=yt[:], in1=x1[:, ti, :], op=ALU.add),
                     [yt, x1], [yt])
                K.dma("sp", out[tok0 + i * 128: tok0 + (i + 1) * 128, :], yt[:], [yt], [Dep()])


INPUT_NAMES = ["x", "w_in", "c_norm", "w_uk", "w_uv", "rel_bias", "conv_w", "a_log", "dt_bias", "o_norm",
               "w_out", "pre_norm_mix", "post_norm_mix", "pre_norm_mlp", "post_norm_mlp", "w_mlp_in", "w_mlp_out"]


def make_in_maps(inputs, n_cores=8):
    f = lambda a: np.ascontiguousarray(np.asarray(a, dtype=np.float32))
    shared = {
        "w_in": f(inputs["w_in"])[0], "c_norm": f(inputs["c_norm"])[0], "w_uk": f(inputs["w_uk"])[0],
        "w_uv": f(inputs["w_uv"])[0], "rel_bias": f(inputs["rel_bias"]), "conv_w": f(inputs["conv_w"])[0],
        "a_log": f(inputs["a_log"])[0], "dt_bias": f(inputs["dt_bias"])[0], "o_norm": f(inputs["o_norm"])[0],
        "w_out": f(inputs["w_out"])[0], "pre_norm_mix": f(inputs["pre_norm_mix"])[0],
        "post_norm_mix": f(inputs["post_norm_mix"])[0], "pre_norm_mlp": f(inputs["pre_norm_mlp"])[0],
        "post_norm_mlp": f(inputs["post_norm_mlp"])[0], "w_mlp_in": f(inputs["w_mlp_in"])[0],
        "w_mlp_out": f(inputs["w_mlp_out"])[0], "ohr": onehot_rev(),
    }
    xs = f(inputs["x"])
    maps = []
    for c in range(n_cores):
        m = dict(shared)
        m["x"] = np.ascontiguousarray(xs[c * NSEQ:(c + 1) * NSEQ].reshape(NSEQ * T, D))
        maps.append(m)
    return maps


def kernel(**inputs):
    nc = build_nc()
    maps = make_in_maps(inputs)
    res = run_bass_kernel_spmd(nc, maps, core_ids=list(range(8)))
    outs = [np.asarray(r["out"], dtype=np.float32).reshape(NSEQ, T, D) for r in res.results]
    return np.concatenate(outs, axis=0)
```

```python
import numpy as np
from contextlib import ExitStack
import concourse.bass as bass
import concourse.mybir as mybir
from concourse.bass_utils import run_bass_kernel_spmd

F32 = mybir.dt.float32
BF16 = mybir.dt.bfloat16
AF = mybir.ActivationFunctionType
ALU = mybir.AluOpType
AX = mybir.AxisListType

D = 1024
T = 2048
NSEQ = 2
NT = T // 128
INC = 3280
DFF = 4096
EPS = 1e-6
O_QA, O_CKV, O_QI, O_KI, O_WI, O_QB, O_KB, O_VB, O_AB, O_BB, O_ZB = (
    0, 512, 640, 1152, 1216, 1224, 1736, 2248, 2760, 2764, 2768)
NBIS = 16
TOPK = 256
NEG = -30000.0


class Dep:
    __slots__ = ("w", "r", "x")

    def __init__(self):
        self.w = None
        self.r = []
        self.x = False


class Tl:
    def __init__(self, t, nslots=0):
        self.t = t
        self.d = Dep()
        self.s = [Dep() for _ in range(nslots)]

    def __getitem__(self, idx):
        return self.t[idx]


class KB:
    def __init__(self, nc, es):
        self.nc = nc
        self.es = es
        self.engs = {"pe": nc.tensor, "dve": nc.vector, "act": nc.scalar,
                     "pool": nc.gpsimd, "sp": nc.sync}
        self.sems = {}
        self.cnt = {}
        for n in ("pe", "dve", "act", "pool"):
            self.sems[n] = es.enter_context(nc.semaphore("sem_" + n))
            self.cnt[n] = 0
        self.waited = {n: {} for n in self.engs}
        self.dq = {}
        for q, n in (("sp", 20), ("act", 8), ("pool", 8)):
            lst = []
            for i in range(n):
                nm = "dma_%s_%d" % (q, i)
                self.sems[nm] = es.enter_context(nc.semaphore(nm))
                self.cnt[nm] = 0
                lst.append(nm)
            self.dq[q] = [lst, 0]
        self.uid = 0
        self.fence = []
        self.limit = None

    def fence_now(self):
        self.fence = [(k, v) for k, v in self.cnt.items() if v > 0]

    def scope(self):
        kb = self

        class _Scope(ExitStack):
            def __exit__(self, *a):
                r = ExitStack.__exit__(self, *a)
                kb.fence_now()
                return r
        return _Scope()

    def sb(self, es, name, shape, dt, nslots=0):
        self.uid += 1
        t = Tl(es.enter_context(self.nc.sbuf_tensor("%s_%d" % (name, self.uid), shape, dt)), nslots)
        t.d.r = list(self.fence)
        for d in t.s:
            d.r = list(self.fence)
        return t

    def ps(self, es, name, shape, dt):
        self.uid += 1
        t = Tl(es.enter_context(self.nc.psum_tensor("%s_%d" % (name, self.uid), shape, dt)))
        t.d.x = True
        return t

    def _deps(self, eng, reads, writes):
        need = {}

        def add(p):
            if p is None:
                return
            s, v = p
            if need.get(s, 0) < v:
                need[s] = v

        for d in reads:
            add(d.w)
            if d.x:
                for p in d.r:
                    if p[0] != eng:
                        add(p)
        for d in writes:
            if d.w is not None and not (eng == "pe" and d.w[0] == "pe"):
                add(d.w)
            for p in d.r:
                add(p)
        e = self.engs[eng]
        wd = self.waited[eng]
        for s, v in need.items():
            if wd.get(s, 0) < v:
                e.wait_ge(self.sems[s], v)
                wd[s] = v

    def _norm(self, lst):
        out = []
        for x in lst:
            out.append(x.d if isinstance(x, Tl) else x)
        return out

    def _mark(self, me, reads, writes):
        for d in reads:
            d.r = [p for p in d.r if p[0] != me[0]]
            d.r.append(me)
        for d in writes:
            d.w = me
            d.r = []

    def op(self, eng, fn, reads=(), writes=()):
        if self.limit is not None:
            if self.limit <= 0:
                return None
            self.limit -= 1
            if self.limit == 0:
                import traceback
                print("LAST OP:", eng, traceback.extract_stack()[-2].lineno)
        reads = self._norm(reads)
        writes = self._norm(writes)
        self._deps(eng, reads, writes)
        inst = fn(self.engs[eng])
        self.cnt[eng] += 1
        inst.then_inc(self.sems[eng], 1)
        self._mark((eng, self.cnt[eng]), reads, writes)
        return inst

    def dma(self, q, out, in_, reads=(), writes=(), nc_ok=False):
        reads = self._norm(reads)
        writes = self._norm(writes)
        lst, i = self.dq[q]
        sn = lst[i % len(lst)]
        self.dq[q][1] = i + 1
        e = self.engs[q]
        wd = self.waited[q]
        if wd.get(sn, 0) < self.cnt[sn]:
            e.wait_ge(self.sems[sn], self.cnt[sn])
            wd[sn] = self.cnt[sn]
        self._deps(q, reads, writes)
        if nc_ok:
            with self.nc.allow_non_contiguous_dma(reason="small strided"):
                inst = e.dma_start(out=out, in_=in_)
        else:
            inst = e.dma_start(out=out, in_=in_)
        self.cnt[sn] += 16
        inst.then_inc(self.sems[sn], 16)
        self._mark((sn, self.cnt[sn]), reads, writes)
        return inst

    def wait_all(self, eng, deps):
        deps = self._norm(deps)
        self._deps(eng, deps, [])


def bcast_row(ap_1d_dram, nparts, n):
    return bass.AP(tensor=ap_1d_dram.tensor, offset=ap_1d_dram.offset, ap=[[0, nparts], [1, n]])


def t5_bucket_np(rel):
    nb = 16
    max_exact = 8
    side = np.where(rel > 0, nb, 0)
    n = np.abs(rel)
    nf = np.maximum(n, 1).astype(np.float32)
    large = max_exact + (np.log(nf / max_exact) / np.log(np.float32(128 / max_exact))
                         * (nb - max_exact)).astype(np.int32)
    large = np.minimum(large, nb - 1)
    return side + np.where(n < max_exact, n, large)


def onehot_rev():
    j = np.arange(384)
    dd = 127 - j
    b = t5_bucket_np(dd)
    oh = np.zeros((32, 384), np.float32)
    oh[b, j] = 1.0
    return oh


def build_nc(dbg=(), stop_after=None):
    nc = bass.Bass("TRN2", target_bir_lowering=False)

    def din(name, shape, dt=F32):
        return nc.dram_tensor(name, list(shape), dt, kind="ExternalInput").ap()

    x = din("x", [NSEQ * T, D])
    w_in = din("w_in", [D, INC])
    c_norm = din("c_norm", [128])
    w_uk = din("w_uk", [8, 128, 64])
    w_uv = din("w_uv", [8, 128, 64])
    rel_bias = din("rel_bias", [32, 8])
    conv_w = din("conv_w", [4, 1536])
    a_log = din("a_log", [4])
    dt_bias = din("dt_bias", [4])
    o_norm = din("o_norm", [128])
    w_out = din("w_out", [D, D])
    g_pre_mix = din("pre_norm_mix", [D])
    g_post_mix = din("post_norm_mix", [D])
    g_pre_mlp = din("pre_norm_mlp", [D])
    g_post_mlp = din("post_norm_mlp", [D])
    w1 = din("w_mlp_in", [D, DFF])
    w2 = din("w_mlp_out", [DFF, D])
    ohr = din("ohr", [32, 384])
    out = nc.dram_tensor("out", [NSEQ * T, D], F32, kind="ExternalOutput").ap()

    def dscr(name, shape, dt):
        return nc.dram_tensor(name, list(shape), dt, kind="Internal").ap()

    winb = dscr("winb", [D, INC], BF16)
    woutb = dscr("woutb", [D, D], BF16)
    w1b = dscr("w1b", [D, DFF], BF16)
    w2b = dscr("w2b", [DFF, D], BF16)
    vscr = dscr("vscr", [8, 384], F32)

    dbg_out = {}

    def dbg_tensor(name, shape, dt=F32):
        if name in dbg:
            dbg_out[name] = nc.dram_tensor("dbg_" + name, list(shape), dt, kind="ExternalOutput").ap()
            return dbg_out[name]
        return None

    with ExitStack() as es:
        K = KB(nc, es)
        _build(nc, K, es, locals(), dbg, stop_after, dbg_tensor)
    return nc


class Stop(Exception):
    pass


def _build(nc, K, es, g, dbg, stop_after, dbg_tensor):
    x = g["x"]; out = g["out"]
    PS = [K.ps(es, "ps%d" % i, [128, 512], F32) for i in range(8)]

    def psbf(i):
        return PS[i].t[:].bitcast(BF16)

    cst = lambda name, shape, dt=F32: K.sb(es, name, shape, dt)
    dif = cst("dif", [128, 128])
    K.op("pool", lambda e: e.iota(dif[:], pattern=[[1, 128]], base=0, channel_multiplier=-1,
                                  allow_small_or_imprecise_dtypes=True), [], [dif])
    ident = cst("ident", [128, 128])
    identb = cst("identb", [128, 128], BF16)
    K.op("dve", lambda e: e.tensor_scalar(out=ident[:], in0=dif[:], scalar1=0.0, scalar2=None,
                                          op0=ALU.is_equal), [dif], [ident])
    K.op("dve", lambda e: e.tensor_copy(out=identb[:], in_=ident[:]), [ident], [identb])
    ones = cst("ones", [128, 128])
    K.op("dve", lambda e: e.memset(ones[:], 1.0), [], [ones])
    m_le = cst("m_le", [64, 64])
    m_lt = cst("m_lt", [64, 64])
    m_ge = cst("m_ge", [64, 64])
    m_gt = cst("m_gt", [64, 64])
    for m, opx in ((m_le, ALU.is_le), (m_lt, ALU.is_lt), (m_ge, ALU.is_ge), (m_gt, ALU.is_gt)):
        K.op("dve", lambda e, m=m, opx=opx: e.tensor_scalar(out=m[:], in0=dif[0:64, 0:64], scalar1=0.0,
                                                            scalar2=None, op0=opx), [dif], [m])
    cm2 = cst("cm2", [128, 64])
    cmr2 = cst("cmr2", [128, 64])
    K.op("dve", lambda e: e.tensor_scalar(out=cm2[0:64, :], in0=dif[0:64, 0:64], scalar1=0.0, scalar2=None,
                                          op0=ALU.is_ge), [dif], [cm2])
    K.op("dve", lambda e: e.tensor_scalar(out=cm2[64:128, :], in0=dif[64:128, 64:128], scalar1=0.0,
                                          scalar2=None, op0=ALU.is_ge), [dif], [cm2])
    K.op("dve", lambda e: e.tensor_scalar(out=cmr2[0:64, :], in0=dif[0:64, 0:64], scalar1=0.0, scalar2=None,
                                          op0=ALU.is_lt), [dif], [cmr2])
    K.op("dve", lambda e: e.tensor_scalar(out=cmr2[64:128, :], in0=dif[64:128, 64:128], scalar1=0.0,
                                          scalar2=None, op0=ALU.is_lt), [dif], [cmr2])
    negdiag = cst("negdiag", [128, 128])
    K.op("dve", lambda e: e.memset(negdiag[:], 0.0), [], [negdiag])
    K.op("dve", lambda e: e.memset(negdiag[0:64, 64:128], -1e30), [], [negdiag])
    kreq = cst("kreq", [128, NT])
    for qt in range(NT):
        for hf in range(2):
            lim = qt * 128 + (hf + 1) * 64
            K.op("dve", lambda e, qt=qt, hf=hf, lim=lim: e.memset(
                kreq[hf * 64:(hf + 1) * 64, qt:qt + 1], float(min(TOPK, lim))), [], [kreq])
    pow2 = cst("pow2", [128, NBIS])
    for i in range(NBIS):
        K.op("pool", lambda e, i=i: e.memset(pow2[:, i:i + 1], float(2.0 ** (-i))), [], [pow2])
    epsc = cst("epsc", [128, 1])
    K.op("dve", lambda e: e.memset(epsc[:], EPS), [], [epsc])
    onec = cst("onec", [128, 1])
    K.op("dve", lambda e: e.memset(onec[:], 1.0), [], [onec])

    def gT(vec, name):
        t = cst(name, [128, 8])
        K.dma("sp", t[:], vec.rearrange("(k p) -> p k", p=128), [], [t], nc_ok=True)
        return t
    g1T = gT(g["g_pre_mix"], "g1T")
    g3T = gT(g["g_pre_mlp"], "g3T")

    def gB(vec, n, name, parts=128):
        t = cst(name, [parts, n])
        K.dma("sp", t[:], bcast_row(vec, parts, n), [], [t], nc_ok=True)
        return t
    g2B = gB(g["g_post_mix"], D, "g2B")
    g4B = gB(g["g_post_mlp"], D, "g4B")
    cnB = gB(g["c_norm"], 128, "cnB")
    onB = gB(g["o_norm"], 128, "onB", 64)
    alB = gB(g["a_log"], 4, "alB", 64)
    dtB = gB(g["dt_bias"], 4, "dtB", 64)
    b15B = gB(g["rel_bias"][15, :], 8, "b15B")
    negA = cst("negA", [64, 4])
    K.op("act", lambda e: e.activation(out=negA[:], in_=alB[:], func=AF.Exp), [alB], [negA])
    K.op("dve", lambda e: e.tensor_scalar(out=negA[:], in0=negA[:], scalar1=-1.0, scalar2=None, op0=ALU.mult),
         [negA], [negA])
    cw = cst("cw", [128, 12, 4])
    for j in range(4):
        K.dma("sp", cw[:, :, j], g["conv_w"][j, :].rearrange("(k p) -> p k", p=128), [], [cw], nc_ok=True)

    wukT = cst("wukT", [128, 4, 128], BF16)
    wuvb = cst("wuvb", [128, 512], BF16)
    Tb = cst("Tb", [128, 8, 256], BF16)
    with K.scope() as es2:
        tmpk = K.sb(es2, "tmpk", [128, 8, 64], F32)
        tmpv = K.sb(es2, "tmpv", [128, 8, 64], F32)
        K.dma("sp", tmpk[:], g["w_uk"].rearrange("h c d -> c h d"), [], [tmpk], nc_ok=True)
        K.dma("sp", tmpv[:], g["w_uv"].rearrange("h c d -> c h d"), [], [tmpv], nc_ok=True)
        K.op("dve", lambda e: e.tensor_copy(out=wuvb[:], in_=tmpv[:].rearrange("p h d -> p (h d)")),
             [tmpv], [wuvb])
        for j in range(4):
            K.op("pe", lambda e, j=j: e.transpose(
                out=PS[0][:, j * 128:(j + 1) * 128],
                in_=tmpk[:, 2 * j:2 * j + 2, :].rearrange("p h d -> p (h d)"), identity=ident[:]),
                [tmpk, ident], [PS[0]])
        K.op("dve", lambda e: e.tensor_copy(out=wukT[:].rearrange("p j c -> p (j c)"), in_=PS[0][:, :]),
             [PS[0]], [wukT])

        rb = K.sb(es2, "rb", [32, 8], F32)
        rbT = K.sb(es2, "rbT", [8, 32], F32)
        oh = K.sb(es2, "oh", [32, 384], F32)
        K.dma("sp", rb[:], g["rel_bias"], [], [rb])
        K.dma("sp", rbT[:], g["rel_bias"].rearrange("b h -> h b"), [], [rbT], nc_ok=True)
        K.dma("sp", oh[:], g["ohr"], [], [oh])
        K.op("pe", lambda e: e.matmul(PS[1][0:8, 0:384], lhsT=rb[:], rhs=oh[:], start=True, stop=True),
             [rb, oh], [PS[1]])
        vr = K.sb(es2, "vr", [8, 384], F32)
        K.op("dve", lambda e: e.tensor_scalar(out=vr[:], in0=PS[1][0:8, 0:384], scalar1=rbT[:, 15:16],
                                              scalar2=None, op0=ALU.subtract), [PS[1], rbT], [vr])
        vs = g["vscr"]
        dvs = Dep()
        K.dma("sp", vs, vr[:], [vr], [dvs])
        Tb32 = K.sb(es2, "Tb32", [128, 8, 256], F32)
        src = bass.AP(tensor=vs.tensor, offset=vs.offset, ap=[[1, 128], [384, 8], [1, 256]])
        K.dma("sp", Tb32[:], src, [dvs], [Tb32], nc_ok=True)
        smt = K.sb(es2, "smt", [128, 128], F32)
        Jm = K.sb(es2, "Jm", [128, 128], F32)
        K.op("pool", lambda e: e.iota(smt[:], pattern=[[1, 128]], base=0, channel_multiplier=1,
                                      allow_small_or_imprecise_dtypes=True), [], [smt])
        K.op("dve", lambda e: e.tensor_scalar(out=Jm[:], in0=smt[:], scalar1=127.0, scalar2=None,
                                              op0=ALU.is_equal), [smt], [Jm])
        Tbf = Tb32[:].rearrange("p h u -> p (h u)")
        for q in range(4):
            K.op("pe", lambda e, q=q: e.matmul(PS[2 + q][:, :], lhsT=Jm[:], rhs=Tbf[:, q * 512:(q + 1) * 512],
                                               start=True, stop=True), [Jm, Tb32], [PS[2 + q]])
            K.op("dve", lambda e, q=q: e.tensor_copy(
                out=Tb[:].rearrange("p h u -> p (h u)")[:, q * 512:(q + 1) * 512], in_=PS[2 + q][:, :]),
                [PS[2 + q]], [Tb])
        d = dbg_tensor("Tb", [128, 8, 256], BF16)
        if d is not None:
            K.dma("sp", d, Tb[:], [Tb], [Dep()])

        stg_deps = {}
        engs_rr = ["dve", "pool", "act"]
        rr = [0]

        def stage(src, dst, nrows, ncols, gT_tile, key):
            dd = Dep()
            stg_deps[key] = dd
            f = [K.sb(es2, "stf", [128, 2048], F32) for _ in range(2)]
            b = [K.sb(es2, "stb", [128, 2048], BF16) for _ in range(2)]
            i = 0
            for kc in range(nrows // 128):
                for c0 in range(0, ncols, 2048):
                    cn = min(2048, ncols - c0)
                    ft, bt = f[i % 2], b[i % 2]
                    K.dma("sp", ft[:, 0:cn], src[kc * 128:(kc + 1) * 128, c0:c0 + cn], [], [ft])
                    eng = engs_rr[rr[0] % 3]
                    rr[0] += 1
                    if gT_tile is not None:
                        if eng == "act":
                            K.op("act", lambda e, ft=ft, bt=bt, cn=cn, kc=kc: e.activation(
                                out=bt[:, 0:cn], in_=ft[:, 0:cn], func=AF.Copy, scale=gT_tile[:, kc:kc + 1]),
                                [ft, gT_tile], [bt])
                        else:
                            K.op(eng, lambda e, ft=ft, bt=bt, cn=cn, kc=kc: e.tensor_scalar(
                                out=bt[:, 0:cn], in0=ft[:, 0:cn], scalar1=gT_tile[:, kc:kc + 1], scalar2=None,
                                op0=ALU.mult), [ft, gT_tile], [bt])
                    else:
                        if eng == "act":
                            K.op("act", lambda e, ft=ft, bt=bt, cn=cn: e.activation(
                                out=bt[:, 0:cn], in_=ft[:, 0:cn], func=AF.Copy), [ft], [bt])
                        else:
                            K.op(eng, lambda e, ft=ft, bt=bt, cn=cn: e.tensor_copy(out=bt[:, 0:cn], in_=ft[:, 0:cn]),
                                 [ft], [bt])
                    K.dma("sp", dst[kc * 128:(kc + 1) * 128, c0:c0 + cn], bt[:, 0:cn], [bt], [Dep()])
                    i += 1

        stage(g["w_in"], g["winb"], D, INC, g1T, "win")
        stage(g["w_out"], g["woutb"], D, D, None, "wout")
        stage(g["w1"], g["w1b"], D, DFF, g3T, "w1")
        stage(g["w2"], g["w2b"], DFF, D, None, "w2")
    def drain_sp():
        for sn in K.dq["sp"][0]:
            for q in ("sp", "act", "pool"):
                if K.waited[q].get(sn, 0) < K.cnt[sn]:
                    K.engs[q].wait_ge(K.sems[sn], K.cnt[sn])
                    K.waited[q][sn] = K.cnt[sn]
    drain_sp()
    if stop_after == "stage":
        return

    for seq in range(NSEQ):
        with K.scope() as ss:
            _seq(nc, K, ss, g, dbg, stop_after, dbg_tensor, seq, locals())
        if stop_after is not None:
            break
    for q in ("sp", "act", "pool"):
        for sn in K.dq[q][0]:
            if K.waited["sp"].get(sn, 0) < K.cnt[sn]:
                K.engs["sp"].wait_ge(K.sems[sn], K.cnt[sn])
                K.waited["sp"][sn] = K.cnt[sn]


def _seq(nc, K, ss, g, dbg, stop_after, dbg_tensor, seq, L):
    PS = L["PS"]; psbf = L["psbf"]
    ident = L["ident"]; identb = L["identb"]; ones = L["ones"]
    epsc = L["epsc"]
    x = g["x"]; out = g["out"]
    tok0 = seq * T
    dbgon = (seq == 0)

    def dump(name, ap_sb, deps, shape, dt=F32):
        if not dbgon:
            return
        d = dbg_tensor(name, shape, dt)
        if d is not None:
            K.dma("sp", d, ap_sb, deps, [Dep()], nc_ok=True)

    mixT = K.sb(ss, "mixT", [128, 8, T], BF16)

    def make_xT(s1):
        xT = K.sb(s1, "xT", [128, 8, T], BF16)
        with K.scope() as sa:
            xin = [K.sb(sa, "xin", [128, D], F32) for _ in range(2)]
            xb = [K.sb(sa, "xb", [128, D], BF16) for _ in range(2)]
            junk = K.sb(sa, "junk", [128, D], BF16)
            st = [K.sb(sa, "st", [128, 4], F32) for _ in range(2)]
            for i in range(NT):
                xi, xbi, sti = xin[i % 2], xb[i % 2], st[i % 2]
                K.dma("sp", xi[:], x[tok0 + i * 128: tok0 + (i + 1) * 128, :], [], [xi])
                K.op("act", lambda e: e.activation(out=junk[:], in_=xi[:], func=AF.Square,
                                                   accum_out=sti[:, 0:1]), [xi], [junk, sti])
                K.op("act", lambda e: e.activation(out=sti[:, 1:2], in_=sti[:, 0:1], func=AF.Sqrt,
                                                   bias=epsc[:, 0:1], scale=1.0 / D), [sti, epsc], [sti])
                K.op("dve", lambda e: e.reciprocal(out=sti[:, 2:3], in_=sti[:, 1:2]), [sti], [sti])
                K.op("dve", lambda e: e.tensor_scalar(out=xbi[:], in0=xi[:], scalar1=sti[:, 2:3], scalar2=None,
                                                      op0=ALU.mult), [xi, sti], [xbi])
                pb = PS[i % 2]
                for kc in range(8):
                    K.op("pe", lambda e, kc=kc: e.transpose(
                        out=pb.t[:].bitcast(BF16)[:, kc * 128:(kc + 1) * 128],
                        in_=xbi[:, kc * 128:(kc + 1) * 128], identity=identb[:]), [xbi, identb], [pb])
                K.op("act" if i % 2 else "dve", lambda e: (e.tensor_copy if hasattr(e, "tensor_copy") else e.copy)(
                    out=xT[:, :, i * 128:(i + 1) * 128],
                    in_=pb.t[:].bitcast(BF16).rearrange("p (k t) -> p k t", k=8)), [pb], [xT])
        return xT

    if stop_after == "A0":
        with K.scope() as s0:
            xT = make_xT(s0)
            dump("xT", xT[:, :, 0:128], [xT], [128, 8, 128], BF16)
        return
    with K.scope() as s1:
        _gdn(nc, K, s1, g, dbg, stop_after, dump, seq, L, make_xT, mixT)
    if stop_after in ("gdn", "gdn_pre"):
        return
    with K.scope() as s2:
        _dsa(nc, K, s2, g, dbg, stop_after, dump, seq, L, make_xT, mixT)
    if stop_after == "dsa":
        return
    _mlp(nc, K, ss, g, dbg, stop_after, dump, seq, L, mixT)


def bc_mid(ap2, n):
    return ap2.unsqueeze(1).to_broadcast([ap2.shape[0], n, ap2.shape[1]])


def bc_last(ap2, n):
    return ap2.unsqueeze(2).to_broadcast([ap2.shape[0], ap2.shape[1], n])


def _gdn(nc, K, s1, g, dbg, stop_after, dump, seq, L, make_xT, mixT):
    PS = L["PS"]
    ident = L["ident"]; identb = L["identb"]; ones = L["ones"]
    epsc = L["epsc"]; onec = L["onec"]
    cm2 = L["cm2"]; cmr2 = L["cmr2"]
    m_lt = L["m_lt"]; m_gt = L["m_gt"]; m_ge = L["m_ge"]
    cw = L["cw"]; onB = L["onB"]; dtB = L["dtB"]; negA = L["negA"]
    winb = g["winb"]
    NCH = T // 64
    rr = [0]

    def nb():
        rr[0] += 1
        return PS[rr[0] % 8]

    def bfv(p):
        return p.t[:].bitcast(BF16)

    with K.scope() as sg:
        cvT = K.sb(sg, "cvT", [128, 12, T], BF16)
        sz = K.sb(sg, "sz", [64, NCH, 512], BF16)
        abbb = K.sb(sg, "abbb", [64, NCH, 8], F32)
        with K.scope() as sw:
            xT = make_xT(sw)
            NG = INC - O_QB
            wg = K.sb(sw, "wg", [128, 8, NG], BF16)
            K.dma("sp", wg[:], winb[:, O_QB:INC].rearrange("(k p) c -> p k c", p=128), [], [wg])
            ev = 0
            for tb in range(4):
                for cc in range(12):
                    p = nb()
                    for kc in range(8):
                        K.op("pe", lambda e: e.matmul(p[:, :], lhsT=wg[:, kc, cc * 128:(cc + 1) * 128],
                                                      rhs=xT[:, kc, tb * 512:(tb + 1) * 512],
                                                      start=(kc == 0), stop=(kc == 7)), [wg, xT], [p])
                    if ev % 2 == 0:
                        K.op("act", lambda e: e.copy(out=cvT[:, cc, tb * 512:(tb + 1) * 512], in_=p[:, :]),
                             [p], [cvT])
                    else:
                        K.op("dve", lambda e: e.tensor_copy(out=cvT[:, cc, tb * 512:(tb + 1) * 512], in_=p[:, :]),
                             [p], [cvT])
                    ev += 1
            for ch in range(NCH):
                p = nb()
                pz = nb()
                for kc in range(8):
                    K.op("pe", lambda e: e.matmul(p[0:64, 0:8], lhsT=xT[:, kc, ch * 64:(ch + 1) * 64],
                                                  rhs=wg[:, kc, 1536:1544], start=(kc == 0), stop=(kc == 7)),
                         [wg, xT], [p])
                for kc in range(8):
                    K.op("pe", lambda e: e.matmul(pz[0:64, :], lhsT=xT[:, kc, ch * 64:(ch + 1) * 64],
                                                  rhs=wg[:, kc, 1544:2056], start=(kc == 0), stop=(kc == 7)),
                         [wg, xT], [pz])
                K.op("dve", lambda e: e.tensor_copy(out=abbb[:, ch, :], in_=p[0:64, 0:8]), [p], [abbb])
                K.op("act", lambda e: e.activation(out=sz[:, ch, :], in_=pz[0:64, :], func=AF.Silu), [pz], [sz])
        dump("qkv_pre", cvT[:, :, 0:256], [cvT], [128, 12, 256], BF16)
        dump("abbb", abbb[:, 0:4, :], [abbb], [64, 4, 8])

        gst = K.sb(sg, "gst", [64, NCH * 4], F32)
        beta = K.sb(sg, "beta", [64, NCH * 4], F32)
        eg = K.sb(sg, "eg", [64, NCH * 4], F32)
        egr = K.sb(sg, "egr", [64, NCH * 4], F32)
        egl = K.sb(sg, "egl", [128, NCH * 4], F32)
        gv = lambda t: t[:].rearrange("p (c h) -> p c h", h=4)
        K.op("dve", lambda e: e.tensor_tensor(out=gv(gst), in0=abbb[:, :, 0:4], in1=bc_mid(dtB[:, :], NCH),
                                              op=ALU.add), [abbb, dtB], [gst])
        K.op("act", lambda e: e.activation(out=gst[:], in_=gst[:], func=AF.Exp), [gst], [gst])
        K.op("act", lambda e: e.activation(out=gst[:], in_=gst[:], func=AF.Ln, bias=onec[0:64, 0:1], scale=1.0),
             [gst, onec], [gst])
        K.op("dve", lambda e: e.tensor_tensor(out=gv(gst), in0=gv(gst), in1=bc_mid(negA[:, :], NCH),
                                              op=ALU.mult), [gst, negA], [gst])
        K.op("act", lambda e: e.activation(out=gv(beta), in_=abbb[:, :, 4:8], func=AF.Sigmoid), [abbb], [beta])
        pG = nb()
        K.op("pe", lambda e: e.matmul(pG[0:64, 0:128], lhsT=cm2[0:64, :], rhs=gst[:], start=True, stop=True),
             [cm2, gst], [pG])
        K.op("pe", lambda e: e.matmul(pG[0:64, 128:256], lhsT=cmr2[0:64, :], rhs=gst[:], start=True, stop=True),
             [cmr2, gst], [pG])
        K.op("pe", lambda e: e.matmul(pG[:, 256:384], lhsT=ones[0:64, :], rhs=gst[:], start=True, stop=True),
             [ones, gst], [pG])
        K.op("act", lambda e: e.activation(out=eg[:], in_=pG[0:64, 0:128], func=AF.Exp), [pG], [eg])
        K.op("act", lambda e: e.activation(out=egr[:], in_=pG[0:64, 128:256], func=AF.Exp), [pG], [egr])
        K.op("act", lambda e: e.activation(out=egl[:], in_=pG[:, 256:384], func=AF.Exp), [pG], [egl])
        dump("gst", gst[:], [gst], [64, NCH * 4])
        dump("eg", eg[:], [eg], [64, NCH * 4])

        with K.scope() as sc:
            acc = [K.sb(sc, "cacc", [128, T], F32) for _ in range(2)]
            for cc in range(12):
                a = acc[cc % 2]
                K.op("dve", lambda e: e.tensor_scalar(out=a[:, :], in0=cvT[:, cc, :], scalar1=cw[:, cc, 3:4],
                                                      scalar2=None, op0=ALU.mult), [cvT, cw], [a])
                for sh in (1, 2, 3):
                    K.op("dve", lambda e: e.scalar_tensor_tensor(
                        out=a[:, sh:T], in0=cvT[:, cc, 0:T - sh], scalar=cw[:, cc, 3 - sh:4 - sh],
                        in1=a[:, sh:T], op0=ALU.mult, op1=ALU.add), [cvT, cw, a], [a])
                K.op("act", lambda e: e.activation(out=cvT[:, cc, :], in_=a[:, :], func=AF.Silu), [a], [cvT])
        dump("qkv_conv", cvT[:, :, 0:256], [cvT], [128, 12, 256], BF16)
        if stop_after == "gdn_pre":
            return

        S32 = K.sb(sg, "S32", [128, 512], F32)
        Sb = K.sb(sg, "Sb", [128, 512], BF16)
        K.op("dve", lambda e: e.memset(S32[:], 0.0), [], [S32])
        K.op("dve", lambda e: e.memset(Sb[:], 0.0), [], [Sb])
        ncm = K.sb(sg, "ncm", [64, 64], F32)
        K.op("dve", lambda e: e.tensor_scalar(out=ncm[:], in0=cm2[0:64, :], scalar1=-1.0, scalar2=None,
                                              op0=ALU.mult), [cm2], [ncm])
        idb = identb[0:64, 0:64]
        W2 = []
        for par in range(1):
            w = {}
            w["qk32"] = K.sb(sg, "qk32", [64, 8, 128], F32)
            w["sq"] = K.sb(sg, "sq", [64, 8, 128], F32)
            w["ss"] = K.sb(sg, "ss", [64, 8], F32)
            w["rn"] = K.sb(sg, "rn", [64, 8], F32)
            w["sc"] = K.sb(sg, "sc", [64, 6, 4], F32)
            for nm in ("qh", "kh", "kb", "kbg", "kd", "qg", "vb"):
                w[nm] = K.sb(sg, nm, [64, 4, 128], BF16)
            w["fT"] = K.sb(sg, "fT", [128, 16, 64], BF16)
            w["Gb"] = K.sb(sg, "Gb", [64, 4, 64], F32)
            w["Dn"] = K.sb(sg, "Dn", [64, 256], F32)
            w["Dp"] = K.sb(sg, "Dp", [64, 256], F32)
            w["E"] = K.sb(sg, "E", [64, 256], F32)
            w["Et"] = K.sb(sg, "Et", [64, 256], F32)
            w["EL"] = K.sb(sg, "EL", [64, 4, 64], F32)
            w["ELt"] = K.sb(sg, "ELt", [64, 4, 64], F32)
            w["EAt"] = K.sb(sg, "EAt", [64, 4, 64], F32)
            w["M"] = [K.sb(sg, "M", [64, 4, 64], BF16) for _ in range(2)]
            w["Mt"] = [K.sb(sg, "Mt", [64, 4, 64], BF16) for _ in range(2)]
            w["Atm"] = K.sb(sg, "Atm", [64, 4, 64], BF16)
            w["Pt32"] = K.sb(sg, "Pt32", [64, 4, 64], F32)
            w["Ptb"] = K.sb(sg, "Ptb", [64, 4, 64], BF16)
            w["negwT"] = K.sb(sg, "negwT", [128, 4, 64], BF16)
            w["vnew"] = K.sb(sg, "vnew", [64, 512], BF16)
            w["o32"] = K.sb(sg, "o32", [64, 4, 128], F32)
            w["osq"] = K.sb(sg, "osq", [64, 4, 128], F32)
            w["os"] = K.sb(sg, "os", [64, 8], F32)
            w["ob"] = K.sb(sg, "ob", [64, 512], BF16)
            W2.append(w)
        w1_ = dict(W2[0])
        for nm in ("vb", "kd"):
            w1_[nm] = K.sb(sg, nm, [64, 4, 128], BF16)
        w1_["fT"] = K.sb(sg, "fT", [128, 16, 64], BF16)
        w1_["Atm"] = K.sb(sg, "Atm", [64, 4, 64], BF16)
        w1_["Ptb"] = K.sb(sg, "Ptb", [64, 4, 64], BF16)
        w1_["negwT"] = K.sb(sg, "negwT", [128, 4, 64], BF16)
        W2.append(w1_)

        import os as _os
        if _os.environ.get('CH_LIMIT'):
            K.limit = int(_os.environ['CH_LIMIT'])
        def prep(ch):
            w = W2[ch % 2]
            c0, c1 = ch * 64, (ch + 1) * 64
            g0, g1 = ch * 4, ch * 4 + 4
            qk32, sq, ss_, rn, sc = w["qk32"], w["sq"], w["ss"], w["rn"], w["sc"]
            pa = nb(); pb = nb()
            for cc in range(8):
                K.op("pe", lambda e: e.transpose(out=bfv(pa)[0:64, cc * 128:(cc + 1) * 128],
                                                 in_=cvT[:, cc, c0:c1], identity=identb[:]), [cvT, identb], [pa])
            for cc in range(4):
                K.op("pe", lambda e: e.transpose(out=bfv(pb)[0:64, cc * 128:(cc + 1) * 128],
                                                 in_=cvT[:, 8 + cc, c0:c1], identity=identb[:]), [cvT, identb], [pb])
            K.op("act", lambda e: e.copy(out=qk32[:].rearrange("p a b -> p (a b)"), in_=bfv(pa)[0:64, :]),
                 [pa], [qk32])
            K.op("dve", lambda e: e.tensor_tensor(out=sq[:], in0=qk32[:], in1=qk32[:], op=ALU.mult), [qk32], [sq])
            K.op("dve", lambda e: e.tensor_reduce(out=ss_[:], in_=sq[:], axis=AX.X, op=ALU.add), [sq], [ss_])
            K.op("act", lambda e: e.activation(out=rn[:], in_=ss_[:], func=AF.Sqrt, bias=epsc[0:64, 0:1], scale=1.0),
                 [ss_, epsc], [rn])
            K.op("dve", lambda e: e.reciprocal(out=rn[:], in_=rn[:]), [rn], [rn])
            K.op("dve", lambda e: e.tensor_scalar(out=sc[:, 0, :], in0=rn[:, 0:4], scalar1=128.0 ** -0.5,
                                                  scalar2=None, op0=ALU.mult), [rn], [sc])
            K.op("dve", lambda e: e.tensor_tensor(out=sc[:, 1, :], in0=rn[:, 4:8], in1=beta[:, g0:g1],
                                                  op=ALU.mult), [rn, beta], [sc])
            K.op("dve", lambda e: e.tensor_tensor(out=sc[:, 2, :], in0=sc[:, 1, :], in1=eg[:, g0:g1],
                                                  op=ALU.mult), [sc, eg], [sc])
            K.op("dve", lambda e: e.tensor_tensor(out=sc[:, 3, :], in0=rn[:, 4:8], in1=egr[:, g0:g1],
                                                  op=ALU.mult), [rn, egr], [sc])
            K.op("dve", lambda e: e.tensor_tensor(out=sc[:, 4, :], in0=sc[:, 0, :], in1=eg[:, g0:g1],
                                                  op=ALU.mult), [sc, eg], [sc])
            q32 = qk32[:, 0:4, :]
            k32 = qk32[:, 4:8, :]
            plan = (("qh", q32, sc[:, 0, :], "dve"), ("kh", k32, rn[:, 4:8], "dve"),
                    ("kb", k32, sc[:, 1, :], "dve"), ("kbg", k32, sc[:, 2, :], "dve"),
                    ("kd", k32, sc[:, 3, :], "dve"), ("qg", q32, sc[:, 4, :], "dve"))
            for nm, src, scl, eng in plan:
                K.op(eng, lambda e: e.tensor_tensor(out=w[nm][:], in0=src, in1=bc_last(scl, 128), op=ALU.mult),
                     [qk32, sc, rn], [w[nm]])
            K.op("dve", lambda e: e.tensor_tensor(
                out=w["vb"][:], in0=bfv(pb)[0:64, 0:512].rearrange("p (h d) -> p h d", h=4),
                in1=bc_last(beta[:, g0:g1], 128), op=ALU.mult), [pb, beta], [w["vb"]])
            pc = nb()
            for ki, nm in enumerate(("kh", "kb", "qh", "qg")):
                for h in range(4):
                    K.op("pe", lambda e: e.transpose(out=bfv(pc)[:, (ki * 4 + h) * 64:(ki * 4 + h + 1) * 64],
                                                     in_=w[nm][:, h, :], identity=idb), [w[nm], identb], [pc])
            fT = w["fT"]
            K.op("act", lambda e: e.copy(out=fT[:].rearrange("p a b -> p (a b)"), in_=bfv(pc)[:, :]), [pc], [fT])
            pd = nb(); pe_ = nb()
            for h in range(4):
                K.op("pe", lambda e: e.matmul(pd[0:64, h * 64:(h + 1) * 64], lhsT=fT[:, 4 + h, :], rhs=fT[:, h, :],
                                              start=True, stop=True), [fT], [pd])
                K.op("pe", lambda e: e.matmul(pd[0:64, 256 + h * 64:256 + (h + 1) * 64], lhsT=fT[:, h, :],
                                              rhs=fT[:, 4 + h, :], start=True, stop=True), [fT], [pd])
                K.op("pe", lambda e: e.matmul(pe_[0:64, h * 64:(h + 1) * 64], lhsT=fT[:, h, :], rhs=fT[:, 8 + h, :],
                                              start=True, stop=True), [fT], [pe_])
            Gb = w["Gb"]
            K.op("dve", lambda e: e.tensor_copy(out=Gb[:], in_=bc_last(gst[:, g0:g1], 64)), [gst], [Gb])
            for h in range(4):
                K.op("pe", lambda e: e.matmul(pe_[0:64, 256 + h * 64:256 + (h + 1) * 64], lhsT=cm2[0:64, :],
                                              rhs=Gb[:, h, :], start=True, stop=False), [cm2, Gb], [pe_])
                K.op("pe", lambda e: e.matmul(pe_[0:64, 256 + h * 64:256 + (h + 1) * 64], lhsT=Gb[:, h, :],
                                              rhs=ncm[:], start=False, stop=True), [ncm, Gb], [pe_])
            Dn, Dp, E, Et = w["Dn"], w["Dp"], w["E"], w["Et"]
            K.op("dve", lambda e: e.tensor_scalar(out=Dn[:], in0=pe_[0:64, 256:512], scalar1=0.0, scalar2=None,
                                                  op0=ALU.min), [pe_], [Dn])
            K.op("dve", lambda e: e.tensor_scalar(out=Dp[:], in0=pe_[0:64, 256:512], scalar1=0.0, scalar2=None,
                                                  op0=ALU.max), [pe_], [Dp])
            K.op("act", lambda e: e.activation(out=E[:], in_=Dn[:], func=AF.Exp), [Dn], [E])
            K.op("act", lambda e: e.activation(out=Et[:], in_=Dp[:], func=AF.Exp, scale=-1.0), [Dp], [Et])
            v4 = lambda t: t[:].rearrange("p (h s) -> p h s", h=4)
            EL, ELt, EAt = w["EL"], w["ELt"], w["EAt"]
            K.op("dve", lambda e: e.tensor_tensor(out=EL[:], in0=v4(E), in1=bc_mid(m_lt[:, :], 4), op=ALU.mult),
                 [E, m_lt], [EL])
            K.op("dve", lambda e: e.tensor_tensor(out=ELt[:], in0=v4(Et), in1=bc_mid(m_gt[:, :], 4), op=ALU.mult),
                 [Et, m_gt], [ELt])
            K.op("dve", lambda e: e.tensor_tensor(out=EAt[:], in0=v4(Et), in1=bc_mid(m_ge[:, :], 4), op=ALU.mult),
                 [Et, m_ge], [EAt])
            M, Mt = w["M"][0], w["Mt"][0]
            Atm, Pt32, Ptb = w["Atm"], w["Pt32"], w["Ptb"]
            pv4 = lambda p, o: p[0:64, o:o + 256].rearrange("p (h s) -> p h s", h=4)
            K.op("dve", lambda e: e.tensor_tensor(out=M[:], in0=pv4(pd, 0), in1=EL[:], op=ALU.mult), [pd, EL], [M])
            K.op("dve", lambda e: e.tensor_tensor(out=Mt[:], in0=pv4(pd, 256), in1=ELt[:], op=ALU.mult),
                 [pd, ELt], [Mt])
            K.op("dve", lambda e: e.tensor_tensor(out=Atm[:], in0=pv4(pe_, 0), in1=EAt[:], op=ALU.mult),
                 [pe_, EAt], [Atm])
            K.op("dve", lambda e: e.tensor_tensor(out=Pt32[:], in0=bc_mid(ident[0:64, 0:64], 4), in1=Mt[:],
                                                   op=ALU.subtract), [ident, Mt], [Pt32])
            K.op("dve", lambda e: e.tensor_copy(out=Ptb[:], in_=Pt32[:]), [Pt32], [Ptb])
            for lev in range(5):
                Mn, Mtn = w["M"][(lev + 1) % 2], w["Mt"][(lev + 1) % 2]
                p1 = nb()
                for h in range(4):
                    K.op("pe", lambda e: e.matmul(p1[0:64, h * 64:(h + 1) * 64], lhsT=Mt[:, h, :], rhs=M[:, h, :],
                                                  start=True, stop=True), [M, Mt], [p1])
                if lev < 4:
                    for h in range(4):
                        K.op("pe", lambda e: e.matmul(p1[0:64, 256 + h * 64:256 + (h + 1) * 64], lhsT=M[:, h, :],
                                                      rhs=Mt[:, h, :], start=True, stop=True), [M, Mt], [p1])
                K.op("act", lambda e: e.copy(out=Mn[:], in_=pv4(p1, 0)), [p1], [Mn])
                if lev < 4:
                    K.op("dve", lambda e: e.tensor_copy(out=Mtn[:], in_=pv4(p1, 256)), [p1], [Mtn])
                p2 = nb()
                for h in range(4):
                    K.op("pe", lambda e: e.matmul(p2[0:64, h * 64:(h + 1) * 64], lhsT=Mn[:, h, :], rhs=Ptb[:, h, :],
                                                  start=True, stop=True), [Mn, Ptb], [p2])
                K.op("dve", lambda e: e.tensor_tensor(out=Pt32[:], in0=pv4(p2, 0), in1=Pt32[:], op=ALU.add),
                     [p2, Pt32], [Pt32])
                K.op("dve", lambda e: e.tensor_copy(out=Ptb[:], in_=Pt32[:]), [Pt32], [Ptb])
                M, Mt = Mn, Mtn
            if ch < 2:
                dump("Pt%d" % ch, Pt32[:], [Pt32], [64, 4, 64])
            negwT, vnew = w["negwT"], w["vnew"]
            p3 = nb()
            for h in range(4):
                K.op("pe", lambda e: e.matmul(p3[:, h * 64:(h + 1) * 64], lhsT=w["kbg"][:, h, :], rhs=Ptb[:, h, :],
                                              start=True, stop=True), [w["kbg"], Ptb], [p3])
            K.op("act", lambda e: e.activation(out=negwT[:].rearrange("p h c -> p (h c)"), in_=p3[:, 0:256],
                                               func=AF.Copy, scale=-1.0), [p3], [negwT])
        def rec(ch):
            w = W2[ch % 2]
            c0, c1 = ch * 64, (ch + 1) * 64
            g0, g1 = ch * 4, ch * 4 + 4
            fT = w["fT"]
            Atm, Ptb = w["Atm"], w["Ptb"]
            negwT, vnew = w["negwT"], w["vnew"]
            p4 = nb()
            for h in range(4):
                K.op("pe", lambda e: e.matmul(p4[0:64, h * 128:(h + 1) * 128], lhsT=Ptb[:, h, :], rhs=w["vb"][:, h, :],
                                              start=True, stop=False), [Ptb, w["vb"]], [p4])
                K.op("pe", lambda e: e.matmul(p4[0:64, h * 128:(h + 1) * 128], lhsT=negwT[:, h, :],
                                              rhs=Sb[:, h * 128:(h + 1) * 128], start=False, stop=True),
                     [negwT, Sb], [p4])
            K.op("act", lambda e: e.copy(out=vnew[:], in_=p4[0:64, :]), [p4], [vnew])
            p5 = nb()
            for h in range(4):
                K.op("pe", lambda e: e.matmul(p5[0:64, h * 128:(h + 1) * 128], lhsT=fT[:, 12 + h, :],
                                              rhs=Sb[:, h * 128:(h + 1) * 128], start=True, stop=False),
                     [fT, Sb], [p5])
                K.op("pe", lambda e: e.matmul(p5[0:64, h * 128:(h + 1) * 128], lhsT=Atm[:, h, :],
                                              rhs=vnew[:, h * 128:(h + 1) * 128], start=False, stop=True),
                     [Atm, vnew], [p5])
            p6 = nb()
            for h in range(4):
                K.op("pe", lambda e: e.matmul(p6[:, h * 128:(h + 1) * 128], lhsT=w["kd"][:, h, :],
                                              rhs=vnew[:, h * 128:(h + 1) * 128], start=True, stop=True),
                     [w["kd"], vnew], [p6])
            S4 = S32[:].rearrange("p (h d) -> p h d", h=4)
            K.op("dve", lambda e: e.tensor_tensor(out=S4, in0=S4, in1=bc_last(egl[:, g0:g1], 128), op=ALU.mult),
                 [S32, egl], [S32])
            K.op("dve", lambda e: e.tensor_tensor(out=S32[:], in0=p6[:, :], in1=S32[:], op=ALU.add), [p6, S32], [S32])
            K.op("act", lambda e: e.copy(out=Sb[:], in_=S32[:]), [S32], [Sb])
            o32, osq, os_, ob = w["o32"], w["osq"], w["os"], w["ob"]
            K.op("act", lambda e: e.copy(out=o32[:].rearrange("p h d -> p (h d)"), in_=p5[0:64, :]), [p5], [o32])
            K.op("dve", lambda e: e.tensor_tensor(out=osq[:], in0=o32[:], in1=o32[:], op=ALU.mult), [o32], [osq])
            K.op("dve", lambda e: e.tensor_reduce(out=os_[:, 0:4], in_=osq[:], axis=AX.X, op=ALU.add), [osq], [os_])
            K.op("act", lambda e: e.activation(out=os_[:, 4:8], in_=os_[:, 0:4], func=AF.Sqrt, bias=epsc[0:64, 0:1],
                                               scale=1.0 / 128), [os_, epsc], [os_])
            K.op("dve", lambda e: e.reciprocal(out=os_[:, 4:8], in_=os_[:, 4:8]), [os_], [os_])
            K.op("dve", lambda e: e.tensor_tensor(out=o32[:], in0=o32[:], in1=bc_last(os_[:, 4:8], 128), op=ALU.mult),
                 [o32, os_], [o32])
            K.op("dve", lambda e: e.tensor_tensor(out=o32[:], in0=o32[:], in1=bc_mid(onB[:, :], 4), op=ALU.mult),
                 [o32, onB], [o32])
            K.op("dve", lambda e: e.tensor_tensor(out=ob[:], in0=o32[:].rearrange("p h d -> p (h d)"),
                                                  in1=sz[:, ch, :], op=ALU.mult), [o32, sz], [ob])
            p7 = nb()
            for h in range(4):
                K.op("pe", lambda e: e.transpose(out=bfv(p7)[:, h * 64:(h + 1) * 64], in_=ob[:, h * 128:(h + 1) * 128],
                                                 identity=idb), [ob, identb], [p7])
            K.op("act", lambda e: e.copy(out=mixT[:, 4:8, c0:c1],
                                         in_=bfv(p7)[:, 0:256].rearrange("p (h t) -> p h t", h=4)), [p7], [mixT])
        NCHR = int(_os.environ.get('GDN_NCH', NCH))
        if NCHR > 0:
            prep(0)
        for ch in range(NCHR):
            if ch + 1 < NCHR:
                prep(ch + 1)
            rec(ch)
        dump("ob", mixT[:, 4:8, 0:256], [mixT], [128, 4, 256], BF16)


def _dsa(nc, K, s1, g, dbg, stop_after, dump, seq, L, make_xT, mixT):
    PS = L["PS"]
    ident = L["ident"]; identb = L["identb"]; epsc = L["epsc"]
    negdiag = L["negdiag"]; kreq = L["kreq"]; pow2 = L["pow2"]; cnB = L["cnB"]
    wukT = L["wukT"]; wuvb = L["wuvb"]; Tb = L["Tb"]; b15B = L["b15B"]
    winb = g["winb"]
    rr = [0]

    def nb(lo=0, n=6):
        rr[0] += 1
        return PS[lo + rr[0] % n]

    def bfv(p):
        return p.t[:].bitcast(BF16)

    with K.scope() as sd:
        qabsT = K.sb(sd, "qabsT", [128, 8, T], BF16)
        cT = K.sb(sd, "cT", [128, T], BF16)
        Vaug = K.sb(sd, "Vaug", [128, NT, 8, 65], BF16)
        qiT = K.sb(sd, "qiT", [128, 4, T], BF16)
        kiT2 = K.sb(sd, "kiT2", [128, T], BF16)
        wi = K.sb(sd, "wi", [128, NT, 8], F32)
        K.op("pool", lambda e: e.memset(Vaug[:, :, :, 64:65], 1.0), [], [Vaug])
        with K.scope() as sw:
            xT = make_xT(sw)
            wd = K.sb(sw, "wd", [128, 8, 1224], BF16)
            K.dma("sp", wd[:], winb[:, 0:1224].rearrange("(k p) c -> p k c", p=128), [], [wd])
            wki2 = K.sb(sw, "wki2", [128, 8, 128], BF16)
            K.op("dve", lambda e: e.tensor_copy(out=wki2[:, :, 0:64], in_=wd[:, :, O_KI:O_KI + 64]), [wd], [wki2])
            K.op("pool", lambda e: e.tensor_copy(out=wki2[:, :, 64:128], in_=wd[:, :, O_KI:O_KI + 64]), [wd], [wki2])
            qaT = K.sb(sw, "qaT", [128, 4, T], BF16)
            ckv = K.sb(sw, "ckv", [128, NT, 128], F32)
            cb = K.sb(sw, "cb", [128, 128], BF16)
            cst_ = K.sb(sw, "cst", [128, 4], F32)
            cjk = K.sb(sw, "cjk", [128, 128], BF16)
            ev = 0
            for tb in range(4):
                tsl = slice(tb * 512, (tb + 1) * 512)
                jobs = [(wd, j * 128, qaT, j) for j in range(4)] + [(wd, O_QI + j * 128, qiT, j) for j in range(4)]
                for wsrc, c0, dst, j in jobs:
                    p = nb()
                    for kc in range(8):
                        K.op("pe", lambda e: e.matmul(p[:, :], lhsT=wsrc[:, kc, c0:c0 + 128], rhs=xT[:, kc, tsl],
                                                      start=(kc == 0), stop=(kc == 7)), [wsrc, xT], [p])
                    if ev % 2:
                        K.op("act", lambda e: e.copy(out=dst[:, j, tsl], in_=p[:, :]), [p], [dst])
                    else:
                        K.op("dve", lambda e: e.tensor_copy(out=dst[:, j, tsl], in_=p[:, :]), [p], [dst])
                    ev += 1
                p = nb()
                for kc in range(8):
                    K.op("pe", lambda e: e.matmul(p[:, :], lhsT=wki2[:, kc, :], rhs=xT[:, kc, tsl],
                                                  start=(kc == 0), stop=(kc == 7)), [wki2, xT], [p])
                K.op("act", lambda e: e.copy(out=kiT2[:, tsl], in_=p[:, :]), [p], [kiT2])
            for i in range(NT):
                isl = slice(i * 128, (i + 1) * 128)
                p = nb()
                for kc in range(8):
                    K.op("pe", lambda e: e.matmul(p[:, 0:128], lhsT=xT[:, kc, isl], rhs=wd[:, kc, O_CKV:O_CKV + 128],
                                                  start=(kc == 0), stop=(kc == 7)), [wd, xT], [p])
                for kc in range(8):
                    K.op("pe", lambda e: e.matmul(p[:, 128:136], lhsT=xT[:, kc, isl], rhs=wd[:, kc, O_WI:O_WI + 8],
                                                  start=(kc == 0), stop=(kc == 7)), [wd, xT], [p])
                K.op("dve", lambda e: e.tensor_copy(out=ckv[:, i, :], in_=p[:, 0:128]), [p], [ckv])
                K.op("dve", lambda e: e.tensor_copy(out=wi[:, i, :], in_=p[:, 128:136]), [p], [wi])
                K.op("act", lambda e: e.activation(out=cjk[:], in_=ckv[:, i, :], func=AF.Square,
                                                   accum_out=cst_[:, 0:1]), [ckv], [cjk, cst_])
                K.op("act", lambda e: e.activation(out=cst_[:, 1:2], in_=cst_[:, 0:1], func=AF.Sqrt,
                                                   bias=epsc[:, 0:1], scale=1.0 / 128), [cst_, epsc], [cst_])
                K.op("dve", lambda e: e.reciprocal(out=cst_[:, 1:2], in_=cst_[:, 1:2]), [cst_], [cst_])
                K.op("dve", lambda e: e.scalar_tensor_tensor(out=cb[:], in0=ckv[:, i, :], scalar=cst_[:, 1:2],
                                                             in1=cnB[:], op0=ALU.mult, op1=ALU.mult),
                     [ckv, cst_, cnB], [cb])
                p2 = nb()
                K.op("pe", lambda e: e.transpose(out=bfv(p2)[:, 0:128], in_=cb[:], identity=identb[:]),
                     [cb, identb], [p2])
                K.op("act", lambda e: e.copy(out=cT[:, isl], in_=bfv(p2)[:, 0:128]), [p2], [cT])
                p3 = nb()
                K.op("pe", lambda e: e.matmul(p3[:, :], lhsT=cT[:, isl], rhs=wuvb[:], start=True, stop=True),
                     [cT, wuvb], [p3])
                K.op("dve", lambda e: e.tensor_copy(out=Vaug[:, i, :, 0:64],
                                                    in_=p3[:, :].rearrange("p (h d) -> p h d", h=8)), [p3], [Vaug])
            for tb in range(4):
                tsl = slice(tb * 512, (tb + 1) * 512)
                for h in range(8):
                    j, hf = h // 2, h % 2
                    psl = slice(hf * 64, (hf + 1) * 64)
                    p = nb()
                    K.op("pe", lambda e: e.matmul(p[:, :], lhsT=wukT[psl, j, :], rhs=qaT[psl, j, tsl],
                                                  start=True, stop=True), [wukT, qaT], [p])
                    K.op("act", lambda e: e.activation(out=qabsT[:, h, tsl], in_=p[:, :], func=AF.Copy, scale=0.125),
                         [p], [qabsT])
        dump("cT", cT[:, 0:256], [cT], [128, 256], BF16)
        dump("qabsT", qabsT[:, :, 0:128], [qabsT], [128, 8, 128], BF16)

        with K.scope() as sq:
            score = K.sb(sq, "score", [128, T], F32)
            relu = [K.sb(sq, "relu", [128, 512], BF16) for _ in range(4)]
            diagw = K.sb(sq, "diagw", [128, 8, 128], BF16)
            cjunk = K.sb(sq, "cjunk", [128, T], BF16)
            bs = K.sb(sq, "bs", [128, 8], F32)
            wtab = K.sb(sq, "wtab", [128, NBIS], F32)
            sel = K.sb(sq, "sel", [128, T], BF16)
            negselT = K.sb(sq, "negselT", [128, NT, 128], BF16)
            Pm = [K.sb(sq, "Pm", [128, 512], BF16) for _ in range(3)]
            oa = K.sb(sq, "oa", [128, 8, 65], F32)
            rden = K.sb(sq, "rden", [128, 8], F32)
            oab = K.sb(sq, "oab", [128, 8, 64], BF16)
            WSC = (8.0 ** -0.5) * (64.0 ** -0.5)
            pmi = 0
            import os as _os
            def selA(qt):
                qsl = slice(qt * 128, (qt + 1) * 128)
                nkb = qt + 1
                nk = nkb * 128
                for h in range(8):
                    K.op("dve", lambda e: e.tensor_scalar(
                        out=diagw[:, h, :], in0=ident[:], scalar1=wi[:, qt, h:h + 1], scalar2=WSC,
                        op0=ALU.mult, op1=ALU.mult), [ident, wi], [diagw])
                for g4 in range((nkb + 3) // 4):
                    s0 = g4 * 512
                    sn = min(512, nk - s0)
                    psc = PS[6 + g4 % 2]
                    phs = {}

                    def qk(h):
                        j, hf = h // 2, h % 2
                        psl = slice(hf * 64, (hf + 1) * 64)
                        ph = nb()
                        phs[h] = ph
                        K.op("pe", lambda e: e.matmul(ph[:, 0:sn], lhsT=qiT[psl, j, qsl], rhs=kiT2[psl, s0:s0 + sn],
                                                      start=True, stop=True), [qiT, kiT2], [ph])
                    for h in range(3):
                        qk(h)
                    for h in range(8):
                        if h + 3 < 8:
                            qk(h + 3)
                        r = relu[h % 4]
                        ph = phs[h]
                        if h % 2:
                            K.op("act", lambda e: e.activation(out=r[:, 0:sn], in_=ph[:, 0:sn], func=AF.Relu), [ph], [r])
                        else:
                            K.op("dve", lambda e: e.tensor_scalar(out=r[:, 0:sn], in0=ph[:, 0:sn], scalar1=0.0,
                                                                  scalar2=None, op0=ALU.max), [ph], [r])
                        K.op("pe", lambda e: e.matmul(psc[:, 0:sn], lhsT=diagw[:, h, :], rhs=r[:, 0:sn],
                                                      start=(h == 0), stop=(h == 7)), [diagw, r], [psc])
                    K.op("act", lambda e: e.copy(out=score[:, s0:s0 + sn], in_=psc[:, 0:sn]), [psc], [score])
                K.op("dve", lambda e: e.tensor_reduce(out=bs[:, 0:1], in_=score[:, 0:nk], axis=AX.X, op=ALU.max,
                                                      apply_absolute_value=True), [score], [bs])
                K.op("dve", lambda e: e.tensor_scalar(out=bs[:, 0:1], in0=bs[:, 0:1], scalar1=1.0001, scalar2=1e-6,
                                                      op0=ALU.mult, op1=ALU.add), [bs], [bs])
                K.op("dve", lambda e: e.tensor_tensor(out=score[:, nk - 128:nk], in0=score[:, nk - 128:nk],
                                                      in1=negdiag[:], op=ALU.add), [score, negdiag], [score])
                K.op("dve", lambda e: e.tensor_scalar(out=wtab[:], in0=pow2[:], scalar1=bs[:, 0:1], scalar2=None,
                                                      op0=ALU.mult), [pow2, bs], [wtab])
                K.op("dve", lambda e: e.memset(bs[:, 1:2], 0.0), [], [bs])
                for it in range(NBIS):
                    K.op("dve", lambda e: e.tensor_scalar(out=cjunk[:, 0:nk], in0=score[:, 0:nk], scalar1=bs[:, 1:2],
                                                          scalar2=0.0, op0=ALU.is_ge, op1=ALU.add,
                                                          accum_out=bs[:, 2:3]), [score, bs], [cjunk, bs])
                    K.op("dve", lambda e: e.tensor_scalar(out=bs[:, 3:4], in0=bs[:, 2:3], scalar1=kreq[:, qt:qt + 1],
                                                          scalar2=0.5, op0=ALU.is_ge, op1=ALU.subtract),
                         [bs, kreq], [bs])
                    K.op("dve", lambda e: e.scalar_tensor_tensor(out=bs[:, 1:2], in0=bs[:, 3:4],
                                                                 scalar=wtab[:, it:it + 1], in1=bs[:, 1:2],
                                                                 op0=ALU.mult, op1=ALU.add), [bs, wtab], [bs])
                K.op("dve", lambda e: e.scalar_tensor_tensor(out=bs[:, 4:5], in0=wtab[:, NBIS - 1:NBIS], scalar=-0.5,
                                                             in1=bs[:, 1:2], op0=ALU.mult, op1=ALU.add),
                     [bs, wtab], [bs])
                K.op("dve", lambda e: e.tensor_scalar(out=sel[:, 0:nk], in0=score[:, 0:nk], scalar1=bs[:, 4:5],
                                                      scalar2=None, op0=ALU.is_ge), [score, bs], [sel])
                if qt == 3:
                    dump("sel3", sel[:, 0:512], [sel], [128, 512], BF16)
                    dump("score3", score[:, 0:512], [score], [128, 512])
            def selB(qt):
                nkb = qt + 1
                for k0 in range(0, nkb, 8):
                    kn = min(8, nkb - k0)
                    pT = nb()
                    for kk in range(kn):
                        K.op("pe", lambda e: e.transpose(out=bfv(pT)[:, kk * 128:(kk + 1) * 128],
                                                         in_=sel[:, (k0 + kk) * 128:(k0 + kk + 1) * 128],
                                                         identity=identb[:]), [sel, identb], [pT])
                    K.op("dve", lambda e: e.tensor_scalar(
                        out=negselT[:, k0:k0 + kn, :].rearrange("p k t -> p (k t)"), in0=bfv(pT)[:, 0:kn * 128],
                        scalar1=-1.0, scalar2=-NEG, op0=ALU.add, op1=ALU.mult), [pT], [negselT])
            def att(qt):
                nonlocal pmi
                qsl = slice(qt * 128, (qt + 1) * 128)
                nkb = qt + 1
                poA, poB = PS[6], PS[7]
                for h in range(8):
                    po = poA if h < 4 else poB
                    hh = h % 4
                    for g4 in range((nkb + 3) // 4):
                        kbs = list(range(g4 * 4, min(nkb, g4 * 4 + 4)))
                        n = len(kbs)
                        ps_ = nb()
                        for idx, kb in enumerate(kbs):
                            reg = ps_[:, idx * 128:(idx + 1) * 128]
                            near = kb >= qt - 1
                            K.op("pe", lambda e: e.matmul(reg, lhsT=cT[:, kb * 128:(kb + 1) * 128], rhs=qabsT[:, h, qsl],
                                                          start=True, stop=False), [cT, qabsT], [ps_])
                            K.op("pe", lambda e: e.matmul(reg, lhsT=identb[:], rhs=negselT[:, kb, :],
                                                          start=False, stop=(not near)), [identb, negselT], [ps_])
                            if near:
                                u0 = 0 if kb == qt else 128
                                K.op("pe", lambda e: e.matmul(reg, lhsT=identb[:], rhs=Tb[:, h, u0:u0 + 128],
                                                              start=False, stop=True), [identb, Tb], [ps_])
                        pm = Pm[pmi % 3]
                        pmi += 1
                        K.op("act", lambda e: e.activation(out=pm[:, 0:n * 128], in_=ps_[:, 0:n * 128], func=AF.Exp,
                                                           bias=b15B[:, h:h + 1], scale=1.0), [ps_, b15B], [pm])
                        for idx, kb in enumerate(kbs):
                            K.op("pe", lambda e: e.matmul(po[:, hh * 65:(hh + 1) * 65],
                                                          lhsT=pm[:, idx * 128:(idx + 1) * 128], rhs=Vaug[:, kb, h, :],
                                                          start=(kb == 0), stop=(kb == nkb - 1)), [pm, Vaug], [po])
                K.op("act", lambda e: e.copy(out=oa[:, 0:4, :].rearrange("p h d -> p (h d)"), in_=poA[:, 0:260]),
                     [poA], [oa])
                K.op("dve", lambda e: e.tensor_copy(out=oa[:, 4:8, :].rearrange("p h d -> p (h d)"), in_=poB[:, 0:260]),
                     [poB], [oa])
                K.op("dve", lambda e: e.reciprocal(out=rden[:], in_=oa[:, :, 64]), [oa], [rden])
                K.op("dve", lambda e: e.tensor_tensor(out=oab[:], in0=oa[:, :, 0:64], in1=bc_last(rden[:, :], 64),
                                                      op=ALU.mult), [oa, rden], [oab])
                pX = nb()
                for j in range(4):
                    K.op("pe", lambda e: e.transpose(out=bfv(pX)[:, j * 128:(j + 1) * 128],
                                                     in_=oab[:, 2 * j:2 * j + 2, :].rearrange("p h d -> p (h d)"),
                                                     identity=identb[:]), [oab, identb], [pX])
                K.op("act", lambda e: e.copy(out=mixT[:, 0:4, qsl],
                                             in_=bfv(pX)[:, 0:512].rearrange("p (k t) -> p k t", k=4)), [pX], [mixT])
            NQ = int(_os.environ.get("DSA_NQT", NT))
            selA(0)
            selB(0)
            for qt in range(NQ):
                if qt + 1 < NQ:
                    selA(qt + 1)
                att(qt)
                if qt + 1 < NQ:
                    selB(qt + 1)
        dump("oa", mixT[:, 0:4, 0:512], [mixT], [128, 4, 512], BF16)


def _mlp(nc, K, ss, g, dbg, stop_after, dump, seq, L, mixT):
    PS = L["PS"]
    identb = L["identb"]; epsc = L["epsc"]; g2B = L["g2B"]; g4B = L["g4B"]
    woutb, w1b, w2b = g["woutb"], g["w1b"], g["w2b"]
    x = g["x"]; out = g["out"]
    tok0 = seq * T

    def bfv(p):
        return p.t[:].bitcast(BF16)

    with K.scope() as sm:
        wo = K.sb(sm, "wo", [128, 8, D], BF16)
        K.dma("sp", wo[:], woutb.rearrange("(k p) c -> p k c", p=128), [], [wo])
        x1 = K.sb(sm, "x1", [128, 4, D], F32)
        xr = [K.sb(sm, "xr", [128, D], F32) for _ in range(2)]
        xnb = K.sb(sm, "xnb", [128, D], BF16)
        xnT = K.sb(sm, "xnT", [128, 8, 512], BF16)
        hT = K.sb(sm, "hT", [128, 32, 512], BF16)
        w1t = [K.sb(sm, "w1t", [128, 8, 512], BF16) for _ in range(2)]
        w2t = [K.sb(sm, "w2t", [128, 4, D], BF16) for _ in range(2)]
        r32 = [K.sb(sm, "r32", [128, 512], F32) for _ in range(2)]
        yt = K.sb(sm, "yt", [128, D], F32)
        junk = K.sb(sm, "mjunk", [128, 512], BF16)
        st = K.sb(sm, "mst", [128, 8], F32)

        def rms_from_psum(pA, pB, col):
            K.op("act", lambda e: e.activation(out=junk[:], in_=pA[:, :], func=AF.Square, accum_out=st[:, 0:1]),
                 [pA], [junk, st])
            K.op("act", lambda e: e.activation(out=junk[:], in_=pB[:, :], func=AF.Square, accum_out=st[:, 1:2]),
                 [pB], [junk, st])
            K.op("dve", lambda e: e.tensor_tensor(out=st[:, 2:3], in0=st[:, 0:1], in1=st[:, 1:2], op=ALU.add),
                 [st], [st])
            K.op("act", lambda e: e.activation(out=st[:, col:col + 1], in_=st[:, 2:3], func=AF.Sqrt,
                                               bias=epsc[:, 0:1], scale=1.0 / D), [st, epsc], [st])
            K.op("dve", lambda e: e.reciprocal(out=st[:, col:col + 1], in_=st[:, col:col + 1]), [st], [st])

        cnt = 0
        for tb in range(4):
            for ti in range(4):
                i = tb * 4 + ti
                xri = xr[i % 2]
                K.dma("sp", xri[:], x[tok0 + i * 128: tok0 + (i + 1) * 128, :], [], [xri])
                pA, pB = PS[2 * (ti % 2)], PS[2 * (ti % 2) + 1]
                for half, p in enumerate((pA, pB)):
                    for kc in range(8):
                        K.op("pe", lambda e: e.matmul(p[:, :], lhsT=mixT[:, kc, i * 128:(i + 1) * 128],
                                                      rhs=wo[:, kc, half * 512:(half + 1) * 512],
                                                      start=(kc == 0), stop=(kc == 7)), [mixT, wo], [p])
                rms_from_psum(pA, pB, 3)
                for half, p in enumerate((pA, pB)):
                    K.op("dve", lambda e: e.scalar_tensor_tensor(
                        out=x1[:, ti, half * 512:(half + 1) * 512], in0=p[:, :], scalar=st[:, 3:4],
                        in1=g2B[:, half * 512:(half + 1) * 512], op0=ALU.mult, op1=ALU.mult), [p, st, g2B], [x1])
                K.op("dve", lambda e: e.tensor_tensor(out=x1[:, ti, :], in0=x1[:, ti, :], in1=xri[:], op=ALU.add),
                     [x1, xri], [x1])
                K.op("act", lambda e: e.activation(out=xnb[:], in_=x1[:, ti, :], func=AF.Square,
                                                   accum_out=st[:, 4:5]), [x1], [xnb, st])
                K.op("act", lambda e: e.activation(out=st[:, 5:6], in_=st[:, 4:5], func=AF.Sqrt, bias=epsc[:, 0:1],
                                                   scale=1.0 / D), [st, epsc], [st])
                K.op("dve", lambda e: e.reciprocal(out=st[:, 5:6], in_=st[:, 5:6]), [st], [st])
                K.op("dve", lambda e: e.tensor_scalar(out=xnb[:], in0=x1[:, ti, :], scalar1=st[:, 5:6], scalar2=None,
                                                      op0=ALU.mult), [x1, st], [xnb])
                pt = PS[4 + (ti % 2)]
                for kc in range(8):
                    K.op("pe", lambda e: e.transpose(out=bfv(pt)[:, kc * 128:(kc + 1) * 128],
                                                     in_=xnb[:, kc * 128:(kc + 1) * 128], identity=identb[:]),
                         [xnb, identb], [pt])
                K.op("act", lambda e: e.copy(out=xnT[:, :, ti * 128:(ti + 1) * 128],
                                             in_=bfv(pt).rearrange("p (k t) -> p k t", k=8)), [pt], [xnT])
            for fb in range(8):
                wt = w1t[fb % 2]
                K.dma("sp", wt[:], w1b[:, fb * 512:(fb + 1) * 512].rearrange("(k p) c -> p k c", p=128), [], [wt])
                for fc in range(4):
                    p = PS[cnt % 8]
                    r = r32[cnt % 2]
                    for kc in range(8):
                        K.op("pe", lambda e: e.matmul(p[:, :], lhsT=wt[:, kc, fc * 128:(fc + 1) * 128],
                                                      rhs=xnT[:, kc, :], start=(kc == 0), stop=(kc == 7)),
                             [wt, xnT], [p])
                    K.op("act", lambda e: e.activation(out=r[:], in_=p[:, :], func=AF.Relu), [p], [r])
                    K.op("dve", lambda e: e.tensor_tensor(
                        out=hT[:, fb * 4 + fc, :], in0=r[:], in1=r[:], op=ALU.mult), [r], [hT])
                    cnt += 1
            for fg in range(8):
                wt2 = w2t[fg % 2]
                K.dma("sp", wt2[:], w2b[fg * 512:(fg + 1) * 512, :].rearrange("(c p) n -> p c n", p=128), [], [wt2])
                for c4 in range(4):
                    fc = fg * 4 + c4
                    for ti in range(4):
                        for half in range(2):
                            p = PS[ti * 2 + half]
                            K.op("pe", lambda e: e.matmul(p[:, :], lhsT=hT[:, fc, ti * 128:(ti + 1) * 128],
                                                          rhs=wt2[:, c4, half * 512:(half + 1) * 512],
                                                          start=(fc == 0), stop=(fc == 31)), [hT, wt2], [p])
            for ti in range(4):
                i = tb * 4 + ti
                pA, pB = PS[ti * 2], PS[ti * 2 + 1]
                rms_from_psum(pA, pB, 6)
                for half, p in enumerate((pA, pB)):
                    K.op("dve", lambda e: e.scalar_tensor_tensor(
                        out=yt[:, half * 512:(half + 1) * 512], in0=p[:, :], scalar=st[:, 6:7],
                        in1=g4B[:, half * 512:(half + 1) * 512], op0=ALU.mult, op1=ALU.mult), [p, st, g4B], [yt])
                K.op("dve", lambda e: e.tensor_tensor(out=yt[:], in0=yt[:], in1=x1[:, ti, :], op=ALU.add),
                     [yt, x1], [yt])
                K.dma("sp", out[tok0 + i * 128: tok0 + (i + 1) * 128, :], yt[:], [yt], [Dep()])


INPUT_NAMES = ["x", "w_in", "c_norm", "w_uk", "w_uv", "rel_bias", "conv_w", "a_log", "dt_bias", "o_norm",
               "w_out", "pre_norm_mix", "post_norm_mix", "pre_norm_mlp", "post_norm_mlp", "w_mlp_in", "w_mlp_out"]


def make_in_maps(inputs, n_cores=8):
    f = lambda a: np.ascontiguousarray(np.asarray(a, dtype=np.float32))
    shared = {
        "w_in": f(inputs["w_in"])[0], "c_norm": f(inputs["c_norm"])[0], "w_uk": f(inputs["w_uk"])[0],
        "w_uv": f(inputs["w_uv"])[0], "rel_bias": f(inputs["rel_bias"]), "conv_w": f(inputs["conv_w"])[0],
        "a_log": f(inputs["a_log"])[0], "dt_bias": f(inputs["dt_bias"])[0], "o_norm": f(inputs["o_norm"])[0],
        "w_out": f(inputs["w_out"])[0], "pre_norm_mix": f(inputs["pre_norm_mix"])[0],
        "post_norm_mix": f(inputs["post_norm_mix"])[0], "pre_norm_mlp": f(inputs["pre_norm_mlp"])[0],
        "post_norm_mlp": f(inputs["post_norm_mlp"])[0], "w_mlp_in": f(inputs["w_mlp_in"])[0],
        "w_mlp_out": f(inputs["w_mlp_out"])[0], "ohr": onehot_rev(),
    }
    xs = f(inputs["x"])
    maps = []
    for c in range(n_cores):
        m = dict(shared)
        m["x"] = np.ascontiguousarray(xs[c * NSEQ:(c + 1) * NSEQ].reshape(NSEQ * T, D))
        maps.append(m)
    return maps


def kernel(**inputs):
    nc = build_nc()
    maps = make_in_maps(inputs)
    res = run_bass_kernel_spmd(nc, maps, core_ids=list(range(8)))
    outs = [np.asarray(r["out"], dtype=np.float32).reshape(NSEQ, T, D) for r in res.results]
    return np.concatenate(outs, axis=0)
```

```python
import numpy as np
from contextlib import ExitStack
import concourse.bass as bass
import concourse.mybir as mybir
from concourse.bass_utils import run_bass_kernel_spmd

F32 = mybir.dt.float32
BF16 = mybir.dt.bfloat16
AF = mybir.ActivationFunctionType
ALU = mybir.AluOpType
AX = mybir.AxisListType

D = 1024
T = 2048
NSEQ = 2
NT = T // 128
INC = 3280
DFF = 4096
EPS = 1e-6
O_QA, O_CKV, O_QI, O_KI, O_WI, O_QB, O_KB, O_VB, O_AB, O_BB, O_ZB = (
    0, 512, 640, 1152, 1216, 1224, 1736, 2248, 2760, 2764, 2768)
NBIS = 16
TOPK = 256
NEG = -30000.0


class Dep:
    __slots__ = ("w", "r", "x")

    def __init__(self):
        self.w = None
        self.r = []
        self.x = False


class Tl:
    def __init__(self, t, nslots=0):
        self.t = t
        self.d = Dep()
        self.s = [Dep() for _ in range(nslots)]

    def __getitem__(self, idx):
        return self.t[idx]


class KB:
    def __init__(self, nc, es):
        self.nc = nc
        self.es = es
        self.engs = {"pe": nc.tensor, "dve": nc.vector, "act": nc.scalar,
                     "pool": nc.gpsimd, "sp": nc.sync}
        self.sems = {}
        self.cnt = {}
        for n in ("pe", "dve", "act", "pool"):
            self.sems[n] = es.enter_context(nc.semaphore("sem_" + n))
            self.cnt[n] = 0
        self.waited = {n: {} for n in self.engs}
        self.dq = {}
        for q, n in (("sp", 20), ("act", 8), ("pool", 8)):
            lst = []
            for i in range(n):
                nm = "dma_%s_%d" % (q, i)
                self.sems[nm] = es.enter_context(nc.semaphore(nm))
                self.cnt[nm] = 0
                lst.append(nm)
            self.dq[q] = [lst, 0]
        self.uid = 0
        self.fence = []
        self.limit = None
        self.hook = None

    def fence_now(self):
        self.fence = [(k, v) for k, v in self.cnt.items() if v > 0]

    def scope(self):
        kb = self

        class _Scope(ExitStack):
            def __exit__(self, *a):
                r = ExitStack.__exit__(self, *a)
                kb.fence_now()
                return r
        return _Scope()

    def sb(self, es, name, shape, dt, nslots=0):
        self.uid += 1
        t = Tl(es.enter_context(self.nc.sbuf_tensor("%s_%d" % (name, self.uid), shape, dt)), nslots)
        t.d.r = list(self.fence)
        for d in t.s:
            d.r = list(self.fence)
        return t

    def ps(self, es, name, shape, dt):
        self.uid += 1
        t = Tl(es.enter_context(self.nc.psum_tensor("%s_%d" % (name, self.uid), shape, dt)))
        t.d.x = True
        return t

    def _deps(self, eng, reads, writes):
        need = {}

        def add(p):
            if p is None:
                return
            s, v = p
            if need.get(s, 0) < v:
                need[s] = v

        for d in reads:
            add(d.w)
            if d.x:
                for p in d.r:
                    if p[0] != eng:
                        add(p)
        for d in writes:
            if d.w is not None and not (eng == "pe" and d.w[0] == "pe"):
                add(d.w)
            for p in d.r:
                add(p)
        e = self.engs[eng]
        wd = self.waited[eng]
        for s, v in need.items():
            if wd.get(s, 0) < v:
                e.wait_ge(self.sems[s], v)
                wd[s] = v

    def _norm(self, lst):
        out = []
        for x in lst:
            out.append(x.d if isinstance(x, Tl) else x)
        return out

    def _mark(self, me, reads, writes):
        for d in reads:
            d.r = [p for p in d.r if p[0] != me[0]]
            d.r.append(me)
        for d in writes:
            d.w = me
            d.r = []

    def interleave(self, fns):
        import threading
        n = len(fns)
        cv = threading.Condition()
        state = {"turn": 0, "done": [False] * n, "err": None}
        tls = threading.local()

        def nxt(i):
            for k in range(1, n + 1):
                j = (i + k) % n
                if not state["done"][j]:
                    return j
            return -1

        def hook():
            i = getattr(tls, "idx", None)
            if i is None:
                return
            with cv:
                j = nxt(i)
                if j != i and j >= 0:
                    state["turn"] = j
                    cv.notify_all()
                    while state["turn"] != i:
                        cv.wait()

        def runner(i):
            tls.idx = i
            with cv:
                while state["turn"] != i:
                    cv.wait()
            try:
                fns[i]()
            except BaseException as ex:
                state["err"] = ex
            finally:
                with cv:
                    state["done"][i] = True
                    state["turn"] = nxt(i)
                    cv.notify_all()

        self.hook = hook
        ths = [threading.Thread(target=runner, args=(i,)) for i in range(n)]
        for t in ths:
            t.start()
        for t in ths:
            t.join()
        self.hook = None
        if state["err"] is not None:
            raise state["err"]

    def op(self, eng, fn, reads=(), writes=()):
        if self.hook is not None:
            self.hook()
        if self.limit is not None:
            if self.limit <= 0:
                return None
            self.limit -= 1
            if self.limit == 0:
                import traceback
                print("LAST OP:", eng, traceback.extract_stack()[-2].lineno)
        reads = self._norm(reads)
        writes = self._norm(writes)
        self._deps(eng, reads, writes)
        inst = fn(self.engs[eng])
        self.cnt[eng] += 1
        inst.then_inc(self.sems[eng], 1)
        self._mark((eng, self.cnt[eng]), reads, writes)
        return inst

    def dma(self, q, out, in_, reads=(), writes=(), nc_ok=False):
        reads = self._norm(reads)
        writes = self._norm(writes)
        lst, i = self.dq[q]
        sn = lst[i % len(lst)]
        self.dq[q][1] = i + 1
        e = self.engs[q]
        wd = self.waited[q]
        if wd.get(sn, 0) < self.cnt[sn]:
            e.wait_ge(self.sems[sn], self.cnt[sn])
            wd[sn] = self.cnt[sn]
        self._deps(q, reads, writes)
        if nc_ok:
            with self.nc.allow_non_contiguous_dma(reason="small strided"):
                inst = e.dma_start(out=out, in_=in_)
        else:
            inst = e.dma_start(out=out, in_=in_)
        self.cnt[sn] += 16
        inst.then_inc(self.sems[sn], 16)
        self._mark((sn, self.cnt[sn]), reads, writes)
        return inst

    def wait_all(self, eng, deps):
        deps = self._norm(deps)
        self._deps(eng, deps, [])


def bcast_row(ap_1d_dram, nparts, n):
    return bass.AP(tensor=ap_1d_dram.tensor, offset=ap_1d_dram.offset, ap=[[0, nparts], [1, n]])


def t5_bucket_np(rel):
    nb = 16
    max_exact = 8
    side = np.where(rel > 0, nb, 0)
    n = np.abs(rel)
    nf = np.maximum(n, 1).astype(np.float32)
    large = max_exact + (np.log(nf / max_exact) / np.log(np.float32(128 / max_exact))
                         * (nb - max_exact)).astype(np.int32)
    large = np.minimum(large, nb - 1)
    return side + np.where(n < max_exact, n, large)


def onehot_rev():
    j = np.arange(384)
    dd = 127 - j
    b = t5_bucket_np(dd)
    oh = np.zeros((32, 384), np.float32)
    oh[b, j] = 1.0
    return oh


def build_nc(dbg=(), stop_after=None):
    nc = bass.Bass("TRN2", target_bir_lowering=False)

    def din(name, shape, dt=F32):
        return nc.dram_tensor(name, list(shape), dt, kind="ExternalInput").ap()

    x = din("x", [NSEQ * T, D])
    w_in = din("w_in", [D, INC])
    c_norm = din("c_norm", [128])
    w_uk = din("w_uk", [8, 128, 64])
    w_uv = din("w_uv", [8, 128, 64])
    rel_bias = din("rel_bias", [32, 8])
    conv_w = din("conv_w", [4, 1536])
    a_log = din("a_log", [4])
    dt_bias = din("dt_bias", [4])
    o_norm = din("o_norm", [128])
    w_out = din("w_out", [D, D])
    g_pre_mix = din("pre_norm_mix", [D])
    g_post_mix = din("post_norm_mix", [D])
    g_pre_mlp = din("pre_norm_mlp", [D])
    g_post_mlp = din("post_norm_mlp", [D])
    w1 = din("w_mlp_in", [D, DFF])
    w2 = din("w_mlp_out", [DFF, D])
    ohr = din("ohr", [32, 384])
    out = nc.dram_tensor("out", [NSEQ * T, D], F32, kind="ExternalOutput").ap()

    def dscr(name, shape, dt):
        return nc.dram_tensor(name, list(shape), dt, kind="Internal").ap()

    winb = dscr("winb", [D, INC], BF16)
    woutb = dscr("woutb", [D, D], BF16)
    w1b = dscr("w1b", [D, DFF], BF16)
    w2b = dscr("w2b", [DFF, D], BF16)
    vscr = dscr("vscr", [8, 384], F32)

    dbg_out = {}

    def dbg_tensor(name, shape, dt=F32):
        if name in dbg:
            dbg_out[name] = nc.dram_tensor("dbg_" + name, list(shape), dt, kind="ExternalOutput").ap()
            return dbg_out[name]
        return None

    with ExitStack() as es:
        K = KB(nc, es)
        _build(nc, K, es, locals(), dbg, stop_after, dbg_tensor)
    return nc


class Stop(Exception):
    pass


def _build(nc, K, es, g, dbg, stop_after, dbg_tensor):
    x = g["x"]; out = g["out"]
    PS = [K.ps(es, "ps%d" % i, [128, 512], F32) for i in range(8)]

    def psbf(i):
        return PS[i].t[:].bitcast(BF16)

    cst = lambda name, shape, dt=F32: K.sb(es, name, shape, dt)
    dif = cst("dif", [128, 128])
    K.op("pool", lambda e: e.iota(dif[:], pattern=[[1, 128]], base=0, channel_multiplier=-1,
                                  allow_small_or_imprecise_dtypes=True), [], [dif])
    ident = cst("ident", [128, 128])
    identb = cst("identb", [128, 128], BF16)
    K.op("dve", lambda e: e.tensor_scalar(out=ident[:], in0=dif[:], scalar1=0.0, scalar2=None,
                                          op0=ALU.is_equal), [dif], [ident])
    K.op("dve", lambda e: e.tensor_copy(out=identb[:], in_=ident[:]), [ident], [identb])
    ones = cst("ones", [128, 128])
    K.op("dve", lambda e: e.memset(ones[:], 1.0), [], [ones])
    m_le = cst("m_le", [64, 64])
    m_lt = cst("m_lt", [64, 64])
    m_ge = cst("m_ge", [64, 64])
    m_gt = cst("m_gt", [64, 64])
    for m, opx in ((m_le, ALU.is_le), (m_lt, ALU.is_lt), (m_ge, ALU.is_ge), (m_gt, ALU.is_gt)):
        K.op("dve", lambda e, m=m, opx=opx: e.tensor_scalar(out=m[:], in0=dif[0:64, 0:64], scalar1=0.0,
                                                            scalar2=None, op0=opx), [dif], [m])
    cm2 = cst("cm2", [128, 64])
    cmr2 = cst("cmr2", [128, 64])
    K.op("dve", lambda e: e.tensor_scalar(out=cm2[0:64, :], in0=dif[0:64, 0:64], scalar1=0.0, scalar2=None,
                                          op0=ALU.is_ge), [dif], [cm2])
    K.op("dve", lambda e: e.tensor_scalar(out=cm2[64:128, :], in0=dif[64:128, 64:128], scalar1=0.0,
                                          scalar2=None, op0=ALU.is_ge), [dif], [cm2])
    K.op("dve", lambda e: e.tensor_scalar(out=cmr2[0:64, :], in0=dif[0:64, 0:64], scalar1=0.0, scalar2=None,
                                          op0=ALU.is_lt), [dif], [cmr2])
    K.op("dve", lambda e: e.tensor_scalar(out=cmr2[64:128, :], in0=dif[64:128, 64:128], scalar1=0.0,
                                          scalar2=None, op0=ALU.is_lt), [dif], [cmr2])
    negdiag = cst("negdiag", [128, 128])
    K.op("dve", lambda e: e.memset(negdiag[:], 0.0), [], [negdiag])
    K.op("dve", lambda e: e.memset(negdiag[0:64, 64:128], -1e30), [], [negdiag])
    kreq = cst("kreq", [128, NT])
    for qt in range(NT):
        for hf in range(2):
            lim = qt * 128 + (hf + 1) * 64
            K.op("dve", lambda e, qt=qt, hf=hf, lim=lim: e.memset(
                kreq[hf * 64:(hf + 1) * 64, qt:qt + 1], float(min(TOPK, lim))), [], [kreq])
    pow2 = cst("pow2", [128, NBIS])
    for i in range(NBIS):
        K.op("pool", lambda e, i=i: e.memset(pow2[:, i:i + 1], float(2.0 ** (-i))), [], [pow2])
    epsc = cst("epsc", [128, 1])
    K.op("dve", lambda e: e.memset(epsc[:], EPS), [], [epsc])
    onec = cst("onec", [128, 1])
    K.op("dve", lambda e: e.memset(onec[:], 1.0), [], [onec])

    def gT(vec, name):
        t = cst(name, [128, 8])
        K.dma("sp", t[:], vec.rearrange("(k p) -> p k", p=128), [], [t], nc_ok=True)
        return t
    g1T = gT(g["g_pre_mix"], "g1T")
    g3T = gT(g["g_pre_mlp"], "g3T")

    def gB(vec, n, name, parts=128):
        t = cst(name, [parts, n])
        K.dma("sp", t[:], bcast_row(vec, parts, n), [], [t], nc_ok=True)
        return t
    g2B = gB(g["g_post_mix"], D, "g2B")
    g4B = gB(g["g_post_mlp"], D, "g4B")
    cnB = gB(g["c_norm"], 128, "cnB")
    onB = gB(g["o_norm"], 128, "onB", 64)
    alB = gB(g["a_log"], 4, "alB", 64)
    dtB = gB(g["dt_bias"], 4, "dtB", 64)
    b15B = gB(g["rel_bias"][15, :], 8, "b15B")
    negA = cst("negA", [64, 4])
    K.op("act", lambda e: e.activation(out=negA[:], in_=alB[:], func=AF.Exp), [alB], [negA])
    K.op("dve", lambda e: e.tensor_scalar(out=negA[:], in0=negA[:], scalar1=-1.0, scalar2=None, op0=ALU.mult),
         [negA], [negA])
    cw = cst("cw", [128, 12, 4])
    for j in range(4):
        K.dma("sp", cw[:, :, j], g["conv_w"][j, :].rearrange("(k p) -> p k", p=128), [], [cw], nc_ok=True)

    wukT = cst("wukT", [128, 4, 128], BF16)
    wuvb = cst("wuvb", [128, 512], BF16)
    Tb = cst("Tb", [128, 8, 256], BF16)
    with K.scope() as es2:
        tmpk = K.sb(es2, "tmpk", [128, 8, 64], F32)
        tmpv = K.sb(es2, "tmpv", [128, 8, 64], F32)
        K.dma("sp", tmpk[:], g["w_uk"].rearrange("h c d -> c h d"), [], [tmpk], nc_ok=True)
        K.dma("sp", tmpv[:], g["w_uv"].rearrange("h c d -> c h d"), [], [tmpv], nc_ok=True)
        K.op("dve", lambda e: e.tensor_copy(out=wuvb[:], in_=tmpv[:].rearrange("p h d -> p (h d)")),
             [tmpv], [wuvb])
        for j in range(4):
            K.op("pe", lambda e, j=j: e.transpose(
                out=PS[0][:, j * 128:(j + 1) * 128],
                in_=tmpk[:, 2 * j:2 * j + 2, :].rearrange("p h d -> p (h d)"), identity=ident[:]),
                [tmpk, ident], [PS[0]])
        K.op("dve", lambda e: e.tensor_copy(out=wukT[:].rearrange("p j c -> p (j c)"), in_=PS[0][:, :]),
             [PS[0]], [wukT])

        rb = K.sb(es2, "rb", [32, 8], F32)
        rbT = K.sb(es2, "rbT", [8, 32], F32)
        oh = K.sb(es2, "oh", [32, 384], F32)
        K.dma("sp", rb[:], g["rel_bias"], [], [rb])
        K.dma("sp", rbT[:], g["rel_bias"].rearrange("b h -> h b"), [], [rbT], nc_ok=True)
        K.dma("sp", oh[:], g["ohr"], [], [oh])
        K.op("pe", lambda e: e.matmul(PS[1][0:8, 0:384], lhsT=rb[:], rhs=oh[:], start=True, stop=True),
             [rb, oh], [PS[1]])
        vr = K.sb(es2, "vr", [8, 384], F32)
        K.op("dve", lambda e: e.tensor_scalar(out=vr[:], in0=PS[1][0:8, 0:384], scalar1=rbT[:, 15:16],
                                              scalar2=None, op0=ALU.subtract), [PS[1], rbT], [vr])
        vs = g["vscr"]
        dvs = Dep()
        K.dma("sp", vs, vr[:], [vr], [dvs])
        Tb32 = K.sb(es2, "Tb32", [128, 8, 256], F32)
        src = bass.AP(tensor=vs.tensor, offset=vs.offset, ap=[[1, 128], [384, 8], [1, 256]])
        K.dma("sp", Tb32[:], src, [dvs], [Tb32], nc_ok=True)
        smt = K.sb(es2, "smt", [128, 128], F32)
        Jm = K.sb(es2, "Jm", [128, 128], F32)
        K.op("pool", lambda e: e.iota(smt[:], pattern=[[1, 128]], base=0, channel_multiplier=1,
                                      allow_small_or_imprecise_dtypes=True), [], [smt])
        K.op("dve", lambda e: e.tensor_scalar(out=Jm[:], in0=smt[:], scalar1=127.0, scalar2=None,
                                              op0=ALU.is_equal), [smt], [Jm])
        Tbf = Tb32[:].rearrange("p h u -> p (h u)")
        for q in range(4):
            K.op("pe", lambda e, q=q: e.matmul(PS[2 + q][:, :], lhsT=Jm[:], rhs=Tbf[:, q * 512:(q + 1) * 512],
                                               start=True, stop=True), [Jm, Tb32], [PS[2 + q]])
            K.op("dve", lambda e, q=q: e.tensor_copy(
                out=Tb[:].rearrange("p h u -> p (h u)")[:, q * 512:(q + 1) * 512], in_=PS[2 + q][:, :]),
                [PS[2 + q]], [Tb])
        d = dbg_tensor("Tb", [128, 8, 256], BF16)
        if d is not None:
            K.dma("sp", d, Tb[:], [Tb], [Dep()])

        stg_deps = {}
        engs_rr = ["dve", "pool", "act"]
        rr = [0]

        def stage(src, dst, nrows, ncols, gT_tile, key):
            dd = Dep()
            stg_deps[key] = dd
            f = [K.sb(es2, "stf", [128, 2048], F32) for _ in range(2)]
            b = [K.sb(es2, "stb", [128, 2048], BF16) for _ in range(2)]
            i = 0
            for kc in range(nrows // 128):
                for c0 in range(0, ncols, 2048):
                    cn = min(2048, ncols - c0)
                    ft, bt = f[i % 2], b[i % 2]
                    K.dma("sp", ft[:, 0:cn], src[kc * 128:(kc + 1) * 128, c0:c0 + cn], [], [ft])
                    eng = engs_rr[rr[0] % 3]
                    rr[0] += 1
                    if gT_tile is not None:
                        if eng == "act":
                            K.op("act", lambda e, ft=ft, bt=bt, cn=cn, kc=kc: e.activation(
                                out=bt[:, 0:cn], in_=ft[:, 0:cn], func=AF.Copy, scale=gT_tile[:, kc:kc + 1]),
                                [ft, gT_tile], [bt])
                        else:
                            K.op(eng, lambda e, ft=ft, bt=bt, cn=cn, kc=kc: e.tensor_scalar(
                                out=bt[:, 0:cn], in0=ft[:, 0:cn], scalar1=gT_tile[:, kc:kc + 1], scalar2=None,
                                op0=ALU.mult), [ft, gT_tile], [bt])
                    else:
                        if eng == "act":
                            K.op("act", lambda e, ft=ft, bt=bt, cn=cn: e.activation(
                                out=bt[:, 0:cn], in_=ft[:, 0:cn], func=AF.Copy), [ft], [bt])
                        else:
                            K.op(eng, lambda e, ft=ft, bt=bt, cn=cn: e.tensor_copy(out=bt[:, 0:cn], in_=ft[:, 0:cn]),
                                 [ft], [bt])
                    K.dma("sp", dst[kc * 128:(kc + 1) * 128, c0:c0 + cn], bt[:, 0:cn], [bt], [Dep()])
                    i += 1

        stage(g["w_in"], g["winb"], D, INC, g1T, "win")
        stage(g["w_out"], g["woutb"], D, D, None, "wout")
        stage(g["w1"], g["w1b"], D, DFF, g3T, "w1")
        stage(g["w2"], g["w2b"], DFF, D, None, "w2")
    def drain_sp():
        for sn in K.dq["sp"][0]:
            for q in ("sp", "act", "pool"):
                if K.waited[q].get(sn, 0) < K.cnt[sn]:
                    K.engs[q].wait_ge(K.sems[sn], K.cnt[sn])
                    K.waited[q][sn] = K.cnt[sn]
    drain_sp()
    if stop_after == "stage":
        return

    for seq in range(NSEQ):
        with K.scope() as ss:
            _seq(nc, K, ss, g, dbg, stop_after, dbg_tensor, seq, locals())
        if stop_after is not None:
            break
    for q in ("sp", "act", "pool"):
        for sn in K.dq[q][0]:
            if K.waited["sp"].get(sn, 0) < K.cnt[sn]:
                K.engs["sp"].wait_ge(K.sems[sn], K.cnt[sn])
                K.waited["sp"][sn] = K.cnt[sn]


def _seq(nc, K, ss, g, dbg, stop_after, dbg_tensor, seq, L):
    PS = L["PS"]; psbf = L["psbf"]
    ident = L["ident"]; identb = L["identb"]; ones = L["ones"]
    epsc = L["epsc"]
    x = g["x"]; out = g["out"]
    tok0 = seq * T
    dbgon = (seq == 0)

    def dump(name, ap_sb, deps, shape, dt=F32):
        if not dbgon:
            return
        d = dbg_tensor(name, shape, dt)
        if d is not None:
            K.dma("sp", d, ap_sb, deps, [Dep()], nc_ok=True)

    mixT = K.sb(ss, "mixT", [128, 8, T], BF16)

    def make_xT(s1):
        xT = K.sb(s1, "xT", [128, 8, T], BF16)
        with K.scope() as sa:
            xin = [K.sb(sa, "xin", [128, D], F32) for _ in range(2)]
            xb = [K.sb(sa, "xb", [128, D], BF16) for _ in range(2)]
            junk = K.sb(sa, "junk", [128, D], BF16)
            st = [K.sb(sa, "st", [128, 4], F32) for _ in range(2)]
            for i in range(NT):
                xi, xbi, sti = xin[i % 2], xb[i % 2], st[i % 2]
                K.dma("sp", xi[:], x[tok0 + i * 128: tok0 + (i + 1) * 128, :], [], [xi])
                K.op("act", lambda e: e.activation(out=junk[:], in_=xi[:], func=AF.Square,
                                                   accum_out=sti[:, 0:1]), [xi], [junk, sti])
                K.op("act", lambda e: e.activation(out=sti[:, 1:2], in_=sti[:, 0:1], func=AF.Sqrt,
                                                   bias=epsc[:, 0:1], scale=1.0 / D), [sti, epsc], [sti])
                K.op("dve", lambda e: e.reciprocal(out=sti[:, 2:3], in_=sti[:, 1:2]), [sti], [sti])
                K.op("dve", lambda e: e.tensor_scalar(out=xbi[:], in0=xi[:], scalar1=sti[:, 2:3], scalar2=None,
                                                      op0=ALU.mult), [xi, sti], [xbi])
                pb = PS[i % 2]
                for kc in range(8):
                    K.op("pe", lambda e, kc=kc: e.transpose(
                        out=pb.t[:].bitcast(BF16)[:, kc * 128:(kc + 1) * 128],
                        in_=xbi[:, kc * 128:(kc + 1) * 128], identity=identb[:]), [xbi, identb], [pb])
                K.op("act" if i % 2 else "dve", lambda e: (e.tensor_copy if hasattr(e, "tensor_copy") else e.copy)(
                    out=xT[:, :, i * 128:(i + 1) * 128],
                    in_=pb.t[:].bitcast(BF16).rearrange("p (k t) -> p k t", k=8)), [pb], [xT])
        return xT

    if stop_after == "A0":
        with K.scope() as s0:
            xT = make_xT(s0)
            dump("xT", xT[:, :, 0:128], [xT], [128, 8, 128], BF16)
        return
    with K.scope() as s1:
        _gdn(nc, K, s1, g, dbg, stop_after, dump, seq, L, make_xT, mixT)
    if stop_after in ("gdn", "gdn_pre"):
        return
    with K.scope() as s2:
        _dsa(nc, K, s2, g, dbg, stop_after, dump, seq, L, make_xT, mixT)
    if stop_after == "dsa":
        return
    _mlp(nc, K, ss, g, dbg, stop_after, dump, seq, L, mixT)


def bc_mid(ap2, n):
    return ap2.unsqueeze(1).to_broadcast([ap2.shape[0], n, ap2.shape[1]])


def bc_last(ap2, n):
    return ap2.unsqueeze(2).to_broadcast([ap2.shape[0], ap2.shape[1], n])


def _gdn(nc, K, s1, g, dbg, stop_after, dump, seq, L, make_xT, mixT):
    PS = L["PS"]
    ident = L["ident"]; identb = L["identb"]; ones = L["ones"]
    epsc = L["epsc"]; onec = L["onec"]
    cm2 = L["cm2"]; cmr2 = L["cmr2"]
    m_lt = L["m_lt"]; m_gt = L["m_gt"]; m_ge = L["m_ge"]
    cw = L["cw"]; onB = L["onB"]; dtB = L["dtB"]; negA = L["negA"]
    winb = g["winb"]
    NCH = T // 64
    rr = [0]

    def nb():
        rr[0] += 1
        return PS[rr[0] % 8]

    def bfv(p):
        return p.t[:].bitcast(BF16)

    with K.scope() as sg:
        cvT = K.sb(sg, "cvT", [128, 12, T], BF16)
        sz = K.sb(sg, "sz", [64, NCH, 512], BF16)
        abbb = K.sb(sg, "abbb", [64, NCH, 8], F32)
        with K.scope() as sw:
            xT = make_xT(sw)
            NG = INC - O_QB
            wg = K.sb(sw, "wg", [128, 8, NG], BF16)
            K.dma("sp", wg[:], winb[:, O_QB:INC].rearrange("(k p) c -> p k c", p=128), [], [wg])
            ev = 0
            for tb in range(4):
                for cc in range(12):
                    p = nb()
                    for kc in range(8):
                        K.op("pe", lambda e: e.matmul(p[:, :], lhsT=wg[:, kc, cc * 128:(cc + 1) * 128],
                                                      rhs=xT[:, kc, tb * 512:(tb + 1) * 512],
                                                      start=(kc == 0), stop=(kc == 7)), [wg, xT], [p])
                    if ev % 2 == 0:
                        K.op("act", lambda e: e.copy(out=cvT[:, cc, tb * 512:(tb + 1) * 512], in_=p[:, :]),
                             [p], [cvT])
                    else:
                        K.op("dve", lambda e: e.tensor_copy(out=cvT[:, cc, tb * 512:(tb + 1) * 512], in_=p[:, :]),
                             [p], [cvT])
                    ev += 1
            for ch in range(NCH):
                p = nb()
                pz = nb()
                for kc in range(8):
                    K.op("pe", lambda e: e.matmul(p[0:64, 0:8], lhsT=xT[:, kc, ch * 64:(ch + 1) * 64],
                                                  rhs=wg[:, kc, 1536:1544], start=(kc == 0), stop=(kc == 7)),
                         [wg, xT], [p])
                for kc in range(8):
                    K.op("pe", lambda e: e.matmul(pz[0:64, :], lhsT=xT[:, kc, ch * 64:(ch + 1) * 64],
                                                  rhs=wg[:, kc, 1544:2056], start=(kc == 0), stop=(kc == 7)),
                         [wg, xT], [pz])
                K.op("dve", lambda e: e.tensor_copy(out=abbb[:, ch, :], in_=p[0:64, 0:8]), [p], [abbb])
                K.op("act", lambda e: e.activation(out=sz[:, ch, :], in_=pz[0:64, :], func=AF.Silu), [pz], [sz])
        dump("qkv_pre", cvT[:, :, 0:256], [cvT], [128, 12, 256], BF16)
        dump("abbb", abbb[:, 0:4, :], [abbb], [64, 4, 8])

        gst = K.sb(sg, "gst", [64, NCH * 4], F32)
        beta = K.sb(sg, "beta", [64, NCH * 4], F32)
        eg = K.sb(sg, "eg", [64, NCH * 4], F32)
        egr = K.sb(sg, "egr", [64, NCH * 4], F32)
        egl = K.sb(sg, "egl", [128, NCH * 4], F32)
        gv = lambda t: t[:].rearrange("p (c h) -> p c h", h=4)
        K.op("dve", lambda e: e.tensor_tensor(out=gv(gst), in0=abbb[:, :, 0:4], in1=bc_mid(dtB[:, :], NCH),
                                              op=ALU.add), [abbb, dtB], [gst])
        K.op("act", lambda e: e.activation(out=gst[:], in_=gst[:], func=AF.Exp), [gst], [gst])
        K.op("act", lambda e: e.activation(out=gst[:], in_=gst[:], func=AF.Ln, bias=onec[0:64, 0:1], scale=1.0),
             [gst, onec], [gst])
        K.op("dve", lambda e: e.tensor_tensor(out=gv(gst), in0=gv(gst), in1=bc_mid(negA[:, :], NCH),
                                              op=ALU.mult), [gst, negA], [gst])
        K.op("act", lambda e: e.activation(out=gv(beta), in_=abbb[:, :, 4:8], func=AF.Sigmoid), [abbb], [beta])
        pG = nb()
        K.op("pe", lambda e: e.matmul(pG[0:64, 0:128], lhsT=cm2[0:64, :], rhs=gst[:], start=True, stop=True),
             [cm2, gst], [pG])
        K.op("pe", lambda e: e.matmul(pG[0:64, 128:256], lhsT=cmr2[0:64, :], rhs=gst[:], start=True, stop=True),
             [cmr2, gst], [pG])
        K.op("pe", lambda e: e.matmul(pG[:, 256:384], lhsT=ones[0:64, :], rhs=gst[:], start=True, stop=True),
             [ones, gst], [pG])
        K.op("act", lambda e: e.activation(out=eg[:], in_=pG[0:64, 0:128], func=AF.Exp), [pG], [eg])
        K.op("act", lambda e: e.activation(out=egr[:], in_=pG[0:64, 128:256], func=AF.Exp), [pG], [egr])
        K.op("act", lambda e: e.activation(out=egl[:], in_=pG[:, 256:384], func=AF.Exp), [pG], [egl])
        dump("gst", gst[:], [gst], [64, NCH * 4])
        dump("eg", eg[:], [eg], [64, NCH * 4])

        with K.scope() as sc:
            acc = [K.sb(sc, "cacc", [128, T], F32) for _ in range(2)]
            for cc in range(12):
                a = acc[cc % 2]
                K.op("dve", lambda e: e.tensor_scalar(out=a[:, :], in0=cvT[:, cc, :], scalar1=cw[:, cc, 3:4],
                                                      scalar2=None, op0=ALU.mult), [cvT, cw], [a])
                for sh in (1, 2, 3):
                    K.op("dve", lambda e: e.scalar_tensor_tensor(
                        out=a[:, sh:T], in0=cvT[:, cc, 0:T - sh], scalar=cw[:, cc, 3 - sh:4 - sh],
                        in1=a[:, sh:T], op0=ALU.mult, op1=ALU.add), [cvT, cw, a], [a])
                K.op("act", lambda e: e.activation(out=cvT[:, cc, :], in_=a[:, :], func=AF.Silu), [a], [cvT])
        dump("qkv_conv", cvT[:, :, 0:256], [cvT], [128, 12, 256], BF16)
        if stop_after == "gdn_pre":
            return

        S32 = K.sb(sg, "S32", [128, 512], F32)
        Sb = K.sb(sg, "Sb", [128, 512], BF16)
        K.op("dve", lambda e: e.memset(S32[:], 0.0), [], [S32])
        K.op("dve", lambda e: e.memset(Sb[:], 0.0), [], [Sb])
        ncm = K.sb(sg, "ncm", [64, 64], F32)
        K.op("dve", lambda e: e.tensor_scalar(out=ncm[:], in0=cm2[0:64, :], scalar1=-1.0, scalar2=None,
                                              op0=ALU.mult), [cm2], [ncm])
        idb = identb[0:64, 0:64]
        W2 = []
        for par in range(2):
            w = {}
            w["qk32"] = K.sb(sg, "qk32", [64, 8, 128], F32)
            w["sq"] = K.sb(sg, "sq", [64, 8, 128], F32)
            w["ss"] = K.sb(sg, "ss", [64, 8], F32)
            w["rn"] = K.sb(sg, "rn", [64, 8], F32)
            w["sc"] = K.sb(sg, "sc", [64, 6, 4], F32)
            for nm in ("qh", "kh", "kb", "kbg", "kd", "qg", "vb"):
                w[nm] = K.sb(sg, nm, [64, 4, 128], BF16)
            w["fT"] = K.sb(sg, "fT", [128, 16, 64], BF16)
            w["Gb"] = K.sb(sg, "Gb", [64, 4, 64], F32)
            w["Dn"] = K.sb(sg, "Dn", [64, 256], F32)
            w["Dp"] = K.sb(sg, "Dp", [64, 256], F32)
            w["E"] = K.sb(sg, "E", [64, 256], F32)
            w["Et"] = K.sb(sg, "Et", [64, 256], F32)
            w["EL"] = K.sb(sg, "EL", [64, 4, 64], F32)
            w["ELt"] = K.sb(sg, "ELt", [64, 4, 64], F32)
            w["EAt"] = K.sb(sg, "EAt", [64, 4, 64], F32)
            w["M"] = [K.sb(sg, "M", [64, 4, 64], BF16) for _ in range(2)]
            w["Mt"] = [K.sb(sg, "Mt", [64, 4, 64], BF16) for _ in range(2)]
            w["Atm"] = K.sb(sg, "Atm", [64, 4, 64], BF16)
            w["Pt32"] = K.sb(sg, "Pt32", [64, 4, 64], F32)
            w["Ptb"] = K.sb(sg, "Ptb", [64, 4, 64], BF16)
            w["negwT"] = K.sb(sg, "negwT", [128, 4, 64], BF16)
            w["vnew"] = K.sb(sg, "vnew", [64, 512], BF16)
            w["o32"] = K.sb(sg, "o32", [64, 4, 128], F32)
            _v = Tl.__new__(Tl)
            _v.t = w["sq"].t[:, 0:4, :]
            _v.d = w["sq"].d
            _v.s = []
            w["osq"] = _v
            w["os"] = K.sb(sg, "os", [64, 8], F32)
            w["ob"] = K.sb(sg, "ob", [64, 512], BF16)
            W2.append(w)

        import os as _os
        if _os.environ.get('CH_LIMIT'):
            K.limit = int(_os.environ['CH_LIMIT'])
        def prep(ch):
            w = W2[ch % 2]
            c0, c1 = ch * 64, (ch + 1) * 64
            g0, g1 = ch * 4, ch * 4 + 4
            qk32, sq, ss_, rn, sc = w["qk32"], w["sq"], w["ss"], w["rn"], w["sc"]
            pa = nb(); pb = nb()
            for cc in range(8):
                K.op("pe", lambda e: e.transpose(out=bfv(pa)[0:64, cc * 128:(cc + 1) * 128],
                                                 in_=cvT[:, cc, c0:c1], identity=identb[:]), [cvT, identb], [pa])
            for cc in range(4):
                K.op("pe", lambda e: e.transpose(out=bfv(pb)[0:64, cc * 128:(cc + 1) * 128],
                                                 in_=cvT[:, 8 + cc, c0:c1], identity=identb[:]), [cvT, identb], [pb])
            K.op("act", lambda e: e.copy(out=qk32[:].rearrange("p a b -> p (a b)"), in_=bfv(pa)[0:64, :]),
                 [pa], [qk32])
            K.op("dve", lambda e: e.tensor_tensor(out=sq[:], in0=qk32[:], in1=qk32[:], op=ALU.mult), [qk32], [sq])
            K.op("dve", lambda e: e.tensor_reduce(out=ss_[:], in_=sq[:], axis=AX.X, op=ALU.add), [sq], [ss_])
            K.op("act", lambda e: e.activation(out=rn[:], in_=ss_[:], func=AF.Sqrt, bias=epsc[0:64, 0:1], scale=1.0),
                 [ss_, epsc], [rn])
            K.op("dve", lambda e: e.reciprocal(out=rn[:], in_=rn[:]), [rn], [rn])
            K.op("dve", lambda e: e.tensor_scalar(out=sc[:, 0, :], in0=rn[:, 0:4], scalar1=128.0 ** -0.5,
                                                  scalar2=None, op0=ALU.mult), [rn], [sc])
            K.op("dve", lambda e: e.tensor_tensor(out=sc[:, 1, :], in0=rn[:, 4:8], in1=beta[:, g0:g1],
                                                  op=ALU.mult), [rn, beta], [sc])
            K.op("dve", lambda e: e.tensor_tensor(out=sc[:, 2, :], in0=sc[:, 1, :], in1=eg[:, g0:g1],
                                                  op=ALU.mult), [sc, eg], [sc])
            K.op("dve", lambda e: e.tensor_tensor(out=sc[:, 3, :], in0=rn[:, 4:8], in1=egr[:, g0:g1],
                                                  op=ALU.mult), [rn, egr], [sc])
            K.op("dve", lambda e: e.tensor_tensor(out=sc[:, 4, :], in0=sc[:, 0, :], in1=eg[:, g0:g1],
                                                  op=ALU.mult), [sc, eg], [sc])
            q32 = qk32[:, 0:4, :]
            k32 = qk32[:, 4:8, :]
            plan = (("qh", q32, sc[:, 0, :], "dve"), ("kh", k32, rn[:, 4:8], "dve"),
                    ("kb", k32, sc[:, 1, :], "dve"), ("kbg", k32, sc[:, 2, :], "dve"),
                    ("kd", k32, sc[:, 3, :], "dve"), ("qg", q32, sc[:, 4, :], "dve"))
            for nm, src, scl, eng in plan:
                K.op(eng, lambda e: e.tensor_tensor(out=w[nm][:], in0=src, in1=bc_last(scl, 128), op=ALU.mult),
                     [qk32, sc, rn], [w[nm]])
            K.op("dve", lambda e: e.tensor_tensor(
                out=w["vb"][:], in0=bfv(pb)[0:64, 0:512].rearrange("p (h d) -> p h d", h=4),
                in1=bc_last(beta[:, g0:g1], 128), op=ALU.mult), [pb, beta], [w["vb"]])
            pc = nb()
            for ki, nm in enumerate(("kh", "kb", "qh", "qg")):
                for h in range(4):
                    K.op("pe", lambda e: e.transpose(out=bfv(pc)[:, (ki * 4 + h) * 64:(ki * 4 + h + 1) * 64],
                                                     in_=w[nm][:, h, :], identity=idb), [w[nm], identb], [pc])
            fT = w["fT"]
            K.op("act", lambda e: e.copy(out=fT[:].rearrange("p a b -> p (a b)"), in_=bfv(pc)[:, :]), [pc], [fT])
            pd = nb(); pe_ = nb()
            for h in range(4):
                K.op("pe", lambda e: e.matmul(pd[0:64, h * 64:(h + 1) * 64], lhsT=fT[:, 4 + h, :], rhs=fT[:, h, :],
                                              start=True, stop=True), [fT], [pd])
                K.op("pe", lambda e: e.matmul(pd[0:64, 256 + h * 64:256 + (h + 1) * 64], lhsT=fT[:, h, :],
                                              rhs=fT[:, 4 + h, :], start=True, stop=True), [fT], [pd])
                K.op("pe", lambda e: e.matmul(pe_[0:64, h * 64:(h + 1) * 64], lhsT=fT[:, h, :], rhs=fT[:, 8 + h, :],
                                              start=True, stop=True), [fT], [pe_])
            Gb = w["Gb"]
            K.op("dve", lambda e: e.tensor_copy(out=Gb[:], in_=bc_last(gst[:, g0:g1], 64)), [gst], [Gb])
            for h in range(4):
                K.op("pe", lambda e: e.matmul(pe_[0:64, 256 + h * 64:256 + (h + 1) * 64], lhsT=cm2[0:64, :],
                                              rhs=Gb[:, h, :], start=True, stop=False), [cm2, Gb], [pe_])
                K.op("pe", lambda e: e.matmul(pe_[0:64, 256 + h * 64:256 + (h + 1) * 64], lhsT=Gb[:, h, :],
                                              rhs=ncm[:], start=False, stop=True), [ncm, Gb], [pe_])
            Dn, Dp, E, Et = w["Dn"], w["Dp"], w["E"], w["Et"]
            K.op("dve", lambda e: e.tensor_scalar(out=Dn[:], in0=pe_[0:64, 256:512], scalar1=0.0, scalar2=None,
                                                  op0=ALU.min), [pe_], [Dn])
            K.op("dve", lambda e: e.tensor_scalar(out=Dp[:], in0=pe_[0:64, 256:512], scalar1=0.0, scalar2=None,
                                                  op0=ALU.max), [pe_], [Dp])
            K.op("act", lambda e: e.activation(out=E[:], in_=Dn[:], func=AF.Exp), [Dn], [E])
            K.op("act", lambda e: e.activation(out=Et[:], in_=Dp[:], func=AF.Exp, scale=-1.0), [Dp], [Et])
            v4 = lambda t: t[:].rearrange("p (h s) -> p h s", h=4)
            EL, ELt, EAt = w["EL"], w["ELt"], w["EAt"]
            K.op("dve", lambda e: e.tensor_tensor(out=EL[:], in0=v4(E), in1=bc_mid(m_lt[:, :], 4), op=ALU.mult),
                 [E, m_lt], [EL])
            K.op("dve", lambda e: e.tensor_tensor(out=ELt[:], in0=v4(Et), in1=bc_mid(m_gt[:, :], 4), op=ALU.mult),
                 [Et, m_gt], [ELt])
            K.op("dve", lambda e: e.tensor_tensor(out=EAt[:], in0=v4(Et), in1=bc_mid(m_ge[:, :], 4), op=ALU.mult),
                 [Et, m_ge], [EAt])
            M, Mt = w["M"][0], w["Mt"][0]
            Atm, Pt32, Ptb = w["Atm"], w["Pt32"], w["Ptb"]
            pv4 = lambda p, o: p[0:64, o:o + 256].rearrange("p (h s) -> p h s", h=4)
            K.op("dve", lambda e: e.tensor_tensor(out=M[:], in0=pv4(pd, 0), in1=EL[:], op=ALU.mult), [pd, EL], [M])
            K.op("dve", lambda e: e.tensor_tensor(out=Mt[:], in0=pv4(pd, 256), in1=ELt[:], op=ALU.mult),
                 [pd, ELt], [Mt])
            K.op("dve", lambda e: e.tensor_tensor(out=Atm[:], in0=pv4(pe_, 0), in1=EAt[:], op=ALU.mult),
                 [pe_, EAt], [Atm])
            K.op("dve", lambda e: e.tensor_tensor(out=Pt32[:], in0=bc_mid(ident[0:64, 0:64], 4), in1=Mt[:],
                                                   op=ALU.subtract), [ident, Mt], [Pt32])
            K.op("dve", lambda e: e.tensor_copy(out=Ptb[:], in_=Pt32[:]), [Pt32], [Ptb])
            for lev in range(5):
                Mn, Mtn = w["M"][(lev + 1) % 2], w["Mt"][(lev + 1) % 2]
                p1 = nb()
                for h in range(4):
                    K.op("pe", lambda e: e.matmul(p1[0:64, h * 64:(h + 1) * 64], lhsT=Mt[:, h, :], rhs=M[:, h, :],
                                                  start=True, stop=True), [M, Mt], [p1])
                if lev < 4:
                    for h in range(4):
                        K.op("pe", lambda e: e.matmul(p1[0:64, 256 + h * 64:256 + (h + 1) * 64], lhsT=M[:, h, :],
                                                      rhs=Mt[:, h, :], start=True, stop=True), [M, Mt], [p1])
                K.op("act", lambda e: e.copy(out=Mn[:], in_=pv4(p1, 0)), [p1], [Mn])
                if lev < 4:
                    K.op("dve", lambda e: e.tensor_copy(out=Mtn[:], in_=pv4(p1, 256)), [p1], [Mtn])
                p2 = nb()
                for h in range(4):
                    K.op("pe", lambda e: e.matmul(p2[0:64, h * 64:(h + 1) * 64], lhsT=Mn[:, h, :], rhs=Ptb[:, h, :],
                                                  start=True, stop=True), [Mn, Ptb], [p2])
                K.op("dve", lambda e: e.tensor_tensor(out=Pt32[:], in0=pv4(p2, 0), in1=Pt32[:], op=ALU.add),
                     [p2, Pt32], [Pt32])
                K.op("dve", lambda e: e.tensor_copy(out=Ptb[:], in_=Pt32[:]), [Pt32], [Ptb])
                M, Mt = Mn, Mtn
            if ch < 2:
                dump("Pt%d" % ch, Pt32[:], [Pt32], [64, 4, 64])
            negwT, vnew = w["negwT"], w["vnew"]
            p3 = nb()
            for h in range(4):
                K.op("pe", lambda e: e.matmul(p3[:, h * 64:(h + 1) * 64], lhsT=w["kbg"][:, h, :], rhs=Ptb[:, h, :],
                                              start=True, stop=True), [w["kbg"], Ptb], [p3])
            K.op("act", lambda e: e.activation(out=negwT[:].rearrange("p h c -> p (h c)"), in_=p3[:, 0:256],
                                               func=AF.Copy, scale=-1.0), [p3], [negwT])
        def rec(ch):
            w = W2[ch % 2]
            c0, c1 = ch * 64, (ch + 1) * 64
            g0, g1 = ch * 4, ch * 4 + 4
            fT = w["fT"]
            Atm, Ptb = w["Atm"], w["Ptb"]
            negwT, vnew = w["negwT"], w["vnew"]
            p4 = nb()
            for h in range(4):
                K.op("pe", lambda e: e.matmul(p4[0:64, h * 128:(h + 1) * 128], lhsT=Ptb[:, h, :], rhs=w["vb"][:, h, :],
                                              start=True, stop=False), [Ptb, w["vb"]], [p4])
                K.op("pe", lambda e: e.matmul(p4[0:64, h * 128:(h + 1) * 128], lhsT=negwT[:, h, :],
                                              rhs=Sb[:, h * 128:(h + 1) * 128], start=False, stop=True),
                     [negwT, Sb], [p4])
            K.op("act", lambda e: e.copy(out=vnew[:], in_=p4[0:64, :]), [p4], [vnew])
            p5 = nb()
            for h in range(4):
                K.op("pe", lambda e: e.matmul(p5[0:64, h * 128:(h + 1) * 128], lhsT=fT[:, 12 + h, :],
                                              rhs=Sb[:, h * 128:(h + 1) * 128], start=True, stop=False),
                     [fT, Sb], [p5])
                K.op("pe", lambda e: e.matmul(p5[0:64, h * 128:(h + 1) * 128], lhsT=Atm[:, h, :],
                                              rhs=vnew[:, h * 128:(h + 1) * 128], start=False, stop=True),
                     [Atm, vnew], [p5])
            p6 = nb()
            for h in range(4):
                K.op("pe", lambda e: e.matmul(p6[:, h * 128:(h + 1) * 128], lhsT=w["kd"][:, h, :],
                                              rhs=vnew[:, h * 128:(h + 1) * 128], start=True, stop=True),
                     [w["kd"], vnew], [p6])
            S4 = S32[:].rearrange("p (h d) -> p h d", h=4)
            K.op("dve", lambda e: e.tensor_tensor(out=S4, in0=S4, in1=bc_last(egl[:, g0:g1], 128), op=ALU.mult),
                 [S32, egl], [S32])
            K.op("dve", lambda e: e.tensor_tensor(out=S32[:], in0=p6[:, :], in1=S32[:], op=ALU.add), [p6, S32], [S32])
            K.op("act", lambda e: e.copy(out=Sb[:], in_=S32[:]), [S32], [Sb])
            o32, osq, os_, ob = w["o32"], w["osq"], w["os"], w["ob"]
            K.op("act", lambda e: e.copy(out=o32[:].rearrange("p h d -> p (h d)"), in_=p5[0:64, :]), [p5], [o32])
            K.op("dve", lambda e: e.tensor_tensor(out=osq[:], in0=o32[:], in1=o32[:], op=ALU.mult), [o32], [osq])
            K.op("dve", lambda e: e.tensor_reduce(out=os_[:, 0:4], in_=osq[:], axis=AX.X, op=ALU.add), [osq], [os_])
            K.op("act", lambda e: e.activation(out=os_[:, 4:8], in_=os_[:, 0:4], func=AF.Sqrt, bias=epsc[0:64, 0:1],
                                               scale=1.0 / 128), [os_, epsc], [os_])
            K.op("dve", lambda e: e.reciprocal(out=os_[:, 4:8], in_=os_[:, 4:8]), [os_], [os_])
            K.op("dve", lambda e: e.tensor_tensor(out=o32[:], in0=o32[:], in1=bc_last(os_[:, 4:8], 128), op=ALU.mult),
                 [o32, os_], [o32])
            K.op("dve", lambda e: e.tensor_tensor(out=o32[:], in0=o32[:], in1=bc_mid(onB[:, :], 4), op=ALU.mult),
                 [o32, onB], [o32])
            K.op("dve", lambda e: e.tensor_tensor(out=ob[:], in0=o32[:].rearrange("p h d -> p (h d)"),
                                                  in1=sz[:, ch, :], op=ALU.mult), [o32, sz], [ob])
            p7 = nb()
            for h in range(4):
                K.op("pe", lambda e: e.transpose(out=bfv(p7)[:, h * 64:(h + 1) * 64], in_=ob[:, h * 128:(h + 1) * 128],
                                                 identity=idb), [ob, identb], [p7])
            K.op("act", lambda e: e.copy(out=mixT[:, 4:8, c0:c1],
                                         in_=bfv(p7)[:, 0:256].rearrange("p (h t) -> p h t", h=4)), [p7], [mixT])
        NCHR = int(_os.environ.get('GDN_NCH', NCH))
        ch = 0
        while ch < NCHR:
            if ch + 1 < NCHR:
                K.interleave([lambda c=ch: prep(c), lambda c=ch + 1: prep(c)])
                rec(ch)
                rec(ch + 1)
                ch += 2
            else:
                prep(ch)
                rec(ch)
                ch += 1
        dump("ob", mixT[:, 4:8, 0:256], [mixT], [128, 4, 256], BF16)


def _dsa(nc, K, s1, g, dbg, stop_after, dump, seq, L, make_xT, mixT):
    PS = L["PS"]
    ident = L["ident"]; identb = L["identb"]; epsc = L["epsc"]
    negdiag = L["negdiag"]; kreq = L["kreq"]; pow2 = L["pow2"]; cnB = L["cnB"]
    wukT = L["wukT"]; wuvb = L["wuvb"]; Tb = L["Tb"]; b15B = L["b15B"]
    winb = g["winb"]
    rr = [0]

    def nb(lo=0, n=6):
        rr[0] += 1
        return PS[lo + rr[0] % n]

    def bfv(p):
        return p.t[:].bitcast(BF16)

    with K.scope() as sd:
        qabsT = K.sb(sd, "qabsT", [128, 8, T], BF16)
        cT = K.sb(sd, "cT", [128, T], BF16)
        Vaug = K.sb(sd, "Vaug", [128, NT, 8, 65], BF16)
        qiT = K.sb(sd, "qiT", [128, 4, T], BF16)
        kiT2 = K.sb(sd, "kiT2", [128, T], BF16)
        wi = K.sb(sd, "wi", [128, NT, 8], F32)
        K.op("pool", lambda e: e.memset(Vaug[:, :, :, 64:65], 1.0), [], [Vaug])
        with K.scope() as sw:
            xT = make_xT(sw)
            wd = K.sb(sw, "wd", [128, 8, 1224], BF16)
            K.dma("sp", wd[:], winb[:, 0:1224].rearrange("(k p) c -> p k c", p=128), [], [wd])
            wki2 = K.sb(sw, "wki2", [128, 8, 128], BF16)
            K.op("dve", lambda e: e.tensor_copy(out=wki2[:, :, 0:64], in_=wd[:, :, O_KI:O_KI + 64]), [wd], [wki2])
            K.op("pool", lambda e: e.tensor_copy(out=wki2[:, :, 64:128], in_=wd[:, :, O_KI:O_KI + 64]), [wd], [wki2])
            qaT = K.sb(sw, "qaT", [128, 4, T], BF16)
            ckv = K.sb(sw, "ckv", [128, NT, 128], F32)
            cb = K.sb(sw, "cb", [128, 128], BF16)
            cst_ = K.sb(sw, "cst", [128, 4], F32)
            cjk = K.sb(sw, "cjk", [128, 128], BF16)
            ev = 0
            for tb in range(4):
                tsl = slice(tb * 512, (tb + 1) * 512)
                jobs = [(wd, j * 128, qaT, j) for j in range(4)] + [(wd, O_QI + j * 128, qiT, j) for j in range(4)]
                for wsrc, c0, dst, j in jobs:
                    p = nb()
                    for kc in range(8):
                        K.op("pe", lambda e: e.matmul(p[:, :], lhsT=wsrc[:, kc, c0:c0 + 128], rhs=xT[:, kc, tsl],
                                                      start=(kc == 0), stop=(kc == 7)), [wsrc, xT], [p])
                    if ev % 2:
                        K.op("act", lambda e: e.copy(out=dst[:, j, tsl], in_=p[:, :]), [p], [dst])
                    else:
                        K.op("dve", lambda e: e.tensor_copy(out=dst[:, j, tsl], in_=p[:, :]), [p], [dst])
                    ev += 1
                p = nb()
                for kc in range(8):
                    K.op("pe", lambda e: e.matmul(p[:, :], lhsT=wki2[:, kc, :], rhs=xT[:, kc, tsl],
                                                  start=(kc == 0), stop=(kc == 7)), [wki2, xT], [p])
                K.op("act", lambda e: e.copy(out=kiT2[:, tsl], in_=p[:, :]), [p], [kiT2])
            for i in range(NT):
                isl = slice(i * 128, (i + 1) * 128)
                p = nb()
                for kc in range(8):
                    K.op("pe", lambda e: e.matmul(p[:, 0:128], lhsT=xT[:, kc, isl], rhs=wd[:, kc, O_CKV:O_CKV + 128],
                                                  start=(kc == 0), stop=(kc == 7)), [wd, xT], [p])
                for kc in range(8):
                    K.op("pe", lambda e: e.matmul(p[:, 128:136], lhsT=xT[:, kc, isl], rhs=wd[:, kc, O_WI:O_WI + 8],
                                                  start=(kc == 0), stop=(kc == 7)), [wd, xT], [p])
                K.op("dve", lambda e: e.tensor_copy(out=ckv[:, i, :], in_=p[:, 0:128]), [p], [ckv])
                K.op("dve", lambda e: e.tensor_copy(out=wi[:, i, :], in_=p[:, 128:136]), [p], [wi])
                K.op("act", lambda e: e.activation(out=cjk[:], in_=ckv[:, i, :], func=AF.Square,
                                                   accum_out=cst_[:, 0:1]), [ckv], [cjk, cst_])
                K.op("act", lambda e: e.activation(out=cst_[:, 1:2], in_=cst_[:, 0:1], func=AF.Sqrt,
                                                   bias=epsc[:, 0:1], scale=1.0 / 128), [cst_, epsc], [cst_])
                K.op("dve", lambda e: e.reciprocal(out=cst_[:, 1:2], in_=cst_[:, 1:2]), [cst_], [cst_])
                K.op("dve", lambda e: e.scalar_tensor_tensor(out=cb[:], in0=ckv[:, i, :], scalar=cst_[:, 1:2],
                                                             in1=cnB[:], op0=ALU.mult, op1=ALU.mult),
                     [ckv, cst_, cnB], [cb])
                p2 = nb()
                K.op("pe", lambda e: e.transpose(out=bfv(p2)[:, 0:128], in_=cb[:], identity=identb[:]),
                     [cb, identb], [p2])
                K.op("act", lambda e: e.copy(out=cT[:, isl], in_=bfv(p2)[:, 0:128]), [p2], [cT])
                p3 = nb()
                K.op("pe", lambda e: e.matmul(p3[:, :], lhsT=cT[:, isl], rhs=wuvb[:], start=True, stop=True),
                     [cT, wuvb], [p3])
                K.op("dve", lambda e: e.tensor_copy(out=Vaug[:, i, :, 0:64],
                                                    in_=p3[:, :].rearrange("p (h d) -> p h d", h=8)), [p3], [Vaug])
            for tb in range(4):
                tsl = slice(tb * 512, (tb + 1) * 512)
                for h in range(8):
                    j, hf = h // 2, h % 2
                    psl = slice(hf * 64, (hf + 1) * 64)
                    p = nb()
                    K.op("pe", lambda e: e.matmul(p[:, :], lhsT=wukT[psl, j, :], rhs=qaT[psl, j, tsl],
                                                  start=True, stop=True), [wukT, qaT], [p])
                    K.op("act", lambda e: e.activation(out=qabsT[:, h, tsl], in_=p[:, :], func=AF.Copy, scale=0.125),
                         [p], [qabsT])
        dump("cT", cT[:, 0:256], [cT], [128, 256], BF16)
        dump("qabsT", qabsT[:, :, 0:128], [qabsT], [128, 8, 128], BF16)

        with K.scope() as sq:
            score = K.sb(sq, "score", [128, T], F32)
            relu = [K.sb(sq, "relu", [128, 512], BF16) for _ in range(4)]
            diagw = K.sb(sq, "diagw", [128, 8, 128], BF16)
            cjunk = K.sb(sq, "cjunk", [128, T], BF16)
            bs = K.sb(sq, "bs", [128, 8], F32)
            wtab = K.sb(sq, "wtab", [128, NBIS], F32)
            sel = K.sb(sq, "sel", [128, T], BF16)
            negselT = K.sb(sq, "negselT", [128, NT, 128], BF16)
            Pm = [K.sb(sq, "Pm", [128, 512], BF16) for _ in range(3)]
            oa = K.sb(sq, "oa", [128, 8, 65], F32)
            rden = K.sb(sq, "rden", [128, 8], F32)
            oab = K.sb(sq, "oab", [128, 8, 64], BF16)
            WSC = (8.0 ** -0.5) * (64.0 ** -0.5)
            pmi = 0
            import os as _os
            def selA(qt):
                qsl = slice(qt * 128, (qt + 1) * 128)
                nkb = qt + 1
                nk = nkb * 128
                for h in range(8):
                    K.op("dve", lambda e: e.tensor_scalar(
                        out=diagw[:, h, :], in0=ident[:], scalar1=wi[:, qt, h:h + 1], scalar2=WSC,
                        op0=ALU.mult, op1=ALU.mult), [ident, wi], [diagw])
                for g4 in range((nkb + 3) // 4):
                    s0 = g4 * 512
                    sn = min(512, nk - s0)
                    psc = PS[6 + g4 % 2]
                    phs = {}

                    def qk(h):
                        j, hf = h // 2, h % 2
                        psl = slice(hf * 64, (hf + 1) * 64)
                        ph = nb()
                        phs[h] = ph
                        K.op("pe", lambda e: e.matmul(ph[:, 0:sn], lhsT=qiT[psl, j, qsl], rhs=kiT2[psl, s0:s0 + sn],
                                                      start=True, stop=True), [qiT, kiT2], [ph])
                    for h in range(3):
                        qk(h)
                    for h in range(8):
                        if h + 3 < 8:
                            qk(h + 3)
                        r = relu[h % 4]
                        ph = phs[h]
                        if h % 2:
                            K.op("act", lambda e: e.activation(out=r[:, 0:sn], in_=ph[:, 0:sn], func=AF.Relu), [ph], [r])
                        else:
                            K.op("dve", lambda e: e.tensor_scalar(out=r[:, 0:sn], in0=ph[:, 0:sn], scalar1=0.0,
                                                                  scalar2=None, op0=ALU.max), [ph], [r])
                        K.op("pe", lambda e: e.matmul(psc[:, 0:sn], lhsT=diagw[:, h, :], rhs=r[:, 0:sn],
                                                      start=(h == 0), stop=(h == 7)), [diagw, r], [psc])
                    K.op("act", lambda e: e.copy(out=score[:, s0:s0 + sn], in_=psc[:, 0:sn]), [psc], [score])
                K.op("dve", lambda e: e.tensor_reduce(out=bs[:, 0:1], in_=score[:, 0:nk], axis=AX.X, op=ALU.max,
                                                      apply_absolute_value=True), [score], [bs])
                K.op("dve", lambda e: e.tensor_scalar(out=bs[:, 0:1], in0=bs[:, 0:1], scalar1=1.0001, scalar2=1e-6,
                                                      op0=ALU.mult, op1=ALU.add), [bs], [bs])
                K.op("dve", lambda e: e.tensor_tensor(out=score[:, nk - 128:nk], in0=score[:, nk - 128:nk],
                                                      in1=negdiag[:], op=ALU.add), [score, negdiag], [score])
                K.op("dve", lambda e: e.tensor_scalar(out=wtab[:], in0=pow2[:], scalar1=bs[:, 0:1], scalar2=None,
                                                      op0=ALU.mult), [pow2, bs], [wtab])
                K.op("dve", lambda e: e.memset(bs[:, 1:2], 0.0), [], [bs])
                for it in range(NBIS):
                    K.op("dve", lambda e: e.tensor_scalar(out=cjunk[:, 0:nk], in0=score[:, 0:nk], scalar1=bs[:, 1:2],
                                                          scalar2=0.0, op0=ALU.is_ge, op1=ALU.add,
                                                          accum_out=bs[:, 2:3]), [score, bs], [cjunk, bs])
                    K.op("dve", lambda e: e.tensor_scalar(out=bs[:, 3:4], in0=bs[:, 2:3], scalar1=kreq[:, qt:qt + 1],
                                                          scalar2=0.5, op0=ALU.is_ge, op1=ALU.subtract),
                         [bs, kreq], [bs])
                    K.op("dve", lambda e: e.scalar_tensor_tensor(out=bs[:, 1:2], in0=bs[:, 3:4],
                                                                 scalar=wtab[:, it:it + 1], in1=bs[:, 1:2],
                                                                 op0=ALU.mult, op1=ALU.add), [bs, wtab], [bs])
                K.op("dve", lambda e: e.scalar_tensor_tensor(out=bs[:, 4:5], in0=wtab[:, NBIS - 1:NBIS], scalar=-0.5,
                                                             in1=bs[:, 1:2], op0=ALU.mult, op1=ALU.add),
                     [bs, wtab], [bs])
                K.op("dve", lambda e: e.tensor_scalar(out=sel[:, 0:nk], in0=score[:, 0:nk], scalar1=bs[:, 4:5],
                                                      scalar2=None, op0=ALU.is_ge), [score, bs], [sel])
                if qt == 3:
                    dump("sel3", sel[:, 0:512], [sel], [128, 512], BF16)
                    dump("score3", score[:, 0:512], [score], [128, 512])
            def selB(qt):
                nkb = qt + 1
                for k0 in range(0, nkb, 8):
                    kn = min(8, nkb - k0)
                    pT = nb()
                    for kk in range(kn):
                        K.op("pe", lambda e: e.transpose(out=bfv(pT)[:, kk * 128:(kk + 1) * 128],
                                                         in_=sel[:, (k0 + kk) * 128:(k0 + kk + 1) * 128],
                                                         identity=identb[:]), [sel, identb], [pT])
                    K.op("dve", lambda e: e.tensor_scalar(
                        out=negselT[:, k0:k0 + kn, :].rearrange("p k t -> p (k t)"), in0=bfv(pT)[:, 0:kn * 128],
                        scalar1=-1.0, scalar2=-NEG, op0=ALU.add, op1=ALU.mult), [pT], [negselT])
            def att(qt):
                nonlocal pmi
                qsl = slice(qt * 128, (qt + 1) * 128)
                nkb = qt + 1
                poA, poB = PS[6], PS[7]
                for h in range(8):
                    po = poA if h < 4 else poB
                    hh = h % 4
                    for g4 in range((nkb + 3) // 4):
                        kbs = list(range(g4 * 4, min(nkb, g4 * 4 + 4)))
                        n = len(kbs)
                        ps_ = nb()
                        for idx, kb in enumerate(kbs):
                            reg = ps_[:, idx * 128:(idx + 1) * 128]
                            near = kb >= qt - 1
                            K.op("pe", lambda e: e.matmul(reg, lhsT=cT[:, kb * 128:(kb + 1) * 128], rhs=qabsT[:, h, qsl],
                                                          start=True, stop=False), [cT, qabsT], [ps_])
                            K.op("pe", lambda e: e.matmul(reg, lhsT=identb[:], rhs=negselT[:, kb, :],
                                                          start=False, stop=(not near)), [identb, negselT], [ps_])
                            if near:
                                u0 = 0 if kb == qt else 128
                                K.op("pe", lambda e: e.matmul(reg, lhsT=identb[:], rhs=Tb[:, h, u0:u0 + 128],
                                                              start=False, stop=True), [identb, Tb], [ps_])
                        pm = Pm[pmi % 3]
                        pmi += 1
                        K.op("act", lambda e: e.activation(out=pm[:, 0:n * 128], in_=ps_[:, 0:n * 128], func=AF.Exp,
                                                           bias=b15B[:, h:h + 1], scale=1.0), [ps_, b15B], [pm])
                        for idx, kb in enumerate(kbs):
                            K.op("pe", lambda e: e.matmul(po[:, hh * 65:(hh + 1) * 65],
                                                          lhsT=pm[:, idx * 128:(idx + 1) * 128], rhs=Vaug[:, kb, h, :],
                                                          start=(kb == 0), stop=(kb == nkb - 1)), [pm, Vaug], [po])
                K.op("act", lambda e: e.copy(out=oa[:, 0:4, :].rearrange("p h d -> p (h d)"), in_=poA[:, 0:260]),
                     [poA], [oa])
                K.op("dve", lambda e: e.tensor_copy(out=oa[:, 4:8, :].rearrange("p h d -> p (h d)"), in_=poB[:, 0:260]),
                     [poB], [oa])
                K.op("dve", lambda e: e.reciprocal(out=rden[:], in_=oa[:, :, 64]), [oa], [rden])
                K.op("dve", lambda e: e.tensor_tensor(out=oab[:], in0=oa[:, :, 0:64], in1=bc_last(rden[:, :], 64),
                                                      op=ALU.mult), [oa, rden], [oab])
                pX = nb()
                for j in range(4):
                    K.op("pe", lambda e: e.transpose(out=bfv(pX)[:, j * 128:(j + 1) * 128],
                                                     in_=oab[:, 2 * j:2 * j + 2, :].rearrange("p h d -> p (h d)"),
                                                     identity=identb[:]), [oab, identb], [pX])
                K.op("act", lambda e: e.copy(out=mixT[:, 0:4, qsl],
                                             in_=bfv(pX)[:, 0:512].rearrange("p (k t) -> p k t", k=4)), [pX], [mixT])
            NQ = int(_os.environ.get("DSA_NQT", NT))
            selA(0)
            selB(0)
            for qt in range(NQ):
                if qt + 1 < NQ:
                    selA(qt + 1)
                att(qt)
                if qt + 1 < NQ:
                    selB(qt + 1)
        dump("oa", mixT[:, 0:4, 0:512], [mixT], [128, 4, 512], BF16)


def _mlp(nc, K, ss, g, dbg, stop_after, dump, seq, L, mixT):
    PS = L["PS"]
    identb = L["identb"]; epsc = L["epsc"]; g2B = L["g2B"]; g4B = L["g4B"]
    woutb, w1b, w2b = g["woutb"], g["w1b"], g["w2b"]
    x = g["x"]; out = g["out"]
    tok0 = seq * T

    def bfv(p):
        return p.t[:].bitcast(BF16)

    with K.scope() as sm:
        wo = K.sb(sm, "wo", [128, 8, D], BF16)
        K.dma("sp", wo[:], woutb.rearrange("(k p) c -> p k c", p=128), [], [wo])
        x1 = K.sb(sm, "x1", [128, 4, D], F32)
        xr = [K.sb(sm, "xr", [128, D], F32) for _ in range(2)]
        xnb = K.sb(sm, "xnb", [128, D], BF16)
        xnT = K.sb(sm, "xnT", [128, 8, 512], BF16)
        hT = K.sb(sm, "hT", [128, 32, 512], BF16)
        w1t = [K.sb(sm, "w1t", [128, 8, 512], BF16) for _ in range(2)]
        w2t = [K.sb(sm, "w2t", [128, 4, D], BF16) for _ in range(2)]
        r32 = [K.sb(sm, "r32", [128, 512], F32) for _ in range(2)]
        yt = K.sb(sm, "yt", [128, D], F32)
        junk = K.sb(sm, "mjunk", [128, 512], BF16)
        st = K.sb(sm, "mst", [128, 8], F32)

        def rms_from_psum(pA, pB, col):
            K.op("act", lambda e: e.activation(out=junk[:], in_=pA[:, :], func=AF.Square, accum_out=st[:, 0:1]),
                 [pA], [junk, st])
            K.op("act", lambda e: e.activation(out=junk[:], in_=pB[:, :], func=AF.Square, accum_out=st[:, 1:2]),
                 [pB], [junk, st])
            K.op("dve", lambda e: e.tensor_tensor(out=st[:, 2:3], in0=st[:, 0:1], in1=st[:, 1:2], op=ALU.add),
                 [st], [st])
            K.op("act", lambda e: e.activation(out=st[:, col:col + 1], in_=st[:, 2:3], func=AF.Sqrt,
                                               bias=epsc[:, 0:1], scale=1.0 / D), [st, epsc], [st])
            K.op("dve", lambda e: e.reciprocal(out=st[:, col:col + 1], in_=st[:, col:col + 1]), [st], [st])

        cnt = 0
        for tb in range(4):
            for ti in range(4):
                i = tb * 4 + ti
                xri = xr[i % 2]
                K.dma("sp", xri[:], x[tok0 + i * 128: tok0 + (i + 1) * 128, :], [], [xri])
                pA, pB = PS[2 * (ti % 2)], PS[2 * (ti % 2) + 1]
                for half, p in enumerate((pA, pB)):
                    for kc in range(8):
                        K.op("pe", lambda e: e.matmul(p[:, :], lhsT=mixT[:, kc, i * 128:(i + 1) * 128],
                                                      rhs=wo[:, kc, half * 512:(half + 1) * 512],
                                                      start=(kc == 0), stop=(kc == 7)), [mixT, wo], [p])
                rms_from_psum(pA, pB, 3)
                for half, p in enumerate((pA, pB)):
                    K.op("dve", lambda e: e.scalar_tensor_tensor(
                        out=x1[:, ti, half * 512:(half + 1) * 512], in0=p[:, :], scalar=st[:, 3:4],
                        in1=g2B[:, half * 512:(half + 1) * 512], op0=ALU.mult, op1=ALU.mult), [p, st, g2B], [x1])
                K.op("dve", lambda e: e.tensor_tensor(out=x1[:, ti, :], in0=x1[:, ti, :], in1=xri[:], op=ALU.add),
                     [x1, xri], [x1])
                K.op("act", lambda e: e.activation(out=xnb[:], in_=x1[:, ti, :], func=AF.Square,
                                                   accum_out=st[:, 4:5]), [x1], [xnb, st])
                K.op("act", lambda e: e.activation(out=st[:, 5:6], in_=st[:, 4:5], func=AF.Sqrt, bias=epsc[:, 0:1],
                                                   scale=1.0 / D), [st, epsc], [st])
                K.op("dve", lambda e: e.reciprocal(out=st[:, 5:6], in_=st[:, 5:6]), [st], [st])
                K.op("dve", lambda e: e.tensor_scalar(out=xnb[:], in0=x1[:, ti, :], scalar1=st[:, 5:6], scalar2=None,
                                                      op0=ALU.mult), [x1, st], [xnb])
                pt = PS[4 + (ti % 2)]
                for kc in range(8):
                    K.op("pe", lambda e: e.transpose(out=bfv(pt)[:, kc * 128:(kc + 1) * 128],
                                                     in_=xnb[:, kc * 128:(kc + 1) * 128], identity=identb[:]),
                         [xnb, identb], [pt])
                K.op("act", lambda e: e.copy(out=xnT[:, :, ti * 128:(ti + 1) * 128],
                                             in_=bfv(pt).rearrange("p (k t) -> p k t", k=8)), [pt], [xnT])
            for fb in range(8):
                wt = w1t[fb % 2]
                K.dma("sp", wt[:], w1b[:, fb * 512:(fb + 1) * 512].rearrange("(k p) c -> p k c", p=128), [], [wt])
                for fc in range(4):
                    p = PS[cnt % 8]
                    r = r32[cnt % 2]
                    for kc in range(8):
                        K.op("pe", lambda e: e.matmul(p[:, :], lhsT=wt[:, kc, fc * 128:(fc + 1) * 128],
                                                      rhs=xnT[:, kc, :], start=(kc == 0), stop=(kc == 7)),
                             [wt, xnT], [p])
                    K.op("act", lambda e: e.activation(out=r[:], in_=p[:, :], func=AF.Relu), [p], [r])
                    K.op("dve", lambda e: e.tensor_tensor(
                        out=hT[:, fb * 4 + fc, :], in0=r[:], in1=r[:], op=ALU.mult), [r], [hT])
                    cnt += 1
            for fg in range(8):
                wt2 = w2t[fg % 2]
                K.dma("sp", wt2[:], w2b[fg * 512:(fg + 1) * 512, :].rearrange("(c p) n -> p c n", p=128), [], [wt2])
                for c4 in range(4):
                    fc = fg * 4 + c4
                    for ti in range(4):
                        for half in range(2):
                            p = PS[ti * 2 + half]
                            K.op("pe", lambda e: e.matmul(p[:, :], lhsT=hT[:, fc, ti * 128:(ti + 1) * 128],
                                                          rhs=wt2[:, c4, half * 512:(half + 1) * 512],
                                                          start=(fc == 0), stop=(fc == 31)), [hT, wt2], [p])
            for ti in range(4):
                i = tb * 4 + ti
                pA, pB = PS[ti * 2], PS[ti * 2 + 1]
                rms_from_psum(pA, pB, 6)
                for half, p in enumerate((pA, pB)):
                    K.op("dve", lambda e: e.scalar_tensor_tensor(
                        out=yt[:, half * 512:(half + 1) * 512], in0=p[:, :], scalar=st[:, 6:7],
                        in1=g4B[:, half * 512:(half + 1) * 512], op0=ALU.mult, op1=ALU.mult), [p, st, g4B], [yt])
                K.op("dve", lambda e: e.tensor_tensor(out=yt[:], in0=yt[:], in1=x1[:, ti, :], op=ALU.add),
                     [yt, x1], [yt])
                K.dma("sp", out[tok0 + i * 128: tok0 + (i + 1) * 128, :], yt[:], [yt], [Dep()])


INPUT_NAMES = ["x", "w_in", "c_norm", "w_uk", "w_uv", "rel_bias", "conv_w", "a_log", "dt_bias", "o_norm",
               "w_out", "pre_norm_mix", "post_norm_mix", "pre_norm_mlp", "post_norm_mlp", "w_mlp_in", "w_mlp_out"]


def make_in_maps(inputs, n_cores=8):
    f = lambda a: np.ascontiguousarray(np.asarray(a, dtype=np.float32))
    shared = {
        "w_in": f(inputs["w_in"])[0], "c_norm": f(inputs["c_norm"])[0], "w_uk": f(inputs["w_uk"])[0],
        "w_uv": f(inputs["w_uv"])[0], "rel_bias": f(inputs["rel_bias"]), "conv_w": f(inputs["conv_w"])[0],
        "a_log": f(inputs["a_log"])[0], "dt_bias": f(inputs["dt_bias"])[0], "o_norm": f(inputs["o_norm"])[0],
        "w_out": f(inputs["w_out"])[0], "pre_norm_mix": f(inputs["pre_norm_mix"])[0],
        "post_norm_mix": f(inputs["post_norm_mix"])[0], "pre_norm_mlp": f(inputs["pre_norm_mlp"])[0],
        "post_norm_mlp": f(inputs["post_norm_mlp"])[0], "w_mlp_in": f(inputs["w_mlp_in"])[0],
        "w_mlp_out": f(inputs["w_mlp_out"])[0], "ohr": onehot_rev(),
    }
    xs = f(inputs["x"])
    maps = []
    for c in range(n_cores):
        m = dict(shared)
        m["x"] = np.ascontiguousarray(xs[c * NSEQ:(c + 1) * NSEQ].reshape(NSEQ * T, D))
        maps.append(m)
    return maps


def kernel(**inputs):
    nc = build_nc()
    maps = make_in_maps(inputs)
    res = run_bass_kernel_spmd(nc, maps, core_ids=list(range(8)))
    outs = [np.asarray(r["out"], dtype=np.float32).reshape(NSEQ, T, D) for r in res.results]
    return np.concatenate(outs, axis=0)
```
